# Optimizing a Trainium2 kernel written in Bass

```python
import math
import jax, jax.numpy as jnp
from jax import lax
import numpy as np

D_MODEL = 1024
BATCH = 2
SEQ = 8192
DEPTH = 1

N_META = 16
BLOCK = 128
PAD_FRONT = BLOCK - N_META
N_HEADS = 8
QK_NOPE = 64
QK_ROPE = 32
V_DIM = 64
Q_LORA = 256
KV_LORA = 128
ROPE_THETA = 10000.0
ATTN_SCALE = (QK_NOPE + QK_ROPE) ** -0.5
ATTN_WIDTH = N_HEADS * V_DIM
CONV_DIM = 512
CONV_GROUPS = 8
CONV_W = 3
MIX_WIDTH = ATTN_WIDTH + CONV_DIM
IN_COLS = Q_LORA + KV_LORA + QK_ROPE + 3 * CONV_DIM
SPLITS = [Q_LORA, Q_LORA + KV_LORA, Q_LORA + KV_LORA + QK_ROPE,
          Q_LORA + KV_LORA + QK_ROPE + CONV_DIM, Q_LORA + KV_LORA + QK_ROPE + 2 * CONV_DIM]
N_GROUPS = 4
EXPERTS_PER_GROUP = 8
N_EXPERTS = N_GROUPS * EXPERTS_PER_GROUP
TOP_K = 2
D_FF = 256
EPS = 1e-6
NEG_INF = -1e30

kernel_name = "hymba_mla_shortconv_hier_moe"


def rms_norm(x, g):
    xf = x.astype(jnp.float32)
    y = xf * lax.rsqrt(jnp.mean(xf * xf, axis=-1, keepdims=True) + EPS)
    return (y * g.astype(jnp.float32)).astype(x.dtype)


def rope_angles(pos):
    inv_freq = 1.0 / (ROPE_THETA ** (jnp.arange(0, QK_ROPE, 2, dtype=jnp.float32) / QK_ROPE))
    ang = pos.astype(jnp.float32)[..., None] * inv_freq
    return jnp.cos(ang), jnp.sin(ang)


def apply_rope(x, cos, sin):
    xf = x.astype(jnp.float32)
    x1, x2 = jnp.split(xf, 2, axis=-1)
    return jnp.concatenate([x1 * cos - x2 * sin, x2 * cos + x1 * sin], axis=-1).astype(x.dtype)


def causal_block_attention(q_nope, q_rope, k_nope, k_rope, v):
    bsz, plen = q_nope.shape[0], q_nope.shape[1]
    nb = plen // BLOCK
    qn = q_nope.reshape(bsz, nb, BLOCK, N_HEADS, QK_NOPE).transpose(1, 0, 2, 3, 4)
    qr = q_rope.reshape(bsz, nb, BLOCK, N_HEADS, QK_ROPE).transpose(1, 0, 2, 3, 4)
    starts = jnp.arange(nb, dtype=jnp.int32) * BLOCK
    k_idx = jnp.arange(plen, dtype=jnp.int32)

    def one_block(args):
        qn_b, qr_b, s0 = args
        s = (jnp.einsum('bqhd,bkhd->bhqk', qn_b, k_nope, preferred_element_type=jnp.float32)
             + jnp.einsum('bqhr,bkr->bhqk', qr_b, k_rope, preferred_element_type=jnp.float32)) * ATTN_SCALE
        q_idx = s0 + jnp.arange(BLOCK, dtype=jnp.int32)
        mask = (k_idx[None, :] >= PAD_FRONT) & (k_idx[None, :] <= q_idx[:, None])
        s = jnp.where(mask[None, None], s, NEG_INF)
        p = jax.nn.softmax(s, axis=-1).astype(v.dtype)
        return jnp.einsum('bhqk,bkhd->bqhd', p, v)

    out = lax.map(one_block, (qn, qr, starts))
    return out.transpose(1, 0, 2, 3, 4).reshape(bsz, plen, N_HEADS * V_DIM)


def hybrid_mixer(h, cos, sin, w_in, q_norm, w_uq, kv_norm, w_ukv, conv_w,
                 attn_out_norm, conv_out_norm, w_out):
    bsz = h.shape[0]
    hp = jnp.pad(h, ((0, 0), (PAD_FRONT, 0), (0, 0)))
    plen = hp.shape[1]
    z = hp @ w_in
    c_q, c_kv, k_pe, gate_b, gate_c, x_in = jnp.split(z, SPLITS, axis=-1)

    q = (rms_norm(c_q, q_norm) @ w_uq).reshape(bsz, plen, N_HEADS, QK_NOPE + QK_ROPE)
    q_nope = q[..., :QK_NOPE]
    q_rope = apply_rope(q[..., QK_NOPE:], cos[:, :, None, :], sin[:, :, None, :])
    kv = (rms_norm(c_kv, kv_norm) @ w_ukv).reshape(bsz, plen, N_HEADS, QK_NOPE + V_DIM)
    k_nope, v = kv[..., :QK_NOPE], kv[..., QK_NOPE:]
    k_rope = apply_rope(k_pe, cos, sin)
    attn = causal_block_attention(q_nope, q_rope, k_nope, k_rope, v)

    u = gate_c * x_in
    u_pad = jnp.pad(u, ((0, 0), (CONV_W - 1, 0), (0, 0)))
    y = sum(conv_w[k] * u_pad[:, k:k + plen] for k in range(CONV_W))
    conv = gate_b * y

    merged = jnp.concatenate([rms_norm(attn[:, PAD_FRONT:], attn_out_norm),
                              rms_norm(conv[:, PAD_FRONT:], conv_out_norm)], axis=-1)
    return merged @ w_out


def hier_moe(h, w_group_router, b_group_router, w_expert_router, b_expert_router,
             w_gate, w_up, w_down, w_sh_gate, w_sh_up, w_sh_down):
    bsz, slen, d = h.shape
    t = h.reshape(-1, d)
    g_prob = jax.nn.softmax((t @ w_group_router).astype(jnp.float32) + b_group_router.astype(jnp.float32), axis=-1)
    g_w, g_idx = lax.top_k(g_prob, 1)
    e_logits = ((t @ w_expert_router).astype(jnp.float32) + b_expert_router.astype(jnp.float32))
    e_logits = e_logits.reshape(-1, N_GROUPS, EXPERTS_PER_GROUP)
    e_logits = jnp.take_along_axis(e_logits, g_idx[:, :, None], axis=1)[:, 0]
    e_prob = jax.nn.softmax(e_logits, axis=-1)
    e_w, e_idx = lax.top_k(e_prob, TOP_K)
    e_w = e_w / jnp.sum(e_w, axis=-1, keepdims=True)
    weights = g_w * e_w
    eid = g_idx * EXPERTS_PER_GROUP + e_idx
    combine = jnp.einsum('nk,nke->ne', weights,
                         jax.nn.one_hot(eid, N_EXPERTS, dtype=jnp.float32)).astype(t.dtype)
    a = jnp.einsum('nd,edf->nef', t, w_gate)
    b = jnp.einsum('nd,edf->nef', t, w_up)
    hid = jax.nn.silu(a) * b * combine[:, :, None]
    routed = jnp.einsum('nef,efd->nd', hid, w_down)
    shared = (jax.nn.silu(t @ w_sh_gate) * (t @ w_sh_up)) @ w_sh_down
    return (routed + shared).reshape(bsz, slen, d)


def setup_inputs(seed: int = 0) -> dict:
    key = jax.random.key(seed)
    ks = jax.random.split(key, 32)
    f32 = jnp.float32

    def nrm(k, shape, fan_in):
        return jax.random.normal(k, shape, f32) * (fan_in ** -0.5)

    def gain(k, shape):
        return 1.0 + 0.05 * jax.random.normal(k, shape, f32)

    L = DEPTH
    return {
        "x": jax.random.normal(ks[0], (BATCH, SEQ, D_MODEL), f32),
        "positions": jnp.broadcast_to(jnp.arange(SEQ, dtype=jnp.int32), (BATCH, SEQ)),
        "meta_tokens": jax.random.normal(ks[1], (N_META, D_MODEL), f32),
        "pre_mix_norm": gain(ks[2], (L, D_MODEL)),
        "w_in": nrm(ks[3], (L, D_MODEL, IN_COLS), D_MODEL),
        "q_norm": gain(ks[4], (L, Q_LORA)),
        "w_uq": nrm(ks[5], (L, Q_LORA, N_HEADS * (QK_NOPE + QK_ROPE)), Q_LORA),
        "kv_norm": gain(ks[6], (L, KV_LORA)),
        "w_ukv": nrm(ks[7], (L, KV_LORA, N_HEADS * (QK_NOPE + V_DIM)), KV_LORA),
        "conv_w": nrm(ks[8], (L, CONV_W, CONV_DIM), CONV_W),
        "attn_out_norm": gain(ks[9], (L, ATTN_WIDTH)),
        "conv_out_norm": gain(ks[10], (L, CONV_DIM)),
        "w_out": nrm(ks[11], (L, MIX_WIDTH, D_MODEL), MIX_WIDTH),
        "post_mix_norm": gain(ks[12], (L, D_MODEL)),
        "pre_ffn_norm": gain(ks[13], (L, D_MODEL)),
        "w_group_router": nrm(ks[14], (L, D_MODEL, N_GROUPS), D_MODEL),
        "b_group_router": 0.01 * jax.random.normal(ks[15], (L, N_GROUPS), f32),
        "w_expert_router": nrm(ks[16], (L, D_MODEL, N_EXPERTS), D_MODEL),
        "b_expert_router": 0.01 * jax.random.normal(ks[17], (L, N_EXPERTS), f32),
        "w_gate": nrm(ks[18], (L, N_EXPERTS, D_MODEL, D_FF), D_MODEL),
        "w_up": nrm(ks[19], (L, N_EXPERTS, D_MODEL, D_FF), D_MODEL),
        "w_down": nrm(ks[20], (L, N_EXPERTS, D_FF, D_MODEL), D_FF),
        "w_sh_gate": nrm(ks[21], (L, D_MODEL, D_FF), D_MODEL),
        "w_sh_up": nrm(ks[22], (L, D_MODEL, D_FF), D_MODEL),
        "w_sh_down": nrm(ks[23], (L, D_FF, D_MODEL), D_FF),
        "post_ffn_norm": gain(ks[24], (L, D_MODEL)),
    }


def reference(x, positions, meta_tokens, pre_mix_norm, w_in, q_norm, w_uq, kv_norm, w_ukv, conv_w,
              attn_out_norm, conv_out_norm, w_out, post_mix_norm, pre_ffn_norm,
              w_group_router, b_group_router, w_expert_router, b_expert_router,
              w_gate, w_up, w_down, w_sh_gate, w_sh_up, w_sh_down, post_ffn_norm):
    bsz = x.shape[0]
    meta = jnp.broadcast_to(meta_tokens[None].astype(x.dtype), (bsz, N_META, D_MODEL))
    h = jnp.concatenate([meta, x], axis=1)
    rope_pos = jnp.concatenate([
        jnp.zeros((bsz, PAD_FRONT), jnp.int32),
        jnp.broadcast_to(jnp.arange(N_META, dtype=jnp.int32), (bsz, N_META)),
        positions.astype(jnp.int32) + N_META], axis=1)
    cos, sin = rope_angles(rope_pos)

    for l in range(DEPTH):
        mix = hybrid_mixer(rms_norm(h, pre_mix_norm[l]), cos, sin, w_in[l], q_norm[l], w_uq[l],
                           kv_norm[l], w_ukv[l], conv_w[l], attn_out_norm[l], conv_out_norm[l], w_out[l])
        h = h + rms_norm(mix, post_mix_norm[l])
        ffn = hier_moe(rms_norm(h, pre_ffn_norm[l]), w_group_router[l], b_group_router[l],
                       w_expert_router[l], b_expert_router[l], w_gate[l], w_up[l], w_down[l],
                       w_sh_gate[l], w_sh_up[l], w_sh_down[l])
        h = h + rms_norm(ffn, post_ffn_norm[l])

    return h[:, N_META:]
```

```python
import os
from contextlib import ExitStack

import numpy as np
import concourse.bass as bass
import concourse.mybir as mybir
from concourse.bass_utils import run_bass_kernel_spmd

F32 = mybir.dt.float32
BF16 = mybir.dt.bfloat16
I32 = mybir.dt.int32
AF = mybir.ActivationFunctionType
ALU = mybir.AluOpType
AX = mybir.AxisListType

D = 1024
SEQ = 8192
NCH = 17
P_TOT = 128 + 16 * 512
NKB = 65
EXT = [17, 33, 49, 65]
UNC0 = [1, 17, 33, 49]
SCALE = float((64 + 32) ** -0.5)
EPS = 1e-6
MAGIC = 12582912.0
TWO_PI = 6.283185307179586
NEXP = 32
CAP = int(os.environ.get('MK_CAP', '128'))
NOV = 30
NSLOT = 64 * 128 + NOV * 128


class Q:
    def __init__(self, eng, sem):
        self.eng, self.sem, self.n, self.seen = eng, sem, 0, {}


class Buf:
    __slots__ = ("w", "rs", "dsem", "dn")

    def __init__(self, dsem=None):
        self.w, self.rs, self.dsem, self.dn = None, {}, dsem, 0


def _wait(q, tok):
    if tok is None:
        return
    sem, v = tok
    k = id(sem)
    if q.seen.get(k, 0) >= v:
        return
    q.eng.wait_ge(sem, v)
    q.seen[k] = v


_LD = {"pending": [], "buf": None, "last_total": 0, "waited": set()}


def _finish_loads():
    ld = _LD["buf"]
    if _LD["pending"]:
        for b in _LD["pending"]:
            b.w = (ld.dsem, ld.dn)
        _LD["pending"] = []
        _LD["last_total"] = ld.dn
        _LD["waited"] = set()


def op(q, fn, reads=(), writes=(), dma=None):
    if _LD["pending"] and dma is not _LD["buf"]:
        _finish_loads()
    for b in reads:
        _wait(q, b.w)
    for b in writes:
        _wait(q, b.w)
        for k, t in b.rs.items():
            _wait(q, t)
    ins = fn()
    if dma is not None:
        ins.then_inc(dma.dsem, 16)
        dma.dn += 16
        tok = (dma.dsem, dma.dn)
    else:
        ins.then_inc(q.sem, 1)
        q.n += 1
        tok = (q.sem, q.n)
    for b in reads:
        k = id(tok[0])
        if b.rs.get(k, (None, 0))[1] < tok[1]:
            b.rs[k] = tok
    for b in writes:
        b.w = tok
        b.rs = {}
    return tok


def build_program(dbg=False):
    nc = bass.Bass("TRN2", target_bir_lowering=False)

    def din(name, shape, dt=F32):
        return nc.dram_tensor(name, list(shape), dt, kind="ExternalInput").ap()

    def dout(name, shape, dt=F32):
        return nc.dram_tensor(name, list(shape), dt, kind="ExternalOutput").ap()

    hT0 = din("hT0", [128, 8, 128])
    hTm = din("hTm", [16, 128, 8, 512])
    hq = din("hq", [4, 128, 8, 514])
    posrow = din("pos", [1, SEQ], I32)
    posq = din("posq", [4, 512], I32)
    xown = din("xown", [16, 128, D])
    abias_d = din("abias", [128, 12])
    w_in = din("w_in", [128, 8, 1952])
    w_uq = din("w_uq", [128, 2, 768])
    w_ukv = din("w_ukv", [128, 1024])
    w_out_a = din("w_out_a", [128, 4, D])
    w_out_c = din("w_out_c", [128, 4, D])
    wr_d = din("wr", [128, 8, 36])
    br_d = din("br", [1, 36])
    NROW = (NEXP + 1) * 128
    wgu_lo = din("wgu_lo", [NROW, 2048])
    wgu_hi = din("wgu_hi", [NROW, 2048])
    wd_h = din("wd_h", [NROW, 2048])
    triu_d = din("triu", [128, 128])
    vb_d = din("vbase", [1, 64])
    tokid_d = din("tokid", [128, 16])
    pidx_d = din("pidx", [128, 1])
    jidx_d = din("jidx", [1, NOV])
    mapinit_d = din("mapinit", [NSLOT, 4])
    g_pfb_d = din("g_pfb", [1, D])
    wbf_gu = nc.dram_tensor("wbf_gu", [NROW, 4096], BF16, kind="Internal").ap()
    wbf_d = nc.dram_tensor("wbf_d", [NROW, 2048], BF16, kind="Internal").ap()
    hn2d = nc.dram_tensor("hn2d", [2049, D], BF16, kind="Internal").ap()
    maps_d = nc.dram_tensor("maps", [NSLOT, 4], F32, kind="Internal").ap()
    Yd = nc.dram_tensor("Yd", [4096, D], F32, kind="Internal").ap()
    g_pre_d = din("g_pre", [128, 8])
    g_q_d = din("g_q", [128, 2])
    g_kv_d = din("g_kv", [128, 1])
    g_a_d = din("g_a", [128, 4])
    g_c_d = din("g_c", [128, 4])
    convw_d = din("convw", [128, 4, 3])
    g_pm_d = din("g_pm", [1, D])
    g_pf_d = din("g_pf", [128, 8])
    g_po_d = din("g_po", [1, D])
    ident_d = din("ident", [128, 128])
    rot_d = din("rot96", [96, 96])
    invf_d = din("invf", [96, 1])
    dmask_d = din("dmask", [128, 4, 512])
    prefix_d = din("prefix", [1, 128], I32)
    y = dout("y", [16, 128, D])
    dbg_out = {}

    with ExitStack() as top:
        def sem(name):
            return top.enter_context(nc.semaphore(name))

        pe = Q(nc.tensor, sem("s_pe"))
        act = Q(nc.scalar, sem("s_act"))
        dve = Q(nc.vector, sem("s_dve"))
        pool = Q(nc.gpsimd, sem("s_pool"))
        sp = Q(nc.sync, sem("s_sp"))
        nsem = [0]

        def dbuf():
            nsem[0] += 1
            return Buf(sem("d%d" % nsem[0]))

        ARENA_BYTES = 210000
        arena = top.enter_context(nc.sbuf_tensor("arena", [128, ARENA_BYTES], mybir.dt.uint8))
        RA = (0, 32768)
        RB = (32768, 98304)
        RC = (98304, 131072)
        RD = (131072, 147456)
        RE = (147456, 155648)
        RF = (155648, ARENA_BYTES)

        class Bump:
            def __init__(self, *segs):
                self.segs = [list(sg) for sg in segs]

            def alloc(self, nbytes):
                nbytes = (nbytes + 63) // 64 * 64
                for sg in self.segs:
                    if sg[1] - sg[0] >= nbytes:
                        off = sg[0]
                        sg[0] += nbytes
                        return off
                raise RuntimeError("arena exhausted for %d bytes: %s" % (nbytes, self.segs))

        DTB = {F32: 4, BF16: 2, I32: 4}

        def SB(es, name, shape, dt):
            shape = list(shape)
            n = 1
            for d_ in shape[1:]:
                n *= d_
            off = es.alloc(n * DTB[dt])
            v = arena[0:shape[0], off:off + n * DTB[dt]].bitcast(dt)
            if len(shape) == 3:
                v = v.rearrange("p (a b) -> p a b", a=shape[1])
            elif len(shape) != 2:
                raise ValueError(shape)
            return v

        def at(region_off, shape, dt):
            return SB(Bump((region_off, ARENA_BYTES)), "", shape, dt)

        def PS(pes, name, shape, dt=F32):
            return pes.enter_context(nc.psum_tensor("ps_" + name, list(shape), dt))

        _LD.update(pending=[], buf=dbuf(), last_total=0, waited=set())
        BND = {}
        for _v in (NSLOT - 1, NROW - 1, 2048, 4095):
            BND[_v] = nc.gpsimd.alloc_register("bnd%d" % _v)
            nc.gpsimd.reg_mov(BND[_v], _v)

        def load(es, name, src, shape, dt=F32, q=None, cast=False):
            t = SB(es, name, shape, dt)
            b = Buf()
            qq = pool if cast else (q or sp)
            ld = _LD["buf"]
            if id(qq) not in _LD["waited"]:
                _wait(qq, (ld.dsem, _LD["last_total"]) if _LD["last_total"] else None)
                _LD["waited"].add(id(qq))
            op(qq, lambda: qq.eng.dma_start(out=t[:], in_=src), writes=[b], dma=ld)
            _LD["pending"].append(b)
            return t, b

        cst = Bump(RE)
        ones_bf = SB(cst, "ones_bf", [128, 128], BF16)
        b_ones = Buf()
        op(pool, lambda: nc.gpsimd.memset(ones_bf[:], 1.0), writes=[b_ones])
        rot_t, b_rot = load(cst, "rot_t", rot_d[:, :], [96, 96])
        invf_t, b_invf = load(cst, "invf_t", invf_d[:, :], [96, 1])

        QT = at(RA[0], [96, 8, 2048], BF16)
        b_QT = [[Buf() for _ in range(4)] for _ in range(8)]
        convnT = at(RD[0], [128, 4, 2048], BF16)
        b_convn = [Buf() for _ in range(4)]

        def emit_rope(es_tiles, src_ap, width, add16, tagbufs):
            pos_i, ang, kf, cs, sn, b_pos, b_ang, b_kf, b_cos, b_sin = tagbufs
            R = slice(64, 96)
            op(sp, lambda: nc.sync.dma_start(out=pos_i[R, 0:width], in_=src_ap.partition_broadcast(32)),
               writes=[b_pos], dma=b_pos)
            op(dve, lambda: nc.vector.tensor_scalar(out=ang[R, 0:width], in0=pos_i[R, 0:width], scalar1=float(add16),
                                                    scalar2=invf_t[R, 0:1], op0=ALU.add, op1=ALU.mult),
               reads=[b_pos, b_invf], writes=[b_ang])
            op(dve, lambda: nc.vector.tensor_scalar(out=kf[R, 0:width], in0=ang[R, 0:width], scalar1=1.0 / TWO_PI,
                                                    scalar2=MAGIC, op0=ALU.mult, op1=ALU.add),
               reads=[b_ang], writes=[b_kf])
            op(dve, lambda: nc.vector.tensor_scalar(out=kf[R, 0:width], in0=kf[R, 0:width], scalar1=-MAGIC,
                                                    scalar2=-TWO_PI, op0=ALU.add, op1=ALU.mult),
               reads=[b_kf], writes=[b_kf])
            op(dve, lambda: nc.vector.tensor_tensor(out=ang[R, 0:width], in0=ang[R, 0:width], in1=kf[R, 0:width], op=ALU.add),
               reads=[b_ang, b_kf], writes=[b_ang])
            op(dve, lambda: nc.vector.tensor_scalar(out=ang[R, 0:width], in0=ang[R, 0:width], scalar1=-3.1415925,
                                                    scalar2=3.1415925, op0=ALU.max, op1=ALU.min),
               reads=[b_ang], writes=[b_ang])
            op(act, lambda: nc.scalar.activation(out=sn[R, 0:width], in_=ang[R, 0:width], func=AF.Sin),
               reads=[b_ang], writes=[b_sin])
            op(act, lambda: nc.scalar.activation(out=kf[R, 0:width], in_=ang[R, 0:width], func=AF.Sin, scale=0.5),
               reads=[b_ang], writes=[b_kf])
            op(dve, lambda: nc.vector.tensor_tensor(out=kf[R, 0:width], in0=kf[R, 0:width], in1=kf[R, 0:width], op=ALU.mult),
               reads=[b_kf], writes=[b_kf])
            op(dve, lambda: nc.vector.tensor_scalar(out=cs[R, 0:width], in0=kf[R, 0:width], scalar1=-2.0,
                                                    scalar2=1.0, op0=ALU.mult, op1=ALU.add),
               reads=[b_kf], writes=[b_cos])

        def rstd_from_psum(ps_t, b_ps, rows, width, inv_n, tmp, b_tmp, out_t, b_out):
            op(act, lambda: nc.scalar.activation(out=tmp[0:rows, 0:width], in_=ps_t[0:rows, 0:width], func=AF.Sqrt,
                                                 scale=float(inv_n), bias=eps_t[0:rows, 0:1]),
               reads=[b_ps, b_eps], writes=[b_tmp])
            op(dve, lambda: nc.vector.reciprocal(out=out_t[0:rows, 0:width], in_=tmp[0:rows, 0:width]),
               reads=[b_tmp], writes=[b_out])

        eps_t = SB(cst, "eps_t", [128, 1], F32)
        b_eps = Buf()
        op(pool, lambda: nc.gpsimd.memset(eps_t[:], EPS), writes=[b_eps])

        def mm_acc(ps_ap, lhs_fn, rhs_fn, nk):
            def f():
                ins = None
                for c in range(nk):
                    ins = nc.tensor.matmul(ps_ap, lhsT=lhs_fn(c), rhs=rhs_fn(c), start=(c == 0), stop=(c == nk - 1))
                return ins
            return f

        def load_scaled_w(es, name, src_fn, ncols, g_t, b_g, stage, b_stage):
            t = SB(es, name, [128, 8, ncols], BF16)
            b = Buf()
            for c in range(8):
                k = c % 2
                op(sp, lambda: nc.sync.dma_start(out=stage[k][:, 0:ncols], in_=src_fn(c)), writes=[b_stage[k]], dma=b_stage[k])
                op(dve, lambda: nc.vector.tensor_scalar(out=t[:, c, :], in0=stage[k][:, 0:ncols], scalar1=g_t[:, c:c + 1], scalar2=None, op0=ALU.mult),
                   reads=[b_stage[k], b_g], writes=[b])
            return t, b

        with ExitStack() as pes:
            es = Bump(RF, RB, RC)
            g_pre, b_gpre = load(es, "g_pre", g_pre_d[:, :], [128, 8])
            convf = SB(es, "q_convf", [128, 4, 512], F32); b_convf = Buf()
            _stg = convf.rearrange("p a b -> p (a b)")
            _bst = dbuf()
            w_in_b, b_win = load_scaled_w(es, "w_in_b", lambda c: w_in[:, c, :], 1952, g_pre, b_gpre, [_stg, _stg], [_bst, _bst])
            w_uq_b, b_wuq = load(es, "w_uq_b", w_uq[:, :, :], [128, 2, 768], BF16, cast=True)
            g_q, b_gq = load(es, "g_q", g_q_d[:, :], [128, 2])
            g_c, b_gc = load(es, "g_c", g_c_d[:, :], [128, 4])
            convw, b_cw = load(es, "convw", convw_d[:, :, :], [128, 4, 3])

            def two(name, shape, dt, dma=False):
                return [SB(es, "%s%d" % (name, i), shape, dt) for i in range(2)], [dbuf() if dma else Buf() for i in range(2)]

            def one2(name, shape, dt):
                t_ = SB(es, name, shape, dt); b_ = Buf()
                return [t_, t_], [b_, b_]
            xf_, b_xf_ = two("q_xf", [128, 8, 514], F32, dma=True)
            xb_, b_xb_ = one2("q_xb", [128, 8, 514], BF16)
            xsq = SB(es, "q_xsq", [128, 8, 514], BF16); b_xsq = Buf()
            rstd_, b_rstd_ = one2("q_rstd", [128, 514], F32)
            rstd2_, b_rstd2_ = one2("q_rstd2", [128, 514], F32)
            tmp = SB(es, "q_tmp", [128, 514], F32); b_tmp = Buf()
            cqf = SB(es, "q_cqf", [128, 2, 512], F32); b_cqf = Buf()
            cqsq = SB(es, "q_cqsq", [128, 2, 512], BF16); b_cqsq = Buf()
            rq = SB(es, "q_rq", [128, 512], F32); b_rq = Buf()
            cqn_, b_cqn_ = one2("q_cqn", [128, 2, 512], BF16)
            QF_, b_QF_ = two("q_QF", [96, 512], F32)
            t1_, b_t1_ = one2("q_t1", [96, 512], F32)
            t2_, b_t2_ = one2("q_t2", [96, 512], F32)
            pos_i = SB(es, "q_pos", [96, 512], I32); ang = SB(es, "q_ang", [96, 512], F32)
            kf = SB(es, "q_kf", [96, 512], F32)
            cs_, b_cos_ = one2("q_cos", [96, 512], F32)
            sn_, b_sin_ = one2("q_sin", [96, 512], F32)
            b_pos = dbuf(); b_ang = Buf(); b_kf = Buf()
            gcf_, b_gcf_ = two("q_gcf", [128, 514], F32)
            u_, b_u_ = two("q_u", [128, 514], F32)
            yv_, b_y_ = two("q_y", [128, 512], F32)
            csq = SB(es, "q_csq", [128, 4, 512], BF16); b_csq = Buf()
            rc = SB(es, "q_rc", [128, 512], F32); b_rc = Buf()

            ps_st = PS(pes, "q_ps_st", [128, 512]); b_ps_st = Buf()
            ps_sm = PS(pes, "q_ps_sm", [128, 8, 2]); b_ps_sm = [Buf(), Buf(), Buf()]
            G = [PS(pes, "q_G%d" % i, [128, 512]) for i in range(6)]; b_G = [Buf() for _ in range(6)]

            for gi in range(4):
                kq = gi % 2
                xf, b_xf, xb, b_xb = xf_[kq], b_xf_[kq], xb_[kq], b_xb_[kq]
                rstd, b_rstd, rstd2, b_rstd2 = rstd_[kq], b_rstd_[kq], rstd2_[kq], b_rstd2_[kq]
                cs, b_cos, sn, b_sin = cs_[kq], b_cos_[kq], sn_[kq], b_sin_[kq]
                cqn, b_cqn = cqn_[kq], b_cqn_[kq]
                gc0 = gi * 512
                op(sp, lambda: nc.sync.dma_start(out=xf[:], in_=hq[gi]), writes=[b_xf], dma=b_xf)
                op(act, lambda: nc.scalar.activation(out=xb[:], in_=xf[:], func=AF.Copy), reads=[b_xf], writes=[b_xb])
                op(act, lambda: nc.scalar.activation(out=xsq[:], in_=xf[:], func=AF.Square), reads=[b_xf], writes=[b_xsq])
                op(pe, mm_acc(ps_st[:, :], lambda c: ones_bf[:, :], lambda c: xsq[:, c, 2:514], 8), reads=[b_ones, b_xsq], writes=[b_ps_st])
                op(pe, mm_acc(ps_sm[:, 0, :], lambda c: ones_bf[:, :], lambda c: xsq[:, c, 0:2], 8), reads=[b_ones, b_xsq], writes=[b_ps_sm[0]])
                op(act, lambda: nc.scalar.activation(out=tmp[:, 2:514], in_=ps_st[:, :], func=AF.Sqrt, scale=1.0 / D, bias=eps_t[:, 0:1]),
                   reads=[b_ps_st, b_eps], writes=[b_tmp])
                op(act, lambda: nc.scalar.activation(out=tmp[:, 0:2], in_=ps_sm[:, 0, :], func=AF.Sqrt, scale=1.0 / D, bias=eps_t[:, 0:1]),
                   reads=[b_ps_sm[0], b_eps], writes=[b_tmp])
                op(dve, lambda: nc.vector.reciprocal(out=rstd[:, :], in_=tmp[:, :]), reads=[b_tmp], writes=[b_rstd])
                op(dve, lambda: nc.vector.tensor_tensor(out=rstd2[:, :], in0=rstd[:, :], in1=rstd[:, :], op=ALU.mult), reads=[b_rstd], writes=[b_rstd2])
                emit_rope(None, posq[gi:gi + 1, :], 512, 16, (pos_i, ang, kf, cs, sn, b_pos, b_ang, b_kf, b_cos, b_sin))

                for m in range(2):
                    op(pe, mm_acc(G[m][:, :], lambda c: w_in_b[:, c, m * 128:(m + 1) * 128], lambda c: xb[:, c, 2:514], 8),
                       reads=[b_win, b_xb], writes=[b_G[m]])
                    op(dve, lambda: nc.vector.tensor_tensor(out=cqf[:, m, :], in0=G[m][:, :], in1=rstd[:, 2:514], op=ALU.mult),
                       reads=[b_G[m], b_rstd], writes=[b_cqf])
                op(act, lambda: nc.scalar.activation(out=cqsq[:], in_=cqf[:], func=AF.Square), reads=[b_cqf], writes=[b_cqsq])
                op(pe, mm_acc(ps_st[:, :], lambda c: ones_bf[:, :], lambda c: cqsq[:, c, :], 2), reads=[b_ones, b_cqsq], writes=[b_ps_st])
                rstd_from_psum(ps_st, b_ps_st, 128, 512, 1.0 / 256, tmp, b_tmp, rq, b_rq)
                for m in range(2):
                    op(dve, lambda: nc.vector.scalar_tensor_tensor(out=cqn[:, m, :], in0=cqf[:, m, :], scalar=g_q[:, m:m + 1], in1=rq[:, :],
                                                                   op0=ALU.mult, op1=ALU.mult),
                       reads=[b_cqf, b_gq, b_rq], writes=[b_cqn])
                def head_step(h):
                    kh = h % 2
                    pq, bq, pr, br_ = G[4], b_G[4], G[5], b_G[5]
                    QF, b_QF, t1, b_t1, t2, b_t2 = QF_[kh], b_QF_[kh], t1_[kh], b_t1_[kh], t2_[kh], b_t2_[kh]
                    op(pe, mm_acc(pq[0:96, :], lambda c: w_uq_b[:, c, h * 96:(h + 1) * 96], lambda c: cqn[:, c, :], 2),
                       reads=[b_wuq, b_cqn], writes=[bq])
                    op(act, lambda: nc.scalar.activation(out=QF[:, :], in_=pq[0:96, :], func=AF.Copy), reads=[bq], writes=[b_QF])
                    op(pe, lambda: nc.tensor.matmul(pr[0:96, :], lhsT=rot_t[:, :], rhs=QF[:, :], start=True, stop=True),
                       reads=[b_rot, b_QF], writes=[br_])
                    op(act, lambda: nc.scalar.activation(out=QT[0:64, h, gc0:gc0 + 512], in_=QF[0:64, :], func=AF.Copy), reads=[b_QF], writes=[b_QT[h][gi]])
                    op(dve, lambda: nc.vector.tensor_tensor(out=t1[64:96, :], in0=QF[64:96, :], in1=cs[64:96, :], op=ALU.mult),
                       reads=[b_QF, b_cos], writes=[b_t1])
                    op(dve, lambda: nc.vector.tensor_tensor(out=t2[64:96, :], in0=pr[64:96, :], in1=sn[64:96, :], op=ALU.mult),
                       reads=[br_, b_sin], writes=[b_t2])
                    op(dve, lambda: nc.vector.tensor_tensor(out=QT[64:96, h, gc0:gc0 + 512], in0=t1[64:96, :], in1=t2[64:96, :], op=ALU.add),
                       reads=[b_t1, b_t2], writes=[b_QT[h][gi]])
                def conv_step(cc):
                    kc = cc % 2
                    pa, ba, pb, bb, pc_, bc_ = G[0], b_G[0], G[1], b_G[1], G[2], b_G[2]
                    bh = b_ps_sm[1 + kc]
                    ph0, ph1 = ps_sm[:, 1 + kc * 2, :], ps_sm[:, 2 + kc * 2, :]
                    gcf, b_gcf, u, b_u, yv, b_y = gcf_[kc], b_gcf_[kc], u_[kc], b_u_[kc], yv_[kc], b_y_[kc]
                    cb = 416 + cc * 128
                    cg = 416 + 512 + cc * 128
                    cx = 416 + 1024 + cc * 128
                    op(pe, mm_acc(pb[:, :], lambda c: w_in_b[:, c, cg:cg + 128], lambda c: xb[:, c, 2:514], 8), reads=[b_win, b_xb], writes=[bb])
                    op(pe, mm_acc(pc_[:, :], lambda c: w_in_b[:, c, cx:cx + 128], lambda c: xb[:, c, 2:514], 8), reads=[b_win, b_xb], writes=[bc_])
                    op(pe, mm_acc(ph0, lambda c: w_in_b[:, c, cg:cg + 128], lambda c: xb[:, c, 0:2], 8), reads=[b_win, b_xb], writes=[bh])
                    op(pe, mm_acc(ph1, lambda c: w_in_b[:, c, cx:cx + 128], lambda c: xb[:, c, 0:2], 8), reads=[b_win, b_xb], writes=[bh])
                    op(pe, mm_acc(pa[:, :], lambda c: w_in_b[:, c, cb:cb + 128], lambda c: xb[:, c, 2:514], 8), reads=[b_win, b_xb], writes=[ba])
                    op(act, lambda: nc.scalar.activation(out=gcf[:, 2:514], in_=pb[:, :], func=AF.Copy), reads=[bb], writes=[b_gcf])
                    op(act, lambda: nc.scalar.activation(out=gcf[:, 0:2], in_=ph0, func=AF.Copy), reads=[bh], writes=[b_gcf])
                    op(dve, lambda: nc.vector.tensor_tensor(out=u[:, 2:514], in0=pc_[:, :], in1=gcf[:, 2:514], op=ALU.mult),
                       reads=[bc_, b_gcf], writes=[b_u])
                    op(dve, lambda: nc.vector.tensor_tensor(out=u[:, 0:2], in0=ph1, in1=gcf[:, 0:2], op=ALU.mult),
                       reads=[bh, b_gcf], writes=[b_u])
                    op(dve, lambda: nc.vector.tensor_tensor(out=u[:, :], in0=u[:, :], in1=rstd2[:, :], op=ALU.mult), reads=[b_u, b_rstd2], writes=[b_u])
                    op(dve, lambda: nc.vector.tensor_scalar(out=yv[:, :], in0=u[:, 0:512], scalar1=convw[:, cc, 0:1], scalar2=None, op0=ALU.mult),
                       reads=[b_u, b_cw], writes=[b_y])
                    op(dve, lambda: nc.vector.scalar_tensor_tensor(out=yv[:, :], in0=u[:, 1:513], scalar=convw[:, cc, 1:2], in1=yv[:, :], op0=ALU.mult, op1=ALU.add),
                       reads=[b_u, b_cw, b_y], writes=[b_y])
                    op(dve, lambda: nc.vector.scalar_tensor_tensor(out=yv[:, :], in0=u[:, 2:514], scalar=convw[:, cc, 2:3], in1=yv[:, :], op0=ALU.mult, op1=ALU.add),
                       reads=[b_u, b_cw, b_y], writes=[b_y])
                    op(dve, lambda: nc.vector.tensor_tensor(out=yv[:, :], in0=yv[:, :], in1=rstd[:, 2:514], op=ALU.mult), reads=[b_y, b_rstd], writes=[b_y])
                    op(dve, lambda: nc.vector.tensor_tensor(out=convf[:, cc, :], in0=pa[:, :], in1=yv[:, :], op=ALU.mult), reads=[ba, b_y], writes=[b_convf])
                for step_ in (lambda: conv_step(0), lambda: head_step(0), lambda: head_step(1), lambda: conv_step(1), lambda: head_step(2), lambda: head_step(3),
                              lambda: conv_step(2), lambda: head_step(4), lambda: head_step(5), lambda: conv_step(3), lambda: head_step(6), lambda: head_step(7)):
                    step_()
                op(act, lambda: nc.scalar.activation(out=csq[:], in_=convf[:], func=AF.Square), reads=[b_convf], writes=[b_csq])
                op(pe, mm_acc(ps_st[:, :], lambda c: ones_bf[:, :], lambda c: csq[:, c, :], 4), reads=[b_ones, b_csq], writes=[b_ps_st])
                rstd_from_psum(ps_st, b_ps_st, 128, 512, 1.0 / 512, tmp, b_tmp, rc, b_rc)
                for cc in range(4):
                    op(dve, lambda: nc.vector.scalar_tensor_tensor(out=convnT[:, cc, gc0:gc0 + 512], in0=convf[:, cc, :], scalar=g_c[:, cc:cc + 1], in1=rc[:, :],
                                                                   op0=ALU.mult, op1=ALU.mult),
                       reads=[b_convf, b_gc, b_rc], writes=[b_convn[gi]])
            nc.all_engine_barrier()

        ckvnT = at(RB[0], [128, P_TOT], BF16)
        b_ckvn = [Buf() for _ in range(NCH)]
        KT0 = at(RB[0] + 16640, [96, P_TOT], BF16)
        b_KT = [Buf(), Buf()]
        chunk_cols = [(0, 128)] + [(128 + 512 * g, 512) for g in range(16)]

        with ExitStack() as pes:
            es = Bump(RF, RC, (RB[0] + 49920, RB[1]))
            g_pre, b_gpre = load(es, "a_g_pre", g_pre_d[:, :], [128, 8])
            g_kv, b_gkv = load(es, "a_g_kv", g_kv_d[:, :], [128, 1])
            stage = [SB(es, "a_stage%d" % i, [128, 160], F32) for i in range(2)]; b_stage = [dbuf(), dbuf()]
            w_kv_b, b_wkv = load_scaled_w(es, "w_kv_b", lambda c: w_in[:, c, 256:384], 128, g_pre, b_gpre, stage, b_stage)
            w_kpe_b = SB(es, "w_kpe_b", [128, 8, 96], BF16)
            b_wkpe = Buf()
            op(dve, lambda: nc.vector.memset(w_kpe_b[:], 0.0), writes=[b_wkpe])
            for c in range(8):
                k = c % 2
                op(sp, lambda: nc.sync.dma_start(out=stage[k][:, 0:32], in_=w_in[:, c, 384:416]), writes=[b_stage[k]], dma=b_stage[k])
                op(dve, lambda: nc.vector.tensor_scalar(out=w_kpe_b[:, c, 64:96], in0=stage[k][:, 0:32], scalar1=g_pre[:, c:c + 1], scalar2=None, op0=ALU.mult),
                   reads=[b_stage[k], b_gpre], writes=[b_wkpe])
            KPE = SB(es, "a_KPE", [96, 512], F32); b_KPE = Buf()
            op(dve, lambda: nc.vector.memset(KPE[:], 0.0), writes=[b_KPE])

            def two(name, shape, dt, dma=False):
                return [SB(es, "%s%d" % (name, i), shape, dt) for i in range(2)], [dbuf() if dma else Buf() for i in range(2)]
            xf, b_xf = two("a_xf", [128, 8, 512], F32, dma=True)
            xb, b_xb = two("a_xb", [128, 8, 512], BF16)
            _xs = SB(es, "a_xsq", [128, 8, 512], BF16); _bxs = Buf()
            xsq, b_xsq = [_xs, _xs], [_bxs, _bxs]
            rstd, b_rstd = two("a_rstd", [128, 512], F32)
            _t = SB(es, "a_tmp", [128, 512], F32); _bt = Buf(); tmp, b_tmp = [_t, _t], [_bt, _bt]
            ckvf, b_ckvf = two("a_ckvf", [128, 512], F32)
            sqb, b_sqb = two("a_sqb", [128, 512], BF16)
            _r = SB(es, "a_rkv", [128, 512], F32); _br = Buf(); rkv, b_rkv = [_r, _r], [_br, _br]
            _a = SB(es, "a_t1", [96, 512], F32); _ba = Buf(); t1, b_t1 = [_a, _a], [_ba, _ba]
            _c = SB(es, "a_t2", [96, 512], F32); _bc = Buf(); t2, b_t2 = [_c, _c], [_bc, _bc]
            pos_i = SB(es, "a_pos", [96, 512], I32); ang = SB(es, "a_ang", [96, 512], F32)
            kf = SB(es, "a_kf", [96, 512], F32)
            cs_, b_cos_ = two("a_cos", [96, 512], F32)
            sn_, b_sin_ = two("a_sin", [96, 512], F32)
            b_pos = dbuf(); b_ang = Buf(); b_kf = Buf()
            ps_st = [PS(pes, "a_ps_st%d" % i, [128, 512]) for i in range(2)]; b_ps_st = [Buf(), Buf()]
            ps_kv = [PS(pes, "a_ps_kv%d" % i, [128, 512]) for i in range(2)]; b_ps_kv = [Buf(), Buf()]
            ps_kp = [PS(pes, "a_ps_kp%d" % i, [96, 512]) for i in range(2)]; b_ps_kp = [Buf(), Buf()]
            ps_r = [PS(pes, "a_ps_r%d" % i, [96, 512]) for i in range(2)]; b_ps_r = [Buf(), Buf()]

            for ci in range(NCH):
                k = ci % 2
                c0, W = chunk_cols[ci]
                cs, b_cos, sn, b_sin = cs_[k], b_cos_[k], sn_[k], b_sin_[k]
                src = hT0[:, :, :] if ci == 0 else hTm[ci - 1]
                op(sp, lambda: nc.sync.dma_start(out=xf[k][:, :, 0:W], in_=src), writes=[b_xf[k]], dma=b_xf[k])
                op(act, lambda: nc.scalar.activation(out=xb[k][:, :, 0:W], in_=xf[k][:, :, 0:W], func=AF.Copy), reads=[b_xf[k]], writes=[b_xb[k]])
                op(act, lambda: nc.scalar.activation(out=xsq[k][:, :, 0:W], in_=xf[k][:, :, 0:W], func=AF.Square), reads=[b_xf[k]], writes=[b_xsq[k]])
                if ci == 0:
                    emit_rope(None, prefix_d[0:1, :], W, 0, (pos_i, ang, kf, cs, sn, b_pos, b_ang, b_kf, b_cos, b_sin))
                else:
                    emit_rope(None, posrow[0:1, 512 * (ci - 1):512 * ci], W, 16, (pos_i, ang, kf, cs, sn, b_pos, b_ang, b_kf, b_cos, b_sin))
                op(pe, mm_acc(ps_st[k][:, 0:W], lambda c: ones_bf[:, :], lambda c: xsq[k][:, c, 0:W], 8), reads=[b_ones, b_xsq[k]], writes=[b_ps_st[k]])
                rstd_from_psum(ps_st[k], b_ps_st[k], 128, W, 1.0 / D, tmp[k], b_tmp[k], rstd[k], b_rstd[k])
                op(pe, mm_acc(ps_kv[k][:, 0:W], lambda c: w_kv_b[:, c, :], lambda c: xb[k][:, c, 0:W], 8), reads=[b_wkv, b_xb[k]], writes=[b_ps_kv[k]])
                op(pe, mm_acc(ps_kp[k][:, 0:W], lambda c: w_kpe_b[:, c, :], lambda c: xb[k][:, c, 0:W], 8), reads=[b_wkpe, b_xb[k]], writes=[b_ps_kp[k]])
                op(dve, lambda: nc.vector.tensor_tensor(out=ckvf[k][:, 0:W], in0=ps_kv[k][:, 0:W], in1=rstd[k][:, 0:W], op=ALU.mult),
                   reads=[b_ps_kv[k], b_rstd[k]], writes=[b_ckvf[k]])
                op(act, lambda: nc.scalar.activation(out=sqb[k][:, 0:W], in_=ckvf[k][:, 0:W], func=AF.Square), reads=[b_ckvf[k]], writes=[b_sqb[k]])
                op(pe, lambda: nc.tensor.matmul(ps_st[k][:, 0:W], lhsT=ones_bf[:, :], rhs=sqb[k][:, 0:W], start=True, stop=True),
                   reads=[b_ones, b_sqb[k]], writes=[b_ps_st[k]])
                op(dve, lambda: nc.vector.tensor_tensor(out=KPE[64:96, 0:W], in0=ps_kp[k][64:96, 0:W], in1=rstd[k][64:96, 0:W], op=ALU.mult),
                   reads=[b_ps_kp[k], b_rstd[k]], writes=[b_KPE])
                op(pe, lambda: nc.tensor.matmul(ps_r[k][:, 0:W], lhsT=rot_t[:, :], rhs=KPE[:, 0:W], start=True, stop=True),
                   reads=[b_rot, b_KPE], writes=[b_ps_r[k]])
                rstd_from_psum(ps_st[k], b_ps_st[k], 128, W, 1.0 / 128, tmp[k], b_tmp[k], rkv[k], b_rkv[k])
                op(dve, lambda: nc.vector.scalar_tensor_tensor(out=ckvnT[:, c0:c0 + W], in0=ckvf[k][:, 0:W], scalar=g_kv[:, 0:1], in1=rkv[k][:, 0:W],
                                                               op0=ALU.mult, op1=ALU.mult),
                   reads=[b_ckvf[k], b_gkv, b_rkv[k]], writes=[b_ckvn[ci]])
                op(dve, lambda: nc.vector.tensor_tensor(out=t1[k][64:96, 0:W], in0=KPE[64:96, 0:W], in1=cs[64:96, 0:W], op=ALU.mult),
                   reads=[b_KPE, b_cos], writes=[b_t1[k]])
                op(dve, lambda: nc.vector.tensor_tensor(out=t2[k][64:96, 0:W], in0=ps_r[k][64:96, 0:W], in1=sn[64:96, 0:W], op=ALU.mult),
                   reads=[b_ps_r[k], b_sin], writes=[b_t2[k]])
                op(dve, lambda: nc.vector.tensor_tensor(out=KT0[64:96, c0:c0 + W], in0=t1[k][64:96, 0:W], in1=t2[k][64:96, 0:W], op=ALU.add),
                   reads=[b_t1[k], b_t2[k]], writes=[b_KT[0]])
            nc.all_engine_barrier()

        attnT = at(RC[0], [128, 4, 2048], BF16)
        b_attn = [Buf() for _ in range(4)]
        with ExitStack() as pes:
            es = Bump(RF, (RC[0] + 16384, RC[1]), (RB[0] + 49920, RB[1]))
            KT1 = at(RB[0] + 33280, [96, P_TOT], BF16)
            KT = [KT0, KT1]
            op(dve, lambda: nc.vector.tensor_copy(out=KT1[64:96, :], in_=KT0[64:96, :]), reads=[b_KT[0]], writes=[b_KT[1]])
            w_ukv_b, b_wukv = load(es, "w_ukv_b", w_ukv[:, :], [128, 1024], BF16, cast=True)
            selm = SB(es, "selm", [128, 2, 128], F32); b_sel = Buf()
            op(dve, lambda: nc.vector.memset(selm[:], 0.0), writes=[b_sel])
            op(dve, lambda: nc.vector.memset(selm[64:65, 0, 0:64], 1.0), writes=[b_sel])
            op(dve, lambda: nc.vector.memset(selm[0:1, 1, 64:128], 1.0), writes=[b_sel])
            dmask_b, b_dmask = load(es, "dmask_b", dmask_d[:, :, :], [128, 4, 512], BF16, cast=True)
            abias, b_abias = load(es, "abias", abias_d[:, :], [128, 12])
            g_a, b_ga = load(es, "g_a", g_a_d[:, :], [128, 4])
            Vaug = [SB(es, "Vaug%d" % i, [128, NKB, 128], BF16) for i in range(2)]
            b_V = [Buf(), Buf()]
            for i in range(2):
                onec = 64 if i == 0 else 0
                op(pool, lambda: nc.gpsimd.memset(Vaug[i][:], 0.0), writes=[b_V[i]])
                op(pool, lambda: nc.gpsimd.memset(Vaug[i][:, :, onec:onec + 1], 1.0), writes=[b_V[i]])
                op(pool, lambda: nc.gpsimd.memset(Vaug[i][0:112, 0, onec:onec + 1], 0.0), writes=[b_V[i]])
            PT = [SB(es, "PT%d" % i, [128, 2, 512], BF16) for i in range(3)]; b_PT = [Buf(), Buf(), Buf()]
            OTsb = SB(es, "OTsb", [128, 512], F32); b_OTsb = Buf()
            rec = SB(es, "rec", [128, 512], F32); b_rec = Buf()
            ps_S = [PS(pes, "ps_S%d" % i, [128, 2, 512]) for i in range(2)]; b_S = [Buf(), Buf()]
            ps_O = [PS(pes, "ps_O%d" % i, [128, 512]) for i in range(2)]; b_O = [Buf(), Buf()]
            ps_bc = PS(pes, "ps_bc", [128, 512]); b_bc = Buf()
            ps_bld = PS(pes, "ps_bld", [128, 512]); b_bld = Buf()

            def build_steps(h):
                sl = h % 2
                vc = 0 if sl == 0 else 64
                out = []
                for kb0 in range(0, NKB, 8):
                    def stepv(kb0=kb0):
                        n = min(8, NKB - kb0)

                        def f():
                            ins = None
                            for r in range(n):
                                kb = kb0 + r
                                ins = nc.tensor.matmul(ps_bld[:, r * 64:(r + 1) * 64], lhsT=ckvnT[:, kb * 128:(kb + 1) * 128],
                                                       rhs=w_ukv_b[:, h * 128 + 64:h * 128 + 128], start=True, stop=True)
                            return ins
                        op(pe, f, reads=[b_wukv] + b_ckvn, writes=[b_bld])
                        op(dve, lambda: nc.vector.tensor_copy(out=Vaug[sl][:, kb0:kb0 + n, vc:vc + 64],
                                                              in_=ps_bld[:, 0:n * 64].rearrange("p (r v) -> p r v", v=64)),
                           reads=[b_bld], writes=[b_V[sl]])
                    out.append(stepv)
                for ci in range(NCH):
                    def stepk(ci=ci):
                        c0, W = chunk_cols[ci]
                        op(pe, lambda: nc.tensor.matmul(ps_bld[0:64, 0:W], lhsT=w_ukv_b[:, h * 128:h * 128 + 64], rhs=ckvnT[:, c0:c0 + W], start=True, stop=True),
                           reads=[b_wukv, b_ckvn[ci]], writes=[b_bld])
                        op(dve, lambda: nc.vector.tensor_copy(out=KT[sl][0:64, c0:c0 + W], in_=ps_bld[0:64, 0:W]), reads=[b_bld], writes=[b_KT[sl]])
                    out.append(stepk)
                return out

            def build_head(h):
                for st_ in build_steps(h):
                    st_()

            stgc = [SB(es, "stgc%d" % i, [128, 1024], F32) for i in range(3)]; b_stgc = [dbuf() for _ in range(3)]
            outc = [SB(es, "outc%d" % i, [128, 1024], BF16) for i in range(3)]; b_outc = [Buf() for _ in range(3)]
            b_outd = [dbuf() for _ in range(3)]
            pieces = []
            for e_ in range(1, NEXP + 1):
                rows_ = slice(e_ * 128, (e_ + 1) * 128)
                for pc_i in range(6):
                    srcd_ = (wgu_lo, wgu_hi, wd_h)[pc_i // 2]
                    col_ = (pc_i % 2) * 1024
                    if pc_i < 4:
                        dst_ = wbf_gu[rows_, pc_i * 1024:(pc_i + 1) * 1024]
                    else:
                        dst_ = wbf_d[rows_, (pc_i - 4) * 1024:(pc_i - 3) * 1024]
                    pieces.append((srcd_[rows_, col_:col_ + 1024], dst_))
            NPC = len(pieces)

            def pc_in(i):
                k3 = i % 3
                op(sp, lambda: nc.sync.dma_start(out=stgc[k3][:, :], in_=pieces[i][0]), writes=[b_stgc[k3]], dma=b_stgc[k3])

            def pc_step(i):
                k3 = i % 3
                if i + 2 < NPC:
                    pc_in(i + 2)
                op(dve, lambda: nc.vector.tensor_copy(out=outc[k3][:, :], in_=stgc[k3][:, :]), reads=[b_stgc[k3]], writes=[b_outc[k3]])
                op(sp, lambda: nc.sync.dma_start(out=pieces[i][1], in_=outc[k3][:, :]), reads=[b_outc[k3]], writes=[b_outd[k3]], dma=b_outd[k3])
            pc_in(0)
            pc_in(1)
            pc_next = [0]

            build_head(0)
            pairs = []
            for gi in range(4):
                seq = []
                base = 1 + 16 * gi
                for r in range(3):
                    for i2 in range(0, 4, 2):
                        seq.append((gi * 3 + r, [(base + 4 * r + i2 + u, None) for u in range(2)]))
                for i2 in range(0, 4, 2):
                    seq.append((None, [(base + 12 + i2 + u, i2 + u) for u in range(2)]))
                fb = list(range(1, base))
                for i2 in range(0, len(fb), 2):
                    seq.append((None, [(b_, None) for b_ in fb[i2:i2 + 2]]))
                seq.append((None, [(0, None)]))
                nst = sum(len(p[1]) for p in seq)
                cnt_ = 0
                for bcol, items in seq:
                    ent = []
                    for (blk, md) in items:
                        ent.append((gi, blk, md, cnt_ == 0, cnt_ == nst - 1))
                        cnt_ += 1
                    pairs.append((bcol, ent))
            npair = len(pairs)
            gp = [0]
            for h in range(8):
                sl = h % 2
                R = slice(0, 64) if sl == 0 else slice(64, 128)
                pending_build = build_steps(h + 1) if h + 1 < 8 else []

                def emit_S(m):
                    pp = (gp[0] + m) % 2
                    ent = pairs[m][1]

                    def f():
                        ins = None
                        for j, (gi, blk, md, fst, lst) in enumerate(ent):
                            ins = nc.tensor.matmul(ps_S[pp][:, j, :], lhsT=KT[sl][0:96, blk * 128:(blk + 1) * 128], rhs=QT[0:96, h, gi * 512:(gi + 1) * 512],
                                                   start=True, stop=True)
                        return ins
                    gis = sorted(set(e_[0] for e_ in ent))
                    op(pe, f, reads=[b_KT[sl]] + [b_QT[h][g_] for g_ in gis], writes=[b_S[pp]])

                emit_S(0)
                emit_S(1)
                for m in range(npair):
                    pp = (gp[0] + m) % 2
                    pt = (gp[0] + m) % 3
                    bcol, ent = pairs[m]
                    nj = len(ent)
                    if bcol is None:
                        op(act, lambda: nc.scalar.activation(out=PT[pt][:, 0:nj, :], in_=ps_S[pp][:, 0:nj, :], func=AF.Exp, scale=SCALE),
                           reads=[b_S[pp]], writes=[b_PT[pt]])
                    else:
                        op(act, lambda: nc.scalar.activation(out=PT[pt][:, 0:nj, :], in_=ps_S[pp][:, 0:nj, :], func=AF.Exp, scale=SCALE, bias=abias[:, bcol:bcol + 1]),
                           reads=[b_S[pp], b_abias], writes=[b_PT[pt]])
                    for j, (gi, blk, md, fst, lst) in enumerate(ent):
                        if md is not None:
                            op(dve, lambda: nc.vector.tensor_tensor(out=PT[pt][:, j, :], in0=PT[pt][:, j, :], in1=dmask_b[:, md, :], op=ALU.mult),
                               reads=[b_dmask, b_PT[pt]], writes=[b_PT[pt]])
                    if m + 2 < npair:
                        emit_S(m + 2)
                    if pending_build and m % 3 == 2:
                        pending_build.pop(0)()
                    if m % 3 == 0 and pc_next[0] < NPC:
                        pc_step(pc_next[0])
                        pc_next[0] += 1
                    for j, (gi, blk, md, fst, lst) in enumerate(ent):
                        ob = (h * 4 + gi) % 2
                        op(pe, lambda: nc.tensor.matmul(ps_O[ob][:, :], lhsT=Vaug[sl][:, blk, :], rhs=PT[pt][:, j, :], start=fst, stop=lst),
                           reads=[b_V[sl], b_PT[pt]], writes=[b_O[ob]])
                        if lst:
                            op(dve, lambda: nc.vector.tensor_copy(out=OTsb[:, :], in_=ps_O[ob][:, :]), reads=[b_O[ob]], writes=[b_OTsb])
                            op(pe, lambda: nc.tensor.matmul(ps_bc[:, :], lhsT=selm[:, sl, :], rhs=OTsb[:, :], start=True, stop=True),
                               reads=[b_sel, b_OTsb], writes=[b_bc])
                            op(dve, lambda: nc.vector.reciprocal(out=rec[R, :], in_=ps_bc[R, :]), reads=[b_bc], writes=[b_rec])
                            op(dve, lambda: nc.vector.tensor_tensor(out=attnT[R, h // 2, gi * 512:(gi + 1) * 512], in0=OTsb[R, :], in1=rec[R, :], op=ALU.mult),
                               reads=[b_OTsb, b_rec], writes=[b_attn[gi]])
                while pending_build:
                    pending_build.pop(0)()
                gp[0] += npair
            while pc_next[0] < NPC:
                pc_step(pc_next[0])
                pc_next[0] += 1
            for k3_ in range(3):
                _wait(sp, b_outd[k3_].w)
            if dbg:
                dbg_out["attnT"] = dout("dbg_attnT", [128, 4, 2048], BF16)
                dbg_out["ckvnT"] = dout("dbg_ckvnT", [128, P_TOT], BF16)
                dbg_out["KT0"] = dout("dbg_KT0", [96, P_TOT], BF16)
                dbg_out["KT1"] = dout("dbg_KT1", [96, P_TOT], BF16)
                bd = dbuf()
                op(sp, lambda: nc.sync.dma_start(out=dbg_out["attnT"][:, :, :], in_=attnT[:]), reads=b_attn, writes=[bd], dma=bd)
                op(sp, lambda: nc.sync.dma_start(out=dbg_out["ckvnT"][:, :], in_=ckvnT[:]), reads=b_ckvn, writes=[bd], dma=bd)
                op(sp, lambda: nc.sync.dma_start(out=dbg_out["KT0"][:, :], in_=KT0[:]), reads=[b_KT[0]], writes=[bd], dma=bd)
                op(sp, lambda: nc.sync.dma_start(out=dbg_out["KT1"][:, :], in_=KT1[:]), reads=[b_KT[1]], writes=[bd], dma=bd)
                _wait(sp, bd.w)
            asq = SB(es, "asq", [128, 4, 512], BF16); b_asq = Buf()
            atmp = SB(es, "atmp", [128, 512], F32); b_atmp = Buf()
            ra = SB(es, "ra", [128, 512], F32); b_ra = Buf()
            for gi in range(4):
                cols = slice(gi * 512, (gi + 1) * 512)
                op(act, lambda: nc.scalar.activation(out=asq[:, :, :], in_=attnT[:, :, cols], func=AF.Square), reads=[b_attn[gi]], writes=[b_asq])
                op(pe, mm_acc(ps_bc[:, :], lambda c: ones_bf[:, :], lambda c: asq[:, c, :], 4), reads=[b_ones, b_asq], writes=[b_bc])
                rstd_from_psum(ps_bc, b_bc, 128, 512, 1.0 / 512, atmp, b_atmp, ra, b_ra)
                for c in range(4):
                    op(dve, lambda: nc.vector.scalar_tensor_tensor(out=attnT[:, c, cols], in0=attnT[:, c, cols], scalar=g_a[:, c:c + 1], in1=ra[:, :],
                                                                   op0=ALU.mult, op1=ALU.mult),
                       reads=[b_attn[gi], b_ga, b_ra], writes=[b_attn[gi]])
            nc.all_engine_barrier()

        hn2T = at(RA[0], [128, 8, 2048], BF16)
        b_hn2 = [Buf() for _ in range(16)]
        comb = SB(cst, "comb", [128, 16, 33], F32)
        b_comb = Buf()
        b_yall = dbuf()
        b_hn2d = dbuf()
        b_mapinit = dbuf()
        b_scat = dbuf()
        BIG = 1.0e30
        NT = 64 + NOV
        mapt = SB(cst, "mapt", [128, NT, 4], F32); b_mapt = dbuf()
        gidx = SB(cst, "gidx", [128, NT], I32); b_gidx = Buf()
        sidx = SB(cst, "sidx", [128, NT], I32); b_sidx = Buf()
        ovidx = SB(cst, "ovidx", [128, NOV], I32); b_ovidx = Buf()
        with ExitStack() as pes:
            es = Bump(RF)
            esl = Bump(RB)
            w_oa_b, b_woa = load(es, "w_oa_b", w_out_a[:, :, :], [128, 4, D], BF16, cast=True)
            w_oc_b, b_woc = load(es, "w_oc_b", w_out_c[:, :, :], [128, 4, D], BF16, cast=True)
            g_pm, b_gpm = load(es, "g_pm_bc", g_pm_d[0:1, :].partition_broadcast(128), [128, D])
            g_pf, b_gpf = load(es, "g_pf", g_pf_d[:, :], [128, 8])
            wr_t, b_wr = load(es, "wr_t", wr_d[:, :, :], [128, 8, 36])
            br_t, b_br = load(es, "br_bc", br_d[0:1, :].partition_broadcast(128), [128, 36])
            ident, b_id = load(es, "ident", ident_d[:, :], [128, 128])
            op(dve, lambda: nc.vector.memset(comb[:, :, 0:1], 1.0), writes=[b_comb])

            def two(name, shape, dt, dma=False):
                return [SB(esl, "%s%d" % (name, i), shape, dt) for i in range(2)], [dbuf() if dma else Buf() for i in range(2)]
            xo = [SB(esl, "xo%d" % i, [128, D], F32) for i in range(3)]; b_xo = [dbuf() for _ in range(3)]

            def load_xo(tb_):
                op(sp, lambda: nc.sync.dma_start(out=xo[tb_ % 3][:, :], in_=xown[tb_]), writes=[b_xo[tb_ % 3]], dma=b_xo[tb_ % 3])
            load_xo(0)
            load_xo(1)
            hm, b_hm = two("hm", [128, D], F32)
            hh, b_hh = two("hh", [128, D], F32)
            hs, b_hs = two("hs", [128, D], F32)
            junk, b_junk = two("junk", [128, D], BF16)
            hsT, b_hsT = two("hsT", [128, 8, 128], F32)
            sm, b_sm = two("sm", [128, 8], F32)
            b_smc = [[Buf() for _ in range(8)] for _ in range(2)]
            lgall = SB(es, "lgall", [128, 16, 36], F32); b_lg = Buf()
            g_pfb, b_gpfb = load(es, "g_pfb", g_pfb_d[0:1, :].partition_broadcast(128), [128, D])
            hrow, b_hrow = two("hrow", [128, D], BF16)
            zrow = SB(es, "zrow", [1, D], BF16); b_zrow = Buf()
            op(dve, lambda: nc.vector.memset(zrow[:], 0.0), writes=[b_zrow])
            op(sp, lambda: nc.sync.dma_start(out=hn2d[2048:2049, :], in_=zrow[:, :]), reads=[b_zrow], dma=b_hn2d)
            op(sp, lambda: nc.sync.dma_start(out=maps_d[:, :], in_=mapinit_d[:, :]), dma=b_mapinit, writes=[b_mapinit])
            ps_mix = [PS(pes, "ps_mix%d" % i, [128, D]) for i in range(2)]; b_pmix = [Buf(), Buf()]
            ps_T = PS(pes, "ps_T", [128, 8, 128]); b_pT = Buf()
            ps_lg = [PS(pes, "ps_lg%d" % i, [128, 36]) for i in range(2)]; b_plg = [Buf(), Buf()]

            hh3 = [hh[0], hh[1], SB(esl, "hh2", [128, D], F32)]; b_hh3 = [b_hh[0], b_hh[1], Buf()]
            sm3 = [SB(esl, "sm3_%d" % i, [128, 8], F32) for i in range(3)]
            b_sm3 = [[Buf() for _ in range(8)] for _ in range(3)]

            def st1(tb):
                k, k3 = tb % 2, tb % 3
                tc0 = tb * 128
                bs = b_sm3[k3]

                def S(i):
                    return sm3[k3][:, i:i + 1]
                if tb + 2 < 16:
                    load_xo(tb + 2)

                def fmix():
                    ins = None
                    for half in range(2):
                        hc = slice(half * 512, (half + 1) * 512)
                        for h in range(4):
                            ins = nc.tensor.matmul(ps_mix[k][:, hc], lhsT=attnT[:, h, tc0:tc0 + 128], rhs=w_oa_b[:, h, hc], start=(h == 0), stop=False)
                        for cc in range(4):
                            ins = nc.tensor.matmul(ps_mix[k][:, hc], lhsT=convnT[:, cc, tc0:tc0 + 128], rhs=w_oc_b[:, cc, hc], start=False, stop=(cc == 3))
                    return ins
                op(pe, fmix, reads=[b_attn[tb // 4], b_convn[tb // 4], b_woa, b_woc], writes=[b_pmix[k]])
                op(act, lambda: nc.scalar.activation(out=junk[k][:, :], in_=ps_mix[k][:, :], func=AF.Square, accum_out=S(0)),
                   reads=[b_pmix[k]], writes=[b_junk[k], bs[0]])
                op(act, lambda: nc.scalar.activation(out=S(2), in_=S(0), func=AF.Sqrt, scale=1.0 / D, bias=eps_t[:, 0:1]), reads=[bs[0], b_eps], writes=[bs[2]])
                op(dve, lambda: nc.vector.reciprocal(out=S(2), in_=S(2)), reads=[bs[2]], writes=[bs[2]])
                op(dve, lambda: nc.vector.scalar_tensor_tensor(out=hm[k][:, :], in0=ps_mix[k][:, :], scalar=S(2), in1=g_pm[:, :], op0=ALU.mult, op1=ALU.mult),
                   reads=[b_pmix[k], bs[2], b_gpm], writes=[b_hm[k]])
                op(dve, lambda: nc.vector.tensor_tensor(out=hh3[k3][:, :], in0=hm[k][:, :], in1=xo[k3][:, :], op=ALU.add), reads=[b_hm[k], b_xo[k3]], writes=[b_hh3[k3]])
                op(sp, lambda: nc.sync.dma_start(out=y[tb], in_=hh3[k3][:, :]), reads=[b_hh3[k3]], writes=[b_yall], dma=b_yall)

            def st2(tb):
                k, k3 = tb % 2, tb % 3
                tc0 = tb * 128
                bs = b_sm3[k3]

                def S(i):
                    return sm3[k3][:, i:i + 1]
                op(act, lambda: nc.scalar.activation(out=junk[k][:, :], in_=hh3[k3][:, :], func=AF.Square, accum_out=S(1)),
                   reads=[b_hh3[k3]], writes=[b_junk[k], bs[1]])
                op(act, lambda: nc.scalar.activation(out=S(3), in_=S(1), func=AF.Sqrt, scale=1.0 / D, bias=eps_t[:, 0:1]), reads=[bs[1], b_eps], writes=[bs[3]])
                op(dve, lambda: nc.vector.reciprocal(out=S(3), in_=S(3)), reads=[bs[3]], writes=[bs[3]])
                op(act, lambda: nc.scalar.activation(out=hs[k][:, :], in_=hh3[k3][:, :], func=AF.Copy, scale=S(3)), reads=[b_hh3[k3], bs[3]], writes=[b_hs[k]])

                def ftr():
                    ins = None
                    for c in range(8):
                        ins = nc.tensor.transpose(ps_T[:, c, :], hs[k][:, c * 128:(c + 1) * 128], ident[:, :])
                    return ins
                op(pe, ftr, reads=[b_hs[k], b_id], writes=[b_pT])
                op(dve, lambda: nc.vector.tensor_tensor(out=hrow[k][:, :], in0=hs[k][:, :], in1=g_pfb[:, :], op=ALU.mult), reads=[b_hs[k], b_gpfb], writes=[b_hrow[k]])
                op(sp, lambda: nc.sync.dma_start(out=hn2d[tc0:tc0 + 128, :], in_=hrow[k][:, :]), reads=[b_hrow[k]], dma=b_hn2d)

            def st3(tb):
                k = tb % 2
                tc0 = tb * 128
                op(dve, lambda: nc.vector.tensor_tensor(out=hsT[k][:, :, :], in0=ps_T[:, :, :], in1=g_pf[:, :].unsqueeze(2).to_broadcast([128, 8, 128]), op=ALU.mult),
                   reads=[b_pT, b_gpf], writes=[b_hsT[k]])
                op(act, lambda: nc.scalar.activation(out=hn2T[:, :, tc0:tc0 + 128], in_=hsT[k][:, :, :], func=AF.Copy), reads=[b_hsT[k]], writes=[b_hn2[tb]])
                op(pe, mm_acc(ps_lg[k][:, :], lambda c: hsT[k][:, c, :], lambda c: wr_t[:, c, :], 8), reads=[b_hsT[k], b_wr], writes=[b_plg[k]])
                op(dve, lambda: nc.vector.tensor_tensor(out=lgall[:, tb, :], in0=ps_lg[k][:, :], in1=br_t[:, :], op=ALU.add), reads=[b_plg[k], b_br], writes=[b_lg])

            for it in range(16 + 2):
                if it < 16:
                    st1(it)
                if 0 <= it - 2 < 16:
                    st3(it - 2)
                if 0 <= it - 1 < 16:
                    st2(it - 1)

            _wait(sp, b_yall.w)
            _wait(sp, (b_hn2d.dsem, b_hn2d.dn))
            nc.all_engine_barrier()
            es = Bump(RB)

            def T(name, shape):
                return SB(es, name, shape, F32), Buf()
            gmax, b_gmax = T("gmax", [128, 16]); gm, b_gm = T("gm", [128, 16, 4]); gsh, b_gsh = T("gsh", [128, 16, 4])
            gsum, b_gsum = T("gsum", [128, 16]); gw, b_gw = T("gw", [128, 16]); pen, b_pen = T("pen", [128, 16, 4])
            elm, b_elm = T("elm", [128, 16, 32]); elm2, b_elm2 = T("elm2", [128, 16, 32])
            m1, b_m1 = T("m1", [128, 16]); m2, b_m2 = T("m2", [128, 16])
            oh1, b_oh1 = T("oh1", [128, 16, 32]); oh2, b_oh2 = T("oh2", [128, 16, 32])
            dd, b_dd = T("dd", [128, 16]); w1, b_w1 = T("w1", [128, 16]); w2, b_w2 = T("w2", [128, 16])
            lg_g = lgall[:, :, 0:4]
            lg_e = lgall[:, :, 4:36]

            def bc(t2d, n):
                return t2d[:, :].unsqueeze(2).to_broadcast([128, 16, n])
            op(dve, lambda: nc.vector.tensor_reduce(out=gmax[:, :], in_=lg_g, axis=AX.X, op=ALU.max), reads=[b_lg], writes=[b_gmax])
            op(dve, lambda: nc.vector.tensor_tensor(out=gm[:, :, :], in0=lg_g, in1=bc(gmax, 4), op=ALU.is_equal), reads=[b_lg, b_gmax], writes=[b_gm])
            op(dve, lambda: nc.vector.tensor_tensor(out=gsh[:, :, :], in0=lg_g, in1=bc(gmax, 4), op=ALU.subtract), reads=[b_lg, b_gmax], writes=[b_gsh])
            op(act, lambda: nc.scalar.activation(out=gsh[:, :, :], in_=gsh[:, :, :], func=AF.Exp), reads=[b_gsh], writes=[b_gsh])
            op(dve, lambda: nc.vector.tensor_reduce(out=gsum[:, :], in_=gsh[:, :, :], axis=AX.X, op=ALU.add), reads=[b_gsh], writes=[b_gsum])
            op(dve, lambda: nc.vector.reciprocal(out=gw[:, :], in_=gsum[:, :]), reads=[b_gsum], writes=[b_gw])
            op(dve, lambda: nc.vector.tensor_scalar(out=pen[:, :, :], in0=gm[:, :, :], scalar1=BIG, scalar2=-BIG, op0=ALU.mult, op1=ALU.add), reads=[b_gm], writes=[b_pen])
            op(dve, lambda: nc.vector.tensor_tensor(out=elm[:, :, :].rearrange("p b (g e) -> p b g e", g=4),
                                                    in0=lg_e.rearrange("p b (g e) -> p b g e", g=4),
                                                    in1=pen[:, :, :].unsqueeze(3).to_broadcast([128, 16, 4, 8]), op=ALU.add),
               reads=[b_lg, b_pen], writes=[b_elm])
            op(dve, lambda: nc.vector.tensor_reduce(out=m1[:, :], in_=elm[:, :, :], axis=AX.X, op=ALU.max), reads=[b_elm], writes=[b_m1])
            op(dve, lambda: nc.vector.tensor_tensor(out=oh1[:, :, :], in0=elm[:, :, :], in1=bc(m1, 32), op=ALU.is_equal), reads=[b_elm, b_m1], writes=[b_oh1])
            op(dve, lambda: nc.vector.scalar_tensor_tensor(out=elm2[:, :, :], in0=oh1[:, :, :], scalar=-BIG, in1=elm[:, :, :], op0=ALU.mult, op1=ALU.add),
               reads=[b_oh1, b_elm], writes=[b_elm2])
            op(dve, lambda: nc.vector.tensor_reduce(out=m2[:, :], in_=elm2[:, :, :], axis=AX.X, op=ALU.max), reads=[b_elm2], writes=[b_m2])
            op(dve, lambda: nc.vector.tensor_tensor(out=oh2[:, :, :], in0=elm2[:, :, :], in1=bc(m2, 32), op=ALU.is_equal), reads=[b_elm2, b_m2], writes=[b_oh2])
            op(dve, lambda: nc.vector.tensor_tensor(out=dd[:, :], in0=m2[:, :], in1=m1[:, :], op=ALU.subtract), reads=[b_m1, b_m2], writes=[b_dd])
            op(act, lambda: nc.scalar.activation(out=dd[:, :], in_=dd[:, :], func=AF.Exp), reads=[b_dd], writes=[b_dd])
            op(dve, lambda: nc.vector.tensor_scalar(out=w1[:, :], in0=dd[:, :], scalar1=1.0, scalar2=None, op0=ALU.add), reads=[b_dd], writes=[b_w1])
            op(dve, lambda: nc.vector.reciprocal(out=w1[:, :], in_=w1[:, :]), reads=[b_w1], writes=[b_w1])
            op(dve, lambda: nc.vector.tensor_tensor(out=w2[:, :], in0=dd[:, :], in1=w1[:, :], op=ALU.mult), reads=[b_dd, b_w1], writes=[b_w2])
            op(dve, lambda: nc.vector.tensor_tensor(out=w1[:, :], in0=w1[:, :], in1=gw[:, :], op=ALU.mult), reads=[b_w1, b_gw], writes=[b_w1])
            op(dve, lambda: nc.vector.tensor_tensor(out=w2[:, :], in0=w2[:, :], in1=gw[:, :], op=ALU.mult), reads=[b_w2, b_gw], writes=[b_w2])
            ps_pp = ps_mix[0][:, :].rearrange("p (a n) -> p a n", a=2); b_pp = b_pmix[0]
            ps_pt = ps_mix[1][:, :].rearrange("p (a n) -> p a n", a=2); b_pt = b_pmix[1]
            triu_b, b_triu = load(es, "triu_b", triu_d[:, :], [128, 128], BF16, cast=True)
            vb, b_vb = load(es, "vb", vb_d[0:1, :].partition_broadcast(128), [128, 64])
            tokid, b_tok = load(es, "tokid", tokid_d[:, :], [128, 16])
            pidx, b_pidx = load(es, "pidx", pidx_d[:, :], [128, 1])
            jidx, b_jidx = load(es, "jidx", jidx_d[0:1, :].partition_broadcast(128), [128, NOV])
            ohv_b = SB(es, "ohv_b", [128, 16, 64], BF16); b_ohvb = Buf()
            ohv, b_ohv = T("ohv", [128, 16, 64])
            tot_s, b_tot = T("tot_s", [128, 16, 64]); boff, b_boff = T("boff", [128, 16, 64])
            rank, b_rank = T("rank", [128, 16, 64]); wk, b_wk = T("wk", [128, 16, 64])
            cnt, b_cnt = T("cnt", [128, 64]); ovt, b_ovt = T("ovt", [128, 64]); ovend, b_ovend = T("ovend", [128, 64])
            ovst, b_ovst = T("ovst", [128, 64]); one64, b_one64 = T("one64", [128, 64])
            rr, b_rr = T("rr", [128, 16, 2]); basev, b_basev = T("basev", [128, 16, 2]); ovs, b_ovs = T("ovs", [128, 16, 2])
            isov, b_isov = T("isov", [128, 16, 2]); posf, b_posf = T("posf", [128, 16, 2])
            pos_i = SB(cst, "pos_i", [128, 16, 2], I32); b_posi = Buf()
            srcm = SB(cst, "srcm", [128, 32, 4], F32); b_srcm = Buf()
            cmpj, b_cmpj = T("cmpj", [128, NOV, 64]); ovv, b_ovv = T("ovv", [128, NOV]); ovf, b_ovf = T("ovf", [128, NOV])
            op(dve, lambda: nc.vector.tensor_copy(out=ohv[:, :, 0:32], in_=oh1[:, :, :]), reads=[b_oh1], writes=[b_ohv])
            op(dve, lambda: nc.vector.tensor_copy(out=ohv[:, :, 32:64], in_=oh2[:, :, :]), reads=[b_oh2], writes=[b_ohv])
            op(dve, lambda: nc.vector.tensor_copy(out=ohv_b[:, :, :], in_=ohv[:, :, :]), reads=[b_ohv], writes=[b_ohvb])
            ohv2d = ohv_b.rearrange("p b v -> p (b v)")
            for hb in range(2):
                op(pe, lambda: nc.tensor.matmul(ps_pp[:, hb, :], lhsT=triu_b[:, :], rhs=ohv2d[:, hb * 512:(hb + 1) * 512], start=True, stop=True),
                   reads=[b_triu, b_ohvb], writes=[b_pp])
                op(pe, lambda: nc.tensor.matmul(ps_pt[:, hb, :], lhsT=ones_bf[:, :], rhs=ohv2d[:, hb * 512:(hb + 1) * 512], start=True, stop=True),
                   reads=[b_ones, b_ohvb], writes=[b_pt])
            op(act, lambda: nc.scalar.activation(out=tot_s.rearrange("p b v -> p (b v)"), in_=ps_pt.rearrange("p a n -> p (a n)"), func=AF.Copy),
               reads=[b_pt], writes=[b_tot])
            op(dve, lambda: nc.vector.memset(boff[:, 0, :], 0.0), writes=[b_boff])
            for b_ in range(1, 16):
                op(dve, lambda: nc.vector.tensor_tensor(out=boff[:, b_, :], in0=boff[:, b_ - 1, :], in1=tot_s[:, b_ - 1, :], op=ALU.add),
                   reads=[b_boff, b_tot], writes=[b_boff])
            op(dve, lambda: nc.vector.tensor_tensor(out=cnt[:, :], in0=boff[:, 15, :], in1=tot_s[:, 15, :], op=ALU.add), reads=[b_boff, b_tot], writes=[b_cnt])
            op(dve, lambda: nc.vector.tensor_tensor(out=rank.rearrange("p b v -> p (b v)"), in0=ps_pp.rearrange("p a n -> p (a n)"),
                                                    in1=boff.rearrange("p b v -> p (b v)"), op=ALU.add), reads=[b_pp, b_boff], writes=[b_rank])
            op(dve, lambda: nc.vector.tensor_tensor(out=wk[:, :, :], in0=ohv[:, :, :], in1=rank[:, :, :], op=ALU.mult), reads=[b_ohv, b_rank], writes=[b_wk])
            op(dve, lambda: nc.vector.tensor_reduce(out=rr[:, :, :], in_=wk.rearrange("p b (k e) -> p b k e", k=2), axis=AX.X, op=ALU.add), reads=[b_wk], writes=[b_rr])
            op(dve, lambda: nc.vector.tensor_scalar(out=rr[:, :, :], in0=rr[:, :, :], scalar1=-1.0, scalar2=None, op0=ALU.add), reads=[b_rr], writes=[b_rr])
            op(dve, lambda: nc.vector.tensor_scalar(out=ovt[:, :], in0=cnt[:, :], scalar1=-float(CAP), scalar2=0.0, op0=ALU.add, op1=ALU.max), reads=[b_cnt], writes=[b_ovt])
            op(dve, lambda: nc.vector.tensor_scalar(out=ovt[:, :], in0=ovt[:, :], scalar1=127.0, scalar2=1.0 / 128, op0=ALU.add, op1=ALU.mult), reads=[b_ovt], writes=[b_ovt])
            op(dve, lambda: nc.vector.tensor_scalar(out=ovt[:, :], in0=ovt[:, :], scalar1=-0.5 + 1.0 / 256, scalar2=MAGIC, op0=ALU.add, op1=ALU.add), reads=[b_ovt], writes=[b_ovt])
            op(dve, lambda: nc.vector.tensor_scalar(out=ovt[:, :], in0=ovt[:, :], scalar1=-MAGIC, scalar2=None, op0=ALU.add), reads=[b_ovt], writes=[b_ovt])
            op(dve, lambda: nc.vector.memset(one64[:, :], 1.0), writes=[b_one64])
            op(dve, lambda: nc.vector.tensor_tensor_scan(out=ovend[:, :], data0=one64[:, :], data1=ovt[:, :], initial=0.0, op0=ALU.mult, op1=ALU.add),
               reads=[b_one64, b_ovt], writes=[b_ovend])
            op(dve, lambda: nc.vector.tensor_tensor(out=ovst[:, :], in0=ovend[:, :], in1=ovt[:, :], op=ALU.subtract), reads=[b_ovend, b_ovt], writes=[b_ovst])
            op(dve, lambda: nc.vector.tensor_scalar(out=ovst[:, :], in0=ovst[:, :], scalar1=128.0, scalar2=None, op0=ALU.mult), reads=[b_ovst], writes=[b_ovst])

            def bcv(t2d):
                return t2d[:, :].unsqueeze(1).to_broadcast([128, 16, 64])
            op(dve, lambda: nc.vector.tensor_tensor(out=wk[:, :, :], in0=ohv[:, :, :], in1=bcv(vb), op=ALU.mult), reads=[b_ohv, b_vb, b_wk], writes=[b_wk])
            op(dve, lambda: nc.vector.tensor_reduce(out=basev[:, :, :], in_=wk.rearrange("p b (k e) -> p b k e", k=2), axis=AX.X, op=ALU.add), reads=[b_wk], writes=[b_basev])
            op(dve, lambda: nc.vector.tensor_tensor(out=wk[:, :, :], in0=ohv[:, :, :], in1=bcv(ovst), op=ALU.mult), reads=[b_ohv, b_ovst, b_wk], writes=[b_wk])
            op(dve, lambda: nc.vector.tensor_reduce(out=ovs[:, :, :], in_=wk.rearrange("p b (k e) -> p b k e", k=2), axis=AX.X, op=ALU.add), reads=[b_wk], writes=[b_ovs])
            op(dve, lambda: nc.vector.tensor_scalar(out=isov[:, :, :], in0=rr[:, :, :], scalar1=float(CAP), scalar2=None, op0=ALU.is_ge), reads=[b_rr], writes=[b_isov])
            op(dve, lambda: nc.vector.tensor_tensor(out=ovs[:, :, :], in0=ovs[:, :, :], in1=basev[:, :, :], op=ALU.subtract), reads=[b_ovs, b_basev], writes=[b_ovs])
            op(dve, lambda: nc.vector.scalar_tensor_tensor(out=ovs[:, :, :], in0=ovs[:, :, :], scalar=float(8192 - CAP), in1=isov[:, :, :], op0=ALU.add, op1=ALU.mult),
               reads=[b_ovs, b_isov], writes=[b_ovs])
            op(dve, lambda: nc.vector.tensor_tensor(out=posf[:, :, :], in0=basev[:, :, :], in1=rr[:, :, :], op=ALU.add), reads=[b_basev, b_rr], writes=[b_posf])
            op(dve, lambda: nc.vector.tensor_tensor(out=posf[:, :, :], in0=posf[:, :, :], in1=ovs[:, :, :], op=ALU.add), reads=[b_posf, b_ovs], writes=[b_posf])
            op(dve, lambda: nc.vector.tensor_copy(out=pos_i[:, :, :], in_=posf[:, :, :]), reads=[b_posf], writes=[b_posi])
            srcv = srcm.rearrange("p (b k) c -> p b k c", k=2)
            op(dve, lambda: nc.vector.memset(srcm[:, :, :], 0.0), writes=[b_srcm])
            for kk in range(2):
                op(dve, lambda: nc.vector.tensor_copy(out=srcv[:, :, kk, 0], in_=tokid[:, :]), reads=[b_tok], writes=[b_srcm])
                op(dve, lambda: nc.vector.tensor_scalar(out=srcv[:, :, kk, 1], in0=tokid[:, :], scalar1=float(2048 * kk), scalar2=None, op0=ALU.add), reads=[b_tok], writes=[b_srcm])
                op(dve, lambda: nc.vector.tensor_copy(out=srcv[:, :, kk, 2], in_=(w1 if kk == 0 else w2)[:, :]), reads=[b_w1, b_w2], writes=[b_srcm])
            for tb in range(16):
                for kk in range(2):
                    op(pool, lambda: nc.gpsimd.indirect_dma_start(out=maps_d[:, :], out_offset=bass.IndirectOffsetOnAxis(ap=pos_i[:, tb, kk:kk + 1], axis=0),
                                                                  in_=srcv[:, tb, kk, :], in_offset=None, bounds_check=BND[NSLOT - 1], oob_is_err=False),
                       reads=[b_posi, b_srcm, b_mapinit], dma=b_scat)
            b_scat.w = (b_scat.dsem, b_scat.dn)
            op(dve, lambda: nc.vector.tensor_tensor(out=cmpj[:, :, :], in0=ovend[:, :].unsqueeze(1).to_broadcast([128, NOV, 64]),
                                                    in1=jidx[:, :].unsqueeze(2).to_broadcast([128, NOV, 64]), op=ALU.is_le), reads=[b_ovend, b_jidx], writes=[b_cmpj])
            op(dve, lambda: nc.vector.tensor_reduce(out=ovv[:, :], in_=cmpj[:, :, :], axis=AX.X, op=ALU.add), reads=[b_cmpj], writes=[b_ovv])
            op(dve, lambda: nc.vector.tensor_scalar(out=ovf[:, :], in0=ovv[:, :], scalar1=32.0, scalar2=-32.0, op0=ALU.is_ge, op1=ALU.mult), reads=[b_ovv], writes=[b_ovf])
            op(dve, lambda: nc.vector.tensor_tensor(out=ovf[:, :], in0=ovf[:, :], in1=ovv[:, :], op=ALU.add), reads=[b_ovf, b_ovv], writes=[b_ovf])
            op(dve, lambda: nc.vector.tensor_scalar(out=ovf[:, :], in0=ovf[:, :], scalar1=1.0, scalar2=128.0, op0=ALU.add, op1=ALU.mult), reads=[b_ovf], writes=[b_ovf])
            op(dve, lambda: nc.vector.tensor_scalar(out=ovf[:, :], in0=ovf[:, :], scalar1=pidx[:, 0:1], scalar2=None, op0=ALU.add), reads=[b_ovf, b_pidx], writes=[b_ovf])
            op(dve, lambda: nc.vector.tensor_scalar(out=ovv[:, :], in0=ovv[:, :], scalar1=64.0, scalar2=1.0e6, op0=ALU.is_ge, op1=ALU.mult), reads=[b_ovv], writes=[b_ovv])
            op(dve, lambda: nc.vector.tensor_tensor(out=ovf[:, :], in0=ovf[:, :], in1=ovv[:, :], op=ALU.add), reads=[b_ovf, b_ovv], writes=[b_ovf])
            op(dve, lambda: nc.vector.tensor_copy(out=ovidx[:, :], in_=ovf[:, :]), reads=[b_ovf], writes=[b_ovidx])
            op(dve, lambda: nc.vector.tensor_tensor(out=oh1[:, :, :], in0=oh1[:, :, :], in1=bc(w1, 32), op=ALU.mult), reads=[b_oh1, b_w1], writes=[b_oh1])
            op(dve, lambda: nc.vector.tensor_tensor(out=oh2[:, :, :], in0=oh2[:, :, :], in1=bc(w2, 32), op=ALU.mult), reads=[b_oh2, b_w2], writes=[b_oh2])
            op(dve, lambda: nc.vector.tensor_tensor(out=comb[:, :, 1:33], in0=oh1[:, :, :], in1=oh2[:, :, :], op=ALU.add), reads=[b_oh1, b_oh2, b_comb], writes=[b_comb])
            if dbg:
                dbg_out["comb"] = dout("dbg_comb", [128, 16, 33])
                bd = dbuf()
                op(sp, lambda: nc.sync.dma_start(out=dbg_out["comb"][:, :, :], in_=comb[:, :, :]), reads=[b_comb], writes=[bd], dma=bd)
                _wait(sp, bd.w)
            _wait(sp, b_yall.w)
            nc.all_engine_barrier()

        acc = at(RB[0], [128, 16, D], F32)
        b_acc = [Buf() for _ in range(16)]
        b_Ysc = dbuf()
        es = Bump(RF, RC, RD)
        NS = 4
        ra_b = Bump(RA)
        wgu = [SB(es if i < 2 else ra_b, "wgu%d" % i, [128, 8, 512], BF16) for i in range(NS)]
        wd = [SB(es if i < 2 else ra_b, "wd%d" % i, [128, 2, D], BF16) for i in range(NS)]
        b_wg = [dbuf() for _ in range(NS)]
        b_wg2 = [dbuf() for _ in range(NS)]
        b_wdn = [dbuf() for _ in range(NS)]

        def load_gu(e):
            s_ = e % NS
            rows = slice(e * 128, (e + 1) * 128)
            op(pool, lambda: nc.gpsimd.dma_start(out=wgu[s_][:, 0:4, :].rearrange("p c f -> p (c f)"), in_=wgu_lo[rows, :]), writes=[b_wg[s_]], dma=b_wg[s_])
            op(pool, lambda: nc.gpsimd.dma_start(out=wgu[s_][:, 4:8, :].rearrange("p c f -> p (c f)"), in_=wgu_hi[rows, :]), writes=[b_wg2[s_]], dma=b_wg2[s_])

        def load_d(e):
            s_ = e % NS
            rows = slice(e * 128, (e + 1) * 128)
            op(pool, lambda: nc.gpsimd.dma_start(out=wd[s_][:, :, :].rearrange("p j d -> p (j d)"), in_=wd_h[rows, :]), writes=[b_wdn[s_]], dma=b_wdn[s_])

        load_gu(0)
        load_d(0)
        with ExitStack() as pes:
            sg = [SB(es, "sg%d" % i, [128, 512], BF16) for i in range(2)]; b_sg = [Buf(), Buf()]
            hid = [SB(es, "hid%d" % i, [128, 2, 512], BF16) for i in range(2)]; b_hid = [Buf(), Buf()]
            ps_g = [PS(pes, "ps_g%d" % i, [128, 512]) for i in range(2)]; b_psg = [Buf(), Buf()]
            ps_u = [PS(pes, "ps_u%d" % i, [128, 512]) for i in range(2)]; b_psu = [Buf(), Buf()]
            ps_d = [PS(pes, "ps_d%d" % i, [128, D]) for i in range(2)]; b_psd = [Buf(), Buf()]

            def GU(t, hsl):
                tcs = slice(t * 512, (t + 1) * 512)
                rd = [b_wg[0], b_wg2[0]] + b_hn2[t * 4:(t + 1) * 4]
                for j in range(2):
                    op(pe, mm_acc(ps_g[j][:, :], lambda c: wgu[0][:, c, j * 128:(j + 1) * 128], lambda c: hn2T[:, c, tcs], 8), reads=rd, writes=[b_psg[j]])
                for j in range(2):
                    op(pe, mm_acc(ps_u[j][:, :], lambda c: wgu[0][:, c, 256 + j * 128:256 + (j + 1) * 128], lambda c: hn2T[:, c, tcs], 8), reads=rd, writes=[b_psu[j]])
                for j in range(2):
                    op(act, lambda: nc.scalar.activation(out=sg[j][:, :], in_=ps_g[j][:, :], func=AF.Silu), reads=[b_psg[j]], writes=[b_sg[j]])
                for j in range(2):
                    op(dve, lambda: nc.vector.tensor_tensor(out=hid[hsl][:, j, :], in0=ps_u[j][:, :], in1=sg[j][:, :], op=ALU.mult),
                       reads=[b_psu[j], b_sg[j]], writes=[b_hid[hsl]])

            def DOWN(t, hsl):
                for r in range(4):
                    tb = t * 4 + r
                    db = tb % 2

                    def f():
                        ins = None
                        for half in range(2):
                            hc = slice(half * 512, (half + 1) * 512)
                            for j in range(2):
                                ins = nc.tensor.matmul(ps_d[db][:, hc], lhsT=hid[hsl][:, j, r * 128:(r + 1) * 128], rhs=wd[0][:, j, hc], start=(j == 0), stop=(j == 1))
                        return ins
                    op(pe, f, reads=[b_hid[hsl], b_wdn[0]], writes=[b_psd[db]])
                    op(act, lambda: nc.scalar.activation(out=acc[:, tb, :], in_=ps_d[db][:, :], func=AF.Copy), reads=[b_psd[db]], writes=[b_acc[tb]])

            for t in range(4):
                GU(t, t % 2)
                if t > 0:
                    DOWN(t - 1, (t - 1) % 2)
            DOWN(3, 1)
            _wait(sp, b_scat.w)
            op(sp, lambda: nc.sync.dma_start(out=mapt[:, :, :], in_=maps_d.rearrange("(s p) c -> p s c", p=128)), reads=[b_scat], writes=[b_mapt], dma=b_mapt)
            op(dve, lambda: nc.vector.tensor_copy(out=gidx[:, :], in_=mapt[:, :, 0]), reads=[b_mapt], writes=[b_gidx])
            op(dve, lambda: nc.vector.tensor_copy(out=sidx[:, :], in_=mapt[:, :, 1]), reads=[b_mapt], writes=[b_sidx])
            nc.all_engine_barrier()

        with ExitStack() as pes:
            ident_b, b_idb = load(es, "ident_b", ident_d[:, :], [128, 128], BF16, cast=True)
            NOVS = 3
            wgu_ov = [SB(es, "wgu_ov%d" % i, [128, 8, 512], BF16) for i in range(NOVS)]
            wd_ov = [SB(es, "wd_ov%d" % i, [128, 2, D], BF16) for i in range(NOVS)]
            b_wgov = [dbuf() for _ in range(NOVS)]
            b_wgov2 = [dbuf() for _ in range(NOVS)]
            b_wdov = [dbuf() for _ in range(NOVS)]
            for i in range(NOVS):
                op(dve, lambda: nc.vector.memset(wgu_ov[i][:], 0.0), writes=[b_wgov[i], b_wgov2[i]])
                op(dve, lambda: nc.vector.memset(wd_ov[i][:], 0.0), writes=[b_wdov[i]])
            NXG = 4
            xg = [SB(es, "xg%d" % i, [128, D], BF16) for i in range(NXG)]; b_xg = [dbuf() for _ in range(NXG)]
            for i in range(NXG):
                op(dve, lambda: nc.vector.memset(xg[i][:], 0.0), writes=[b_xg[i]])
            xT = [SB(es, "xT%d" % i, [128, 8, 128], BF16) for i in range(3)]; b_xT = [Buf() for _ in range(3)]
            sgt = [SB(es, "sgt%d" % i, [128, 256], BF16) for i in range(2)]; b_sgt = [Buf(), Buf()]
            hidt = [SB(es, "hidt%d" % i, [128, 256], BF16) for i in range(3)]; b_hidt = [Buf() for _ in range(3)]
            hT = [SB(es, "hT%d" % i, [128, 2, 128], BF16) for i in range(3)]; b_hT = [Buf() for _ in range(3)]
            yo = [SB(es, "yo%d" % i, [128, D], F32) for i in range(2)]; b_yo = [Buf(), Buf()]
            ps_xT = [PS(pes, "ps_xT%d" % i, [128, 8, 128], BF16) for i in range(2)]; b_pxT = [Buf(), Buf()]
            ps_gu = [PS(pes, "ps_gu%d" % i, [128, 512]) for i in range(2)]; b_pgu = [Buf(), Buf()]
            ps_hT = PS(pes, "ps_hT", [128, 2, 128], BF16); b_phT = Buf()
            ps_dn = PS(pes, "ps_dn", [128, D]); b_pdn = Buf()

            tiles = [(k_ * 32 + e_, e_ + 1, None) for e_ in range(NEXP) for k_ in range(2)] + [(64 + j_, None, j_) for j_ in range(NOV)]
            NTL = len(tiles)
            def load_gu_fast(e):
                s_ = e % NS
                rows = slice(e * 128, (e + 1) * 128)
                op(sp, lambda: nc.sync.dma_start(out=wgu[s_][:, 0:4, :].rearrange("p c f -> p (c f)"), in_=wbf_gu[rows, 0:2048]), reads=b_outd, writes=[b_wg[s_]], dma=b_wg[s_])
                op(sp, lambda: nc.sync.dma_start(out=wgu[s_][:, 4:8, :].rearrange("p c f -> p (c f)"), in_=wbf_gu[rows, 2048:4096]), reads=b_outd, writes=[b_wg2[s_]], dma=b_wg2[s_])

            def load_d_fast(e):
                s_ = e % NS
                rows = slice(e * 128, (e + 1) * 128)
                op(sp, lambda: nc.sync.dma_start(out=wd[s_][:, :, :].rearrange("p j d -> p (j d)"), in_=wbf_d[rows, :]), reads=b_outd, writes=[b_wdn[s_]], dma=b_wdn[s_])

            def wgu_of(ti):
                scol, e_st, j_ov = tiles[ti]
                if e_st is not None:
                    return wgu[e_st % NS], [b_wg[e_st % NS], b_wg2[e_st % NS]]
                return wgu_ov[j_ov % NOVS], [b_wgov[j_ov % NOVS], b_wgov2[j_ov % NOVS]]

            def wd_of(ti):
                scol, e_st, j_ov = tiles[ti]
                if e_st is not None:
                    return wd[e_st % NS], b_wdn[e_st % NS]
                return wd_ov[j_ov % NOVS], b_wdov[j_ov % NOVS]

            def stage_G(ti):
                scol, e_st, j_ov = tiles[ti]
                if e_st is not None:
                    if scol < 32:
                        load_gu_fast(e_st)
                else:
                    W_gu, bW = wgu_of(ti)
                    ioa = bass.IndirectOffsetOnAxis(ap=ovidx[:, j_ov:j_ov + 1], axis=0)
                    op(pool, lambda: nc.gpsimd.indirect_dma_start(out=W_gu[:, :, :].rearrange("p c f -> p (c f)"), out_offset=None, in_=wbf_gu[:, :], in_offset=ioa,
                                                                  bounds_check=BND[NROW - 1], oob_is_err=False),
                       reads=[b_ovidx] + b_outd, writes=[bW[0], bW[1]], dma=bW[0])
                g4 = ti % NXG
                op(pool, lambda: nc.gpsimd.indirect_dma_start(out=xg[g4][:, :], out_offset=None, in_=hn2d[:, :],
                                                              in_offset=bass.IndirectOffsetOnAxis(ap=gidx[:, scol:scol + 1], axis=0), bounds_check=BND[2048], oob_is_err=False),
                   reads=[b_gidx], writes=[b_xg[g4]], dma=b_xg[g4])

            def stage_G2(ti):
                scol, e_st, j_ov = tiles[ti]
                if e_st is not None:
                    if scol < 32:
                        load_d_fast(e_st)
                else:
                    W_d, bW = wd_of(ti)
                    ioa = bass.IndirectOffsetOnAxis(ap=ovidx[:, j_ov:j_ov + 1], axis=0)
                    op(pool, lambda: nc.gpsimd.indirect_dma_start(out=W_d[:, :, :].rearrange("p j d -> p (j d)"), out_offset=None, in_=wbf_d[:, :], in_offset=ioa,
                                                                  bounds_check=BND[NROW - 1], oob_is_err=False),
                       reads=[b_ovidx], writes=[bW], dma=bW)

            def stage_A(ti):
                g4, p2, x3 = ti % NXG, ti % 2, ti % 3

                def ftr():
                    ins = None
                    for c in range(8):
                        ins = nc.tensor.transpose(ps_xT[p2][:, c, :], xg[g4][:, c * 128:(c + 1) * 128], ident_b[:, :])
                    return ins
                op(pe, ftr, reads=[b_xg[g4], b_idb], writes=[b_pxT[p2]])
                op(act, lambda: nc.scalar.activation(out=xT[x3][:, :, :], in_=ps_xT[p2][:, :, :], func=AF.Copy), reads=[b_pxT[p2]], writes=[b_xT[x3]])

            def stage_B(ti):
                p2, x3 = ti % 2, ti % 3
                W_gu, bW = wgu_of(ti)
                op(pe, mm_acc(ps_gu[p2][:, :], lambda c: xT[x3][:, c, :], lambda c: W_gu[:, c, :], 8), reads=[b_xT[x3]] + bW, writes=[b_pgu[p2]])
                op(act, lambda: nc.scalar.activation(out=sgt[p2][:, :], in_=ps_gu[p2][:, 0:256], func=AF.Silu), reads=[b_pgu[p2]], writes=[b_sgt[p2]])
                op(dve, lambda: nc.vector.tensor_tensor(out=hidt[x3][:, :], in0=ps_gu[p2][:, 256:512], in1=sgt[p2][:, :], op=ALU.mult),
                   reads=[b_pgu[p2], b_sgt[p2]], writes=[b_hidt[x3]])

            def stage_C(ti):
                x3 = ti % 3

                def ftr2():
                    ins = None
                    for j in range(2):
                        ins = nc.tensor.transpose(ps_hT[:, j, :], hidt[x3][:, j * 128:(j + 1) * 128], ident_b[:, :])
                    return ins
                op(pe, ftr2, reads=[b_hidt[x3], b_idb], writes=[b_phT])
                op(dve, lambda: nc.vector.tensor_copy(out=hT[x3][:, :, :], in_=ps_hT[:, :, :]), reads=[b_phT], writes=[b_hT[x3]])

            def stage_D(ti):
                scol = tiles[ti][0]
                p2, x3 = ti % 2, ti % 3
                W_d, bW = wd_of(ti)

                def fdn():
                    ins = None
                    for half in range(2):
                        hc = slice(half * 512, (half + 1) * 512)
                        for j in range(2):
                            ins = nc.tensor.matmul(ps_dn[:, hc], lhsT=hT[x3][:, j, :], rhs=W_d[:, j, hc], start=(j == 0), stop=(j == 1))
                    return ins
                op(pe, fdn, reads=[b_hT[x3], bW], writes=[b_pdn])
                op(act, lambda: nc.scalar.activation(out=yo[p2][:, :], in_=ps_dn[:, :], func=AF.Copy, scale=mapt[:, scol, 2:3]),
                   reads=[b_pdn, b_mapt], writes=[b_yo[p2]])
                op(pool, lambda: nc.gpsimd.indirect_dma_start(out=Yd[:, :], out_offset=bass.IndirectOffsetOnAxis(ap=sidx[:, scol:scol + 1], axis=0),
                                                              in_=yo[p2][:, :], in_offset=None, bounds_check=BND[4095], oob_is_err=False),
                   reads=[b_yo[p2], b_sidx], dma=b_Ysc)

            stage_G(0)
            stage_G(1)
            for it in range(NTL + 3):
                if it < NTL:
                    stage_A(it)
                if 0 <= it - 1 < NTL:
                    stage_B(it - 1)
                if 0 <= it - 2 < NTL:
                    stage_C(it - 2)
                if 0 <= it - 3 < NTL:
                    stage_D(it - 3)
                if it + 2 < NTL:
                    stage_G(it + 2)
                if it < NTL:
                    stage_G2(it)
            b_Ysc.w = (b_Ysc.dsem, b_Ysc.dn)
            _wait(pool, b_Ysc.w)
            nc.all_engine_barrier()

        b_yout = dbuf()
        with ExitStack() as pes:
            es = Bump(RF, RC, RD)
            g_po, b_gpo = load(es, "g_po_bc", g_po_d[0:1, :].partition_broadcast(128), [128, D])
            hf = [SB(es, "hf%d" % i, [128, D], F32) for i in range(3)]; b_hf = [dbuf() for _ in range(3)]
            y0 = [SB(es, "y0%d" % i, [128, D], F32) for i in range(3)]; b_y0 = [dbuf() for _ in range(3)]
            y1 = [SB(es, "y1%d" % i, [128, D], F32) for i in range(3)]; b_y1 = [dbuf() for _ in range(3)]

            def load_fin(tb_):
                k3 = tb_ % 3
                op(sp, lambda: nc.sync.dma_start(out=hf[k3][:, :], in_=y[tb_]), reads=[b_yall], writes=[b_hf[k3]], dma=b_hf[k3])
                op(sp, lambda: nc.sync.dma_start(out=y0[k3][:, :], in_=Yd[tb_ * 128:(tb_ + 1) * 128, :]), reads=[b_Ysc], writes=[b_y0[k3]], dma=b_y0[k3])
                op(sp, lambda: nc.sync.dma_start(out=y1[k3][:, :], in_=Yd[2048 + tb_ * 128:2048 + (tb_ + 1) * 128, :]), reads=[b_Ysc], writes=[b_y1[k3]], dma=b_y1[k3])
            load_fin(0)
            load_fin(1)
            oo = [SB(es, "oo%d" % i, [128, D], F32) for i in range(2)]; b_oo = [Buf(), Buf()]
            junk = SB(es, "f_junk", [128, D], BF16); b_junk = Buf()
            sm = SB(es, "f_sm", [128, 4], F32); b_s0 = Buf(); b_s1 = Buf()
            for tb in range(16):
                k = tb % 2
                k3 = tb % 3
                if tb + 2 < 16:
                    load_fin(tb + 2)
                op(dve, lambda: nc.vector.tensor_tensor(out=y0[k3][:, :], in0=y0[k3][:, :], in1=y1[k3][:, :], op=ALU.add), reads=[b_y0[k3], b_y1[k3]], writes=[b_y0[k3]])
                op(dve, lambda: nc.vector.tensor_tensor(out=acc[:, tb, :], in0=acc[:, tb, :], in1=y0[k3][:, :], op=ALU.add), reads=[b_acc[tb], b_y0[k3]], writes=[b_acc[tb]])
                op(act, lambda: nc.scalar.activation(out=junk[:, :], in_=acc[:, tb, :], func=AF.Square, accum_out=sm[:, 0:1]),
                   reads=[b_acc[tb]], writes=[b_junk, b_s0])
                op(act, lambda: nc.scalar.activation(out=sm[:, 1:2], in_=sm[:, 0:1], func=AF.Sqrt, scale=1.0 / D, bias=eps_t[:, 0:1]),
                   reads=[b_s0, b_eps], writes=[b_s1])
                op(dve, lambda: nc.vector.reciprocal(out=sm[:, 1:2], in_=sm[:, 1:2]), reads=[b_s1], writes=[b_s1])
                op(dve, lambda: nc.vector.scalar_tensor_tensor(out=oo[k][:, :], in0=acc[:, tb, :], scalar=sm[:, 1:2], in1=g_po[:, :], op0=ALU.mult, op1=ALU.mult),
                   reads=[b_acc[tb], b_s1, b_gpo], writes=[b_oo[k]])
                op(dve, lambda: nc.vector.tensor_tensor(out=oo[k][:, :], in0=oo[k][:, :], in1=hf[k3][:, :], op=ALU.add), reads=[b_oo[k], b_hf[k3]], writes=[b_oo[k]])
                op(sp, lambda: nc.sync.dma_start(out=y[tb], in_=oo[k][:, :]), reads=[b_oo[k]], writes=[b_yout], dma=b_yout)
            _wait(sp, b_yout.w)
    return nc, dbg_out


def _prep_shared(inp):
    f = np.float32
    def cp(a):
        return np.ascontiguousarray(a, dtype=f)
    w_in = cp(inp["w_in"][0].reshape(8, 128, 1952).transpose(1, 0, 2))
    w_uq = cp(inp["w_uq"][0].reshape(2, 128, 768).transpose(1, 0, 2))
    w_ukv = cp(inp["w_ukv"][0])
    w_out = inp["w_out"][0]
    w_out_a = cp(w_out[:512].reshape(4, 128, D).transpose(1, 0, 2))
    w_out_c = cp(w_out[512:].reshape(4, 128, D).transpose(1, 0, 2))
    wr = np.concatenate([inp["w_group_router"][0], inp["w_expert_router"][0]], axis=1)
    wr = cp(wr.reshape(8, 128, 36).transpose(1, 0, 2))
    br = cp(np.concatenate([inp["b_group_router"][0], inp["b_expert_router"][0]])[None, :])
    w_gate = np.concatenate([inp["w_sh_gate"], inp["w_gate"][0]], axis=0)
    w_up = np.concatenate([inp["w_sh_up"], inp["w_up"][0]], axis=0)
    w_down = np.concatenate([inp["w_sh_down"], inp["w_down"][0]], axis=0)
    gu = np.empty((NEXP + 1, 128, 8, 512), f)
    gu[:, :, :, 0:256] = w_gate.reshape(NEXP + 1, 8, 128, 256).transpose(0, 2, 1, 3)
    gu[:, :, :, 256:512] = w_up.reshape(NEXP + 1, 8, 128, 256).transpose(0, 2, 1, 3)
    wgu_lo = cp(gu[:, :, 0:4].reshape((NEXP + 1) * 128, 2048))
    wgu_hi = cp(gu[:, :, 4:8].reshape((NEXP + 1) * 128, 2048))
    wd_h = cp(w_down.reshape(NEXP + 1, 2, 128, D).transpose(0, 2, 1, 3).reshape((NEXP + 1) * 128, 2048))
    triu = np.triu(np.ones((128, 128), f))
    vbase = (np.arange(64, dtype=f) * 128)[None, :]
    tokid = (np.arange(16, dtype=f)[None, :] * 128 + np.arange(128, dtype=f)[:, None])
    pidx = np.arange(128, dtype=f)[:, None]
    jidx = np.arange(NOV, dtype=f)[None, :]
    mapinit = np.zeros((NSLOT, 4), f); mapinit[:, 0] = 2048.0; mapinit[:, 1] = 1.0e6
    def pc(v, p):
        return cp(v.reshape(-1, p).T)
    ident = np.eye(128, dtype=f)
    rot = np.zeros((96, 96), f)
    for i in range(16):
        rot[64 + i + 16, 64 + i] = -1.0
        rot[64 + i, 64 + i + 16] = 1.0
    sel = np.zeros((65, 64), f); sel[64, :] = 1.0
    inv_freq = (1.0 / (10000.0 ** (np.arange(0, 32, 2, dtype=np.float32) / np.float32(32)))).astype(f)
    invf = np.zeros((96, 1), f); invf[64:80, 0] = inv_freq; invf[80:96, 0] = inv_freq
    dmask = np.zeros((128, 4, 512), f)
    for d_ in range(4):
        dmask[:, d_, :] = (np.arange(512)[None, :] >= (np.arange(128)[:, None] + 128 * d_)).astype(f)
    prefix = np.concatenate([np.zeros(112, np.int32), np.arange(16, dtype=np.int32)])[None, :]
    return dict(
        w_in=w_in, w_uq=w_uq, w_ukv=w_ukv, w_out_a=w_out_a, w_out_c=w_out_c, wr=wr, br=br,
        wgu_lo=wgu_lo, wgu_hi=wgu_hi, wd_h=wd_h, triu=cp(triu), vbase=cp(vbase), tokid=cp(tokid), pidx=cp(pidx), jidx=cp(jidx), mapinit=mapinit,
        g_pfb=cp(inp["pre_ffn_norm"][0][None, :]),
        g_pre=pc(inp["pre_mix_norm"][0], 128), g_q=pc(inp["q_norm"][0], 128), g_kv=pc(inp["kv_norm"][0], 128),
        g_a=pc(inp["attn_out_norm"][0], 128), g_c=pc(inp["conv_out_norm"][0], 128),
        convw=cp(inp["conv_w"][0].T.reshape(4, 128, 3).transpose(1, 0, 2)),
        g_pm=cp(inp["post_mix_norm"][0][None, :]), g_pf=pc(inp["pre_ffn_norm"][0], 128), g_po=cp(inp["post_ffn_norm"][0][None, :]),
        ident=ident, rot96=rot, invf=invf, dmask=dmask, prefix=prefix,
    )


def groups_of(j):
    return [j, 7 - j, 8 + j, 15 - j]


def _prep_core(inp, core, shared, batch_cache):
    b, j = core // 4, core % 4
    f = np.float32
    x = inp["x"]
    if b not in batch_cache:
        xb = np.asarray(x[b], dtype=f)
        hTm = np.ascontiguousarray(xb.reshape(16, 512, 8, 128).transpose(0, 3, 2, 1))
        h0 = np.zeros((128, D), f)
        h0[112:] = np.asarray(inp["meta_tokens"], dtype=f)
        hT0 = np.ascontiguousarray(h0.reshape(128, 8, 128).transpose(2, 1, 0))
        pos = np.ascontiguousarray(np.asarray(inp["positions"][b], dtype=np.int32)[None, :])
        batch_cache[b] = (hTm, hT0, pos)
    hTm, hT0, pos = batch_cache[b]
    Gs = groups_of(j)
    hq = np.empty((4, 128, 8, 514), f)
    posq = np.empty((4, 512), np.int32)
    xown = np.empty((16, 128, D), f)
    order = []
    abias = np.zeros((128, 12), f)
    for s_, G in enumerate(Gs):
        others = [g for g in range(4 * s_, 4 * s_ + 4) if g != G]
        emp = [g for g in others if g > G]
        ful = [g for g in others if g < G]
        reg = emp + ful + [G]
        for r_, g in enumerate(reg[:3]):
            abias[:, s_ * 3 + r_] = -30000.0 if g > G else 0.0
        order += reg
    hTm_c = np.ascontiguousarray(hTm[order])
    pos_c = np.ascontiguousarray(pos.reshape(16, 512)[order].reshape(1, SEQ))
    for gi, G in enumerate(Gs):
        hq[gi, :, :, 2:] = hTm[G]
        hq[gi, :, :, 0:2] = hTm[G - 1][:, :, 510:512] if G > 0 else hT0[:, :, 126:128]
        posq[gi] = pos[0, 512 * G:512 * G + 512]
        xown[gi * 4:(gi + 1) * 4] = np.asarray(x[b, 512 * G:512 * G + 512], dtype=f).reshape(4, 128, D)
    m = dict(shared)
    m.update(hT0=hT0, hTm=hTm_c, hq=hq, pos=pos_c, posq=posq, xown=xown, abias=abias)
    return m


def kernel(**inputs):
    dbg = bool(os.environ.get("MK_DEBUG"))
    inp = {k: np.asarray(v) for k, v in inputs.items()}
    shared = _prep_shared(inp)
    cache = {}
    in_maps = [_prep_core(inp, c, shared, cache) for c in range(8)]
    nc, dbg_out = build_program(dbg=dbg)
    res = run_bass_kernel_spmd(nc, in_maps, core_ids=list(range(8)))
    out = np.empty((2, SEQ, D), np.float32)
    for c in range(8):
        b, j = c // 4, c % 4
        yv = np.asarray(res.results[c]["y"])
        for gi, G in enumerate(groups_of(j)):
            out[b, 512 * G:512 * G + 512] = yv[gi * 4:(gi + 1) * 4].reshape(512, D)
    if dbg:
        kernel.dbg = [{k: np.asarray(res.results[c]["dbg_" + k]) for k in dbg_out} for c in range(8)]
    return out
```

```python
import os
from contextlib import ExitStack

import numpy as np
import concourse.bass as bass
import concourse.mybir as mybir
from concourse.bass_utils import run_bass_kernel_spmd

F32 = mybir.dt.float32
BF16 = mybir.dt.bfloat16
I32 = mybir.dt.int32
AF = mybir.ActivationFunctionType
ALU = mybir.AluOpType
AX = mybir.AxisListType

D = 1024
SEQ = 8192
NCH = 17
P_TOT = 128 + 16 * 512
NKB = 65
EXT = [17, 33, 49, 65]
UNC0 = [1, 17, 33, 49]
SCALE = float((64 + 32) ** -0.5)
EPS = 1e-6
MAGIC = 12582912.0
TWO_PI = 6.283185307179586
NEXP = 32
CAP = int(os.environ.get('MK_CAP', '128'))
NOV = 30
NSLOT = 64 * 128 + NOV * 128


class Q:
    def __init__(self, eng, sem):
        self.eng, self.sem, self.n, self.seen = eng, sem, 0, {}


class Buf:
    __slots__ = ("w", "rs", "dsem", "dn")

    def __init__(self, dsem=None):
        self.w, self.rs, self.dsem, self.dn = None, {}, dsem, 0


def _wait(q, tok):
    if tok is None:
        return
    sem, v = tok
    k = id(sem)
    if q.seen.get(k, 0) >= v:
        return
    q.eng.wait_ge(sem, v)
    q.seen[k] = v


_LD = {"pending": [], "buf": None, "last_total": 0, "waited": set()}


def _finish_loads():
    ld = _LD["buf"]
    if _LD["pending"]:
        for b in _LD["pending"]:
            b.w = (ld.dsem, ld.dn)
        _LD["pending"] = []
        _LD["last_total"] = ld.dn
        _LD["waited"] = set()


def op(q, fn, reads=(), writes=(), dma=None):
    if _LD["pending"] and dma is not _LD["buf"]:
        _finish_loads()
    for b in reads:
        _wait(q, b.w)
    for b in writes:
        _wait(q, b.w)
        for k, t in b.rs.items():
            _wait(q, t)
    ins = fn()
    if dma is not None:
        ins.then_inc(dma.dsem, 16)
        dma.dn += 16
        tok = (dma.dsem, dma.dn)
    else:
        ins.then_inc(q.sem, 1)
        q.n += 1
        tok = (q.sem, q.n)
    for b in reads:
        k = id(tok[0])
        if b.rs.get(k, (None, 0))[1] < tok[1]:
            b.rs[k] = tok
    for b in writes:
        b.w = tok
        b.rs = {}
    return tok


def build_program(dbg=False):
    nc = bass.Bass("TRN2", target_bir_lowering=False)

    def din(name, shape, dt=F32):
        return nc.dram_tensor(name, list(shape), dt, kind="ExternalInput").ap()

    def dout(name, shape, dt=F32):
        return nc.dram_tensor(name, list(shape), dt, kind="ExternalOutput").ap()

    hT0 = din("hT0", [128, 8, 128])
    hTm = din("hTm", [16, 128, 8, 512])
    hq = din("hq", [4, 128, 8, 514])
    posrow = din("pos", [1, SEQ], I32)
    posq = din("posq", [4, 512], I32)
    xown = din("xown", [16, 128, D])
    abias_d = din("abias", [128, 12])
    w_in = din("w_in", [128, 8, 1952])
    w_uq = din("w_uq", [128, 2, 768])
    w_ukv = din("w_ukv", [128, 1024])
    w_out_a = din("w_out_a", [128, 4, D])
    w_out_c = din("w_out_c", [128, 4, D])
    wr_d = din("wr", [128, 8, 36])
    br_d = din("br", [1, 36])
    NROW = (NEXP + 1) * 128
    wgu_lo = din("wgu_lo", [NROW, 2048])
    wgu_hi = din("wgu_hi", [NROW, 2048])
    wd_h = din("wd_h", [NROW, 2048])
    triu_d = din("triu", [128, 128])
    vb_d = din("vbase", [1, 64])
    tokid_d = din("tokid", [128, 16])
    pidx_d = din("pidx", [128, 1])
    jidx_d = din("jidx", [1, NOV])
    mapinit_d = din("mapinit", [NSLOT, 4])
    g_pfb_d = din("g_pfb", [1, D])
    wbf_gu = nc.dram_tensor("wbf_gu", [NROW, 4096], BF16, kind="Internal").ap()
    wbf_d = nc.dram_tensor("wbf_d", [NROW, 2048], BF16, kind="Internal").ap()
    hn2d = nc.dram_tensor("hn2d", [2049, D], BF16, kind="Internal").ap()
    maps_d = nc.dram_tensor("maps", [NSLOT, 4], F32, kind="Internal").ap()
    Yd = nc.dram_tensor("Yd", [4096, D], F32, kind="Internal").ap()
    g_pre_d = din("g_pre", [128, 8])
    g_q_d = din("g_q", [128, 2])
    g_kv_d = din("g_kv", [128, 1])
    g_a_d = din("g_a", [128, 4])
    g_c_d = din("g_c", [128, 4])
    convw_d = din("convw", [128, 4, 3])
    g_pm_d = din("g_pm", [1, D])
    g_pf_d = din("g_pf", [128, 8])
    g_po_d = din("g_po", [1, D])
    ident_d = din("ident", [128, 128])
    rot_d = din("rot96", [96, 96])
    invf_d = din("invf", [96, 1])
    dmask_d = din("dmask", [128, 4, 512])
    prefix_d = din("prefix", [1, 128], I32)
    y = dout("y", [16, 128, D])
    dbg_out = {}

    with ExitStack() as top:
        def sem(name):
            return top.enter_context(nc.semaphore(name))

        pe = Q(nc.tensor, sem("s_pe"))
        act = Q(nc.scalar, sem("s_act"))
        dve = Q(nc.vector, sem("s_dve"))
        pool = Q(nc.gpsimd, sem("s_pool"))
        sp = Q(nc.sync, sem("s_sp"))
        nsem = [0]

        def dbuf():
            nsem[0] += 1
            return Buf(sem("d%d" % nsem[0]))

        ARENA_BYTES = 210000
        arena = top.enter_context(nc.sbuf_tensor("arena", [128, ARENA_BYTES], mybir.dt.uint8))
        RA = (0, 32768)
        RB = (32768, 98304)
        RC = (98304, 131072)
        RD = (131072, 147456)
        RE = (147456, 155648)
        RF = (155648, ARENA_BYTES)

        class Bump:
            def __init__(self, *segs):
                self.segs = [list(sg) for sg in segs]

            def alloc(self, nbytes):
                nbytes = (nbytes + 63) // 64 * 64
                for sg in self.segs:
                    if sg[1] - sg[0] >= nbytes:
                        off = sg[0]
                        sg[0] += nbytes
                        return off
                raise RuntimeError("arena exhausted for %d bytes: %s" % (nbytes, self.segs))

        DTB = {F32: 4, BF16: 2, I32: 4}

        def SB(es, name, shape, dt):
            shape = list(shape)
            n = 1
            for d_ in shape[1:]:
                n *= d_
            off = es.alloc(n * DTB[dt])
            v = arena[0:shape[0], off:off + n * DTB[dt]].bitcast(dt)
            if len(shape) == 3:
                v = v.rearrange("p (a b) -> p a b", a=shape[1])
            elif len(shape) != 2:
                raise ValueError(shape)
            return v

        def at(region_off, shape, dt):
            return SB(Bump((region_off, ARENA_BYTES)), "", shape, dt)

        def PS(pes, name, shape, dt=F32):
            return pes.enter_context(nc.psum_tensor("ps_" + name, list(shape), dt))

        _LD.update(pending=[], buf=dbuf(), last_total=0, waited=set())
        BND = {}
        for _v in (NSLOT - 1, NROW - 1, 2048, 4095):
            BND[_v] = nc.gpsimd.alloc_register("bnd%d" % _v)
            nc.gpsimd.reg_mov(BND[_v], _v)

        def load(es, name, src, shape, dt=F32, q=None, cast=False):
            t = SB(es, name, shape, dt)
            b = Buf()
            qq = pool if cast else (q or sp)
            ld = _LD["buf"]
            if id(qq) not in _LD["waited"]:
                _wait(qq, (ld.dsem, _LD["last_total"]) if _LD["last_total"] else None)
                _LD["waited"].add(id(qq))
            op(qq, lambda: qq.eng.dma_start(out=t[:], in_=src), writes=[b], dma=ld)
            _LD["pending"].append(b)
            return t, b

        cst = Bump(RE)
        ones_bf = SB(cst, "ones_bf", [128, 128], BF16)
        b_ones = Buf()
        op(pool, lambda: nc.gpsimd.memset(ones_bf[:], 1.0), writes=[b_ones])
        rot_t, b_rot = load(cst, "rot_t", rot_d[:, :], [96, 96])
        invf_t, b_invf = load(cst, "invf_t", invf_d[:, :], [96, 1])

        QT = at(RA[0], [96, 8, 2048], BF16)
        b_QT = [[Buf() for _ in range(4)] for _ in range(8)]
        convnT = at(RD[0], [128, 4, 2048], BF16)
        b_convn = [Buf() for _ in range(4)]

        def emit_rope(es_tiles, src_ap, width, add16, tagbufs):
            pos_i, ang, kf, cs, sn, b_pos, b_ang, b_kf, b_cos, b_sin = tagbufs
            R = slice(64, 96)
            op(sp, lambda: nc.sync.dma_start(out=pos_i[R, 0:width], in_=src_ap.partition_broadcast(32)),
               writes=[b_pos], dma=b_pos)
            op(dve, lambda: nc.vector.tensor_scalar(out=ang[R, 0:width], in0=pos_i[R, 0:width], scalar1=float(add16),
                                                    scalar2=invf_t[R, 0:1], op0=ALU.add, op1=ALU.mult),
               reads=[b_pos, b_invf], writes=[b_ang])
            op(dve, lambda: nc.vector.tensor_scalar(out=kf[R, 0:width], in0=ang[R, 0:width], scalar1=1.0 / TWO_PI,
                                                    scalar2=MAGIC, op0=ALU.mult, op1=ALU.add),
               reads=[b_ang], writes=[b_kf])
            op(dve, lambda: nc.vector.tensor_scalar(out=kf[R, 0:width], in0=kf[R, 0:width], scalar1=-MAGIC,
                                                    scalar2=-TWO_PI, op0=ALU.add, op1=ALU.mult),
               reads=[b_kf], writes=[b_kf])
            op(dve, lambda: nc.vector.tensor_tensor(out=ang[R, 0:width], in0=ang[R, 0:width], in1=kf[R, 0:width], op=ALU.add),
               reads=[b_ang, b_kf], writes=[b_ang])
            op(dve, lambda: nc.vector.tensor_scalar(out=ang[R, 0:width], in0=ang[R, 0:width], scalar1=-3.1415925,
                                                    scalar2=3.1415925, op0=ALU.max, op1=ALU.min),
               reads=[b_ang], writes=[b_ang])
            op(act, lambda: nc.scalar.activation(out=sn[R, 0:width], in_=ang[R, 0:width], func=AF.Sin),
               reads=[b_ang], writes=[b_sin])
            op(act, lambda: nc.scalar.activation(out=kf[R, 0:width], in_=ang[R, 0:width], func=AF.Sin, scale=0.5),
               reads=[b_ang], writes=[b_kf])
            op(dve, lambda: nc.vector.tensor_tensor(out=kf[R, 0:width], in0=kf[R, 0:width], in1=kf[R, 0:width], op=ALU.mult),
               reads=[b_kf], writes=[b_kf])
            op(dve, lambda: nc.vector.tensor_scalar(out=cs[R, 0:width], in0=kf[R, 0:width], scalar1=-2.0,
                                                    scalar2=1.0, op0=ALU.mult, op1=ALU.add),
               reads=[b_kf], writes=[b_cos])

        def rstd_from_psum(ps_t, b_ps, rows, width, inv_n, tmp, b_tmp, out_t, b_out):
            op(act, lambda: nc.scalar.activation(out=tmp[0:rows, 0:width], in_=ps_t[0:rows, 0:width], func=AF.Sqrt,
                                                 scale=float(inv_n), bias=eps_t[0:rows, 0:1]),
               reads=[b_ps, b_eps], writes=[b_tmp])
            op(dve, lambda: nc.vector.reciprocal(out=out_t[0:rows, 0:width], in_=tmp[0:rows, 0:width]),
               reads=[b_tmp], writes=[b_out])

        eps_t = SB(cst, "eps_t", [128, 1], F32)
        b_eps = Buf()
        op(pool, lambda: nc.gpsimd.memset(eps_t[:], EPS), writes=[b_eps])

        def mm_acc(ps_ap, lhs_fn, rhs_fn, nk):
            def f():
                ins = None
                for c in range(nk):
                    ins = nc.tensor.matmul(ps_ap, lhsT=lhs_fn(c), rhs=rhs_fn(c), start=(c == 0), stop=(c == nk - 1))
                return ins
            return f

        def load_scaled_w(es, name, src_fn, ncols, g_t, b_g, stage, b_stage):
            t = SB(es, name, [128, 8, ncols], BF16)
            b = Buf()
            for c in range(8):
                k = c % 2
                op(sp, lambda: nc.sync.dma_start(out=stage[k][:, 0:ncols], in_=src_fn(c)), writes=[b_stage[k]], dma=b_stage[k])
                op(dve, lambda: nc.vector.tensor_scalar(out=t[:, c, :], in0=stage[k][:, 0:ncols], scalar1=g_t[:, c:c + 1], scalar2=None, op0=ALU.mult),
                   reads=[b_stage[k], b_g], writes=[b])
            return t, b

        with ExitStack() as pes:
            es = Bump(RF, RB, RC)
            g_pre, b_gpre = load(es, "g_pre", g_pre_d[:, :], [128, 8])
            convf = SB(es, "q_convf", [128, 4, 512], F32); b_convf = Buf()
            _stg = convf.rearrange("p a b -> p (a b)")
            _bst = dbuf()
            w_in_b, b_win = load_scaled_w(es, "w_in_b", lambda c: w_in[:, c, :], 1952, g_pre, b_gpre, [_stg, _stg], [_bst, _bst])
            w_uq_b, b_wuq = load(es, "w_uq_b", w_uq[:, :, :], [128, 2, 768], BF16, cast=True)
            g_q, b_gq = load(es, "g_q", g_q_d[:, :], [128, 2])
            g_c, b_gc = load(es, "g_c", g_c_d[:, :], [128, 4])
            convw, b_cw = load(es, "convw", convw_d[:, :, :], [128, 4, 3])

            def two(name, shape, dt, dma=False):
                return [SB(es, "%s%d" % (name, i), shape, dt) for i in range(2)], [dbuf() if dma else Buf() for i in range(2)]

            def one2(name, shape, dt):
                t_ = SB(es, name, shape, dt); b_ = Buf()
                return [t_, t_], [b_, b_]
            xf_, b_xf_ = two("q_xf", [128, 8, 514], F32, dma=True)
            xb_, b_xb_ = one2("q_xb", [128, 8, 514], BF16)
            xsq = SB(es, "q_xsq", [128, 8, 514], BF16); b_xsq = Buf()
            rstd_, b_rstd_ = one2("q_rstd", [128, 514], F32)
            rstd2_, b_rstd2_ = one2("q_rstd2", [128, 514], F32)
            tmp = SB(es, "q_tmp", [128, 514], F32); b_tmp = Buf()
            cqf = SB(es, "q_cqf", [128, 2, 512], F32); b_cqf = Buf()
            cqsq = SB(es, "q_cqsq", [128, 2, 512], BF16); b_cqsq = Buf()
            rq = SB(es, "q_rq", [128, 512], F32); b_rq = Buf()
            cqn_, b_cqn_ = one2("q_cqn", [128, 2, 512], BF16)
            QF_, b_QF_ = two("q_QF", [96, 512], F32)
            t1_, b_t1_ = one2("q_t1", [96, 512], F32)
            t2_, b_t2_ = one2("q_t2", [96, 512], F32)
            pos_i = SB(es, "q_pos", [96, 512], I32); ang = SB(es, "q_ang", [96, 512], F32)
            kf = SB(es, "q_kf", [96, 512], F32)
            cs_, b_cos_ = one2("q_cos", [96, 512], F32)
            sn_, b_sin_ = one2("q_sin", [96, 512], F32)
            b_pos = dbuf(); b_ang = Buf(); b_kf = Buf()
            gcf_, b_gcf_ = two("q_gcf", [128, 514], F32)
            u_, b_u_ = two("q_u", [128, 514], F32)
            yv_, b_y_ = two("q_y", [128, 512], F32)
            csq = SB(es, "q_csq", [128, 4, 512], BF16); b_csq = Buf()
            rc = SB(es, "q_rc", [128, 512], F32); b_rc = Buf()

            ps_st = PS(pes, "q_ps_st", [128, 512]); b_ps_st = Buf()
            ps_sm = PS(pes, "q_ps_sm", [128, 8, 2]); b_ps_sm = [Buf(), Buf(), Buf()]
            G = [PS(pes, "q_G%d" % i, [128, 512]) for i in range(6)]; b_G = [Buf() for _ in range(6)]

            for gi in range(4):
                kq = gi % 2
                xf, b_xf, xb, b_xb = xf_[kq], b_xf_[kq], xb_[kq], b_xb_[kq]
                rstd, b_rstd, rstd2, b_rstd2 = rstd_[kq], b_rstd_[kq], rstd2_[kq], b_rstd2_[kq]
                cs, b_cos, sn, b_sin = cs_[kq], b_cos_[kq], sn_[kq], b_sin_[kq]
                cqn, b_cqn = cqn_[kq], b_cqn_[kq]
                gc0 = gi * 512
                op(sp, lambda: nc.sync.dma_start(out=xf[:], in_=hq[gi]), writes=[b_xf], dma=b_xf)
                op(act, lambda: nc.scalar.activation(out=xb[:], in_=xf[:], func=AF.Copy), reads=[b_xf], writes=[b_xb])
                op(act, lambda: nc.scalar.activation(out=xsq[:], in_=xf[:], func=AF.Square), reads=[b_xf], writes=[b_xsq])
                op(pe, mm_acc(ps_st[:, :], lambda c: ones_bf[:, :], lambda c: xsq[:, c, 2:514], 8), reads=[b_ones, b_xsq], writes=[b_ps_st])
                op(pe, mm_acc(ps_sm[:, 0, :], lambda c: ones_bf[:, :], lambda c: xsq[:, c, 0:2], 8), reads=[b_ones, b_xsq], writes=[b_ps_sm[0]])
                op(act, lambda: nc.scalar.activation(out=tmp[:, 2:514], in_=ps_st[:, :], func=AF.Sqrt, scale=1.0 / D, bias=eps_t[:, 0:1]),
                   reads=[b_ps_st, b_eps], writes=[b_tmp])
                op(act, lambda: nc.scalar.activation(out=tmp[:, 0:2], in_=ps_sm[:, 0, :], func=AF.Sqrt, scale=1.0 / D, bias=eps_t[:, 0:1]),
                   reads=[b_ps_sm[0], b_eps], writes=[b_tmp])
                op(dve, lambda: nc.vector.reciprocal(out=rstd[:, :], in_=tmp[:, :]), reads=[b_tmp], writes=[b_rstd])
                op(dve, lambda: nc.vector.tensor_tensor(out=rstd2[:, :], in0=rstd[:, :], in1=rstd[:, :], op=ALU.mult), reads=[b_rstd], writes=[b_rstd2])
                emit_rope(None, posq[gi:gi + 1, :], 512, 16, (pos_i, ang, kf, cs, sn, b_pos, b_ang, b_kf, b_cos, b_sin))

                for m in range(2):
                    op(pe, mm_acc(G[m][:, :], lambda c: w_in_b[:, c, m * 128:(m + 1) * 128], lambda c: xb[:, c, 2:514], 8),
                       reads=[b_win, b_xb], writes=[b_G[m]])
                    op(dve, lambda: nc.vector.tensor_tensor(out=cqf[:, m, :], in0=G[m][:, :], in1=rstd[:, 2:514], op=ALU.mult),
                       reads=[b_G[m], b_rstd], writes=[b_cqf])
                op(act, lambda: nc.scalar.activation(out=cqsq[:], in_=cqf[:], func=AF.Square), reads=[b_cqf], writes=[b_cqsq])
                op(pe, mm_acc(ps_st[:, :], lambda c: ones_bf[:, :], lambda c: cqsq[:, c, :], 2), reads=[b_ones, b_cqsq], writes=[b_ps_st])
                rstd_from_psum(ps_st, b_ps_st, 128, 512, 1.0 / 256, tmp, b_tmp, rq, b_rq)
                for m in range(2):
                    op(dve, lambda: nc.vector.scalar_tensor_tensor(out=cqn[:, m, :], in0=cqf[:, m, :], scalar=g_q[:, m:m + 1], in1=rq[:, :],
                                                                   op0=ALU.mult, op1=ALU.mult),
                       reads=[b_cqf, b_gq, b_rq], writes=[b_cqn])
                def head_step(h):
                    kh = h % 2
                    pq, bq, pr, br_ = G[4], b_G[4], G[5], b_G[5]
                    QF, b_QF, t1, b_t1, t2, b_t2 = QF_[kh], b_QF_[kh], t1_[kh], b_t1_[kh], t2_[kh], b_t2_[kh]
                    op(pe, mm_acc(pq[0:96, :], lambda c: w_uq_b[:, c, h * 96:(h + 1) * 96], lambda c: cqn[:, c, :], 2),
                       reads=[b_wuq, b_cqn], writes=[bq])
                    op(act, lambda: nc.scalar.activation(out=QF[:, :], in_=pq[0:96, :], func=AF.Copy), reads=[bq], writes=[b_QF])
                    op(pe, lambda: nc.tensor.matmul(pr[0:96, :], lhsT=rot_t[:, :], rhs=QF[:, :], start=True, stop=True),
                       reads=[b_rot, b_QF], writes=[br_])
                    op(act, lambda: nc.scalar.activation(out=QT[0:64, h, gc0:gc0 + 512], in_=QF[0:64, :], func=AF.Copy), reads=[b_QF], writes=[b_QT[h][gi]])
                    op(dve, lambda: nc.vector.tensor_tensor(out=t1[64:96, :], in0=QF[64:96, :], in1=cs[64:96, :], op=ALU.mult),
                       reads=[b_QF, b_cos], writes=[b_t1])
                    op(dve, lambda: nc.vector.tensor_tensor(out=t2[64:96, :], in0=pr[64:96, :], in1=sn[64:96, :], op=ALU.mult),
                       reads=[br_, b_sin], writes=[b_t2])
                    op(dve, lambda: nc.vector.tensor_tensor(out=QT[64:96, h, gc0:gc0 + 512], in0=t1[64:96, :], in1=t2[64:96, :], op=ALU.add),
                       reads=[b_t1, b_t2], writes=[b_QT[h][gi]])
                def conv_step(cc):
                    kc = cc % 2
                    pa, ba, pb, bb, pc_, bc_ = G[0], b_G[0], G[1], b_G[1], G[2], b_G[2]
                    bh = b_ps_sm[1 + kc]
                    ph0, ph1 = ps_sm[:, 1 + kc * 2, :], ps_sm[:, 2 + kc * 2, :]
                    gcf, b_gcf, u, b_u, yv, b_y = gcf_[kc], b_gcf_[kc], u_[kc], b_u_[kc], yv_[kc], b_y_[kc]
                    cb = 416 + cc * 128
                    cg = 416 + 512 + cc * 128
                    cx = 416 + 1024 + cc * 128
                    op(pe, mm_acc(pb[:, :], lambda c: w_in_b[:, c, cg:cg + 128], lambda c: xb[:, c, 2:514], 8), reads=[b_win, b_xb], writes=[bb])
                    op(pe, mm_acc(pc_[:, :], lambda c: w_in_b[:, c, cx:cx + 128], lambda c: xb[:, c, 2:514], 8), reads=[b_win, b_xb], writes=[bc_])
                    op(pe, mm_acc(ph0, lambda c: w_in_b[:, c, cg:cg + 128], lambda c: xb[:, c, 0:2], 8), reads=[b_win, b_xb], writes=[bh])
                    op(pe, mm_acc(ph1, lambda c: w_in_b[:, c, cx:cx + 128], lambda c: xb[:, c, 0:2], 8), reads=[b_win, b_xb], writes=[bh])
                    op(pe, mm_acc(pa[:, :], lambda c: w_in_b[:, c, cb:cb + 128], lambda c: xb[:, c, 2:514], 8), reads=[b_win, b_xb], writes=[ba])
                    op(act, lambda: nc.scalar.activation(out=gcf[:, 2:514], in_=pb[:, :], func=AF.Copy), reads=[bb], writes=[b_gcf])
                    op(act, lambda: nc.scalar.activation(out=gcf[:, 0:2], in_=ph0, func=AF.Copy), reads=[bh], writes=[b_gcf])
                    op(dve, lambda: nc.vector.tensor_tensor(out=u[:, 2:514], in0=pc_[:, :], in1=gcf[:, 2:514], op=ALU.mult),
                       reads=[bc_, b_gcf], writes=[b_u])
                    op(dve, lambda: nc.vector.tensor_tensor(out=u[:, 0:2], in0=ph1, in1=gcf[:, 0:2], op=ALU.mult),
                       reads=[bh, b_gcf], writes=[b_u])
                    op(dve, lambda: nc.vector.tensor_tensor(out=u[:, :], in0=u[:, :], in1=rstd2[:, :], op=ALU.mult), reads=[b_u, b_rstd2], writes=[b_u])
                    op(dve, lambda: nc.vector.tensor_scalar(out=yv[:, :], in0=u[:, 0:512], scalar1=convw[:, cc, 0:1], scalar2=None, op0=ALU.mult),
                       reads=[b_u, b_cw], writes=[b_y])
                    op(dve, lambda: nc.vector.scalar_tensor_tensor(out=yv[:, :], in0=u[:, 1:513], scalar=convw[:, cc, 1:2], in1=yv[:, :], op0=ALU.mult, op1=ALU.add),
                       reads=[b_u, b_cw, b_y], writes=[b_y])
                    op(dve, lambda: nc.vector.scalar_tensor_tensor(out=yv[:, :], in0=u[:, 2:514], scalar=convw[:, cc, 2:3], in1=yv[:, :], op0=ALU.mult, op1=ALU.add),
                       reads=[b_u, b_cw, b_y], writes=[b_y])
                    op(dve, lambda: nc.vector.tensor_tensor(out=yv[:, :], in0=yv[:, :], in1=rstd[:, 2:514], op=ALU.mult), reads=[b_y, b_rstd], writes=[b_y])
                    op(dve, lambda: nc.vector.tensor_tensor(out=convf[:, cc, :], in0=pa[:, :], in1=yv[:, :], op=ALU.mult), reads=[ba, b_y], writes=[b_convf])
                for step_ in (lambda: conv_step(0), lambda: head_step(0), lambda: head_step(1), lambda: conv_step(1), lambda: head_step(2), lambda: head_step(3),
                              lambda: conv_step(2), lambda: head_step(4), lambda: head_step(5), lambda: conv_step(3), lambda: head_step(6), lambda: head_step(7)):
                    step_()
                op(act, lambda: nc.scalar.activation(out=csq[:], in_=convf[:], func=AF.Square), reads=[b_convf], writes=[b_csq])
                op(pe, mm_acc(ps_st[:, :], lambda c: ones_bf[:, :], lambda c: csq[:, c, :], 4), reads=[b_ones, b_csq], writes=[b_ps_st])
                rstd_from_psum(ps_st, b_ps_st, 128, 512, 1.0 / 512, tmp, b_tmp, rc, b_rc)
                for cc in range(4):
                    op(dve, lambda: nc.vector.scalar_tensor_tensor(out=convnT[:, cc, gc0:gc0 + 512], in0=convf[:, cc, :], scalar=g_c[:, cc:cc + 1], in1=rc[:, :],
                                                                   op0=ALU.mult, op1=ALU.mult),
                       reads=[b_convf, b_gc, b_rc], writes=[b_convn[gi]])
            nc.all_engine_barrier()

        ckvnT = at(RB[0], [128, P_TOT], BF16)
        b_ckvn = [Buf() for _ in range(NCH)]
        KT0 = at(RB[0] + 16640, [96, P_TOT], BF16)
        b_KT = [Buf(), Buf()]
        chunk_cols = [(0, 128)] + [(128 + 512 * g, 512) for g in range(16)]

        with ExitStack() as pes:
            es = Bump(RF, RC, (RB[0] + 49920, RB[1]))
            g_pre, b_gpre = load(es, "a_g_pre", g_pre_d[:, :], [128, 8])
            g_kv, b_gkv = load(es, "a_g_kv", g_kv_d[:, :], [128, 1])
            stage = [SB(es, "a_stage%d" % i, [128, 160], F32) for i in range(2)]; b_stage = [dbuf(), dbuf()]
            w_kv_b, b_wkv = load_scaled_w(es, "w_kv_b", lambda c: w_in[:, c, 256:384], 128, g_pre, b_gpre, stage, b_stage)
            w_kpe_b = SB(es, "w_kpe_b", [128, 8, 96], BF16)
            b_wkpe = Buf()
            op(dve, lambda: nc.vector.memset(w_kpe_b[:], 0.0), writes=[b_wkpe])
            for c in range(8):
                k = c % 2
                op(sp, lambda: nc.sync.dma_start(out=stage[k][:, 0:32], in_=w_in[:, c, 384:416]), writes=[b_stage[k]], dma=b_stage[k])
                op(dve, lambda: nc.vector.tensor_scalar(out=w_kpe_b[:, c, 64:96], in0=stage[k][:, 0:32], scalar1=g_pre[:, c:c + 1], scalar2=None, op0=ALU.mult),
                   reads=[b_stage[k], b_gpre], writes=[b_wkpe])
            KPE = SB(es, "a_KPE", [96, 512], F32); b_KPE = Buf()
            op(dve, lambda: nc.vector.memset(KPE[:], 0.0), writes=[b_KPE])

            def two(name, shape, dt, dma=False):
                return [SB(es, "%s%d" % (name, i), shape, dt) for i in range(2)], [dbuf() if dma else Buf() for i in range(2)]
            xf, b_xf = two("a_xf", [128, 8, 512], F32, dma=True)
            xb, b_xb = two("a_xb", [128, 8, 512], BF16)
            _xs = SB(es, "a_xsq", [128, 8, 512], BF16); _bxs = Buf()
            xsq, b_xsq = [_xs, _xs], [_bxs, _bxs]
            rstd, b_rstd = two("a_rstd", [128, 512], F32)
            _t = SB(es, "a_tmp", [128, 512], F32); _bt = Buf(); tmp, b_tmp = [_t, _t], [_bt, _bt]
            ckvf, b_ckvf = two("a_ckvf", [128, 512], F32)
            sqb, b_sqb = two("a_sqb", [128, 512], BF16)
            _r = SB(es, "a_rkv", [128, 512], F32); _br = Buf(); rkv, b_rkv = [_r, _r], [_br, _br]
            _a = SB(es, "a_t1", [96, 512], F32); _ba = Buf(); t1, b_t1 = [_a, _a], [_ba, _ba]
            _c = SB(es, "a_t2", [96, 512], F32); _bc = Buf(); t2, b_t2 = [_c, _c], [_bc, _bc]
            pos_i = SB(es, "a_pos", [96, 512], I32); ang = SB(es, "a_ang", [96, 512], F32)
            kf = SB(es, "a_kf", [96, 512], F32)
            cs_, b_cos_ = two("a_cos", [96, 512], F32)
            sn_, b_sin_ = two("a_sin", [96, 512], F32)
            b_pos = dbuf(); b_ang = Buf(); b_kf = Buf()
            ps_st = [PS(pes, "a_ps_st%d" % i, [128, 512]) for i in range(2)]; b_ps_st = [Buf(), Buf()]
            ps_kv = [PS(pes, "a_ps_kv%d" % i, [128, 512]) for i in range(2)]; b_ps_kv = [Buf(), Buf()]
            ps_kp = [PS(pes, "a_ps_kp%d" % i, [96, 512]) for i in range(2)]; b_ps_kp = [Buf(), Buf()]
            ps_r = [PS(pes, "a_ps_r%d" % i, [96, 512]) for i in range(2)]; b_ps_r = [Buf(), Buf()]

            for ci in range(NCH):
                k = ci % 2
                c0, W = chunk_cols[ci]
                cs, b_cos, sn, b_sin = cs_[k], b_cos_[k], sn_[k], b_sin_[k]
                src = hT0[:, :, :] if ci == 0 else hTm[ci - 1]
                op(sp, lambda: nc.sync.dma_start(out=xf[k][:, :, 0:W], in_=src), writes=[b_xf[k]], dma=b_xf[k])
                op(act, lambda: nc.scalar.activation(out=xb[k][:, :, 0:W], in_=xf[k][:, :, 0:W], func=AF.Copy), reads=[b_xf[k]], writes=[b_xb[k]])
                op(act, lambda: nc.scalar.activation(out=xsq[k][:, :, 0:W], in_=xf[k][:, :, 0:W], func=AF.Square), reads=[b_xf[k]], writes=[b_xsq[k]])
                if ci == 0:
                    emit_rope(None, prefix_d[0:1, :], W, 0, (pos_i, ang, kf, cs, sn, b_pos, b_ang, b_kf, b_cos, b_sin))
                else:
                    emit_rope(None, posrow[0:1, 512 * (ci - 1):512 * ci], W, 16, (pos_i, ang, kf, cs, sn, b_pos, b_ang, b_kf, b_cos, b_sin))
                op(pe, mm_acc(ps_st[k][:, 0:W], lambda c: ones_bf[:, :], lambda c: xsq[k][:, c, 0:W], 8), reads=[b_ones, b_xsq[k]], writes=[b_ps_st[k]])
                rstd_from_psum(ps_st[k], b_ps_st[k], 128, W, 1.0 / D, tmp[k], b_tmp[k], rstd[k], b_rstd[k])
                op(pe, mm_acc(ps_kv[k][:, 0:W], lambda c: w_kv_b[:, c, :], lambda c: xb[k][:, c, 0:W], 8), reads=[b_wkv, b_xb[k]], writes=[b_ps_kv[k]])
                op(pe, mm_acc(ps_kp[k][:, 0:W], lambda c: w_kpe_b[:, c, :], lambda c: xb[k][:, c, 0:W], 8), reads=[b_wkpe, b_xb[k]], writes=[b_ps_kp[k]])
                op(dve, lambda: nc.vector.tensor_tensor(out=ckvf[k][:, 0:W], in0=ps_kv[k][:, 0:W], in1=rstd[k][:, 0:W], op=ALU.mult),
                   reads=[b_ps_kv[k], b_rstd[k]], writes=[b_ckvf[k]])
                op(act, lambda: nc.scalar.activation(out=sqb[k][:, 0:W], in_=ckvf[k][:, 0:W], func=AF.Square), reads=[b_ckvf[k]], writes=[b_sqb[k]])
                op(pe, lambda: nc.tensor.matmul(ps_st[k][:, 0:W], lhsT=ones_bf[:, :], rhs=sqb[k][:, 0:W], start=True, stop=True),
                   reads=[b_ones, b_sqb[k]], writes=[b_ps_st[k]])
                op(dve, lambda: nc.vector.tensor_tensor(out=KPE[64:96, 0:W], in0=ps_kp[k][64:96, 0:W], in1=rstd[k][64:96, 0:W], op=ALU.mult),
                   reads=[b_ps_kp[k], b_rstd[k]], writes=[b_KPE])
                op(pe, lambda: nc.tensor.matmul(ps_r[k][:, 0:W], lhsT=rot_t[:, :], rhs=KPE[:, 0:W], start=True, stop=True),
                   reads=[b_rot, b_KPE], writes=[b_ps_r[k]])
                rstd_from_psum(ps_st[k], b_ps_st[k], 128, W, 1.0 / 128, tmp[k], b_tmp[k], rkv[k], b_rkv[k])
                op(dve, lambda: nc.vector.scalar_tensor_tensor(out=ckvnT[:, c0:c0 + W], in0=ckvf[k][:, 0:W], scalar=g_kv[:, 0:1], in1=rkv[k][:, 0:W],
                                                               op0=ALU.mult, op1=ALU.mult),
                   reads=[b_ckvf[k], b_gkv, b_rkv[k]], writes=[b_ckvn[ci]])
                op(dve, lambda: nc.vector.tensor_tensor(out=t1[k][64:96, 0:W], in0=KPE[64:96, 0:W], in1=cs[64:96, 0:W], op=ALU.mult),
                   reads=[b_KPE, b_cos], writes=[b_t1[k]])
                op(dve, lambda: nc.vector.tensor_tensor(out=t2[k][64:96, 0:W], in0=ps_r[k][64:96, 0:W], in1=sn[64:96, 0:W], op=ALU.mult),
                   reads=[b_ps_r[k], b_sin], writes=[b_t2[k]])
                op(dve, lambda: nc.vector.tensor_tensor(out=KT0[64:96, c0:c0 + W], in0=t1[k][64:96, 0:W], in1=t2[k][64:96, 0:W], op=ALU.add),
                   reads=[b_t1[k], b_t2[k]], writes=[b_KT[0]])
            nc.all_engine_barrier()

        attnT = at(RC[0], [128, 4, 2048], BF16)
        b_attn = [Buf() for _ in range(4)]
        with ExitStack() as pes:
            es = Bump(RF, (RC[0] + 16384, RC[1]), (RB[0] + 49920, RB[1]))
            KT1 = at(RB[0] + 33280, [96, P_TOT], BF16)
            KT = [KT0, KT1]
            op(dve, lambda: nc.vector.tensor_copy(out=KT1[64:96, :], in_=KT0[64:96, :]), reads=[b_KT[0]], writes=[b_KT[1]])
            w_ukv_b, b_wukv = load(es, "w_ukv_b", w_ukv[:, :], [128, 1024], BF16, cast=True)
            selm = SB(es, "selm", [128, 2, 128], F32); b_sel = Buf()
            op(dve, lambda: nc.vector.memset(selm[:], 0.0), writes=[b_sel])
            op(dve, lambda: nc.vector.memset(selm[64:65, 0, 0:64], 1.0), writes=[b_sel])
            op(dve, lambda: nc.vector.memset(selm[0:1, 1, 64:128], 1.0), writes=[b_sel])
            dmask_b, b_dmask = load(es, "dmask_b", dmask_d[:, :, :], [128, 4, 512], BF16, cast=True)
            abias, b_abias = load(es, "abias", abias_d[:, :], [128, 12])
            g_a, b_ga = load(es, "g_a", g_a_d[:, :], [128, 4])
            Vaug = [SB(es, "Vaug%d" % i, [128, NKB, 128], BF16) for i in range(2)]
            b_V = [Buf(), Buf()]
            for i in range(2):
                onec = 64 if i == 0 else 0
                op(pool, lambda: nc.gpsimd.memset(Vaug[i][:], 0.0), writes=[b_V[i]])
                op(pool, lambda: nc.gpsimd.memset(Vaug[i][:, :, onec:onec + 1], 1.0), writes=[b_V[i]])
                op(pool, lambda: nc.gpsimd.memset(Vaug[i][0:112, 0, onec:onec + 1], 0.0), writes=[b_V[i]])
            PT = [SB(es, "PT%d" % i, [128, 2, 512], BF16) for i in range(3)]; b_PT = [Buf(), Buf(), Buf()]
            OTsb = SB(es, "OTsb", [128, 512], F32); b_OTsb = Buf()
            rec = SB(es, "rec", [128, 512], F32); b_rec = Buf()
            ps_S = [PS(pes, "ps_S%d" % i, [128, 2, 512]) for i in range(2)]; b_S = [Buf(), Buf()]
            ps_O = [PS(pes, "ps_O%d" % i, [128, 512]) for i in range(2)]; b_O = [Buf(), Buf()]
            ps_bc = PS(pes, "ps_bc", [128, 512]); b_bc = Buf()
            ps_bld = PS(pes, "ps_bld", [128, 512]); b_bld = Buf()

            def build_steps(h):
                sl = h % 2
                vc = 0 if sl == 0 else 64
                out = []
                for kb0 in range(0, NKB, 8):
                    def stepv(kb0=kb0):
                        n = min(8, NKB - kb0)

                        def f():
                            ins = None
                            for r in range(n):
                                kb = kb0 + r
                                ins = nc.tensor.matmul(ps_bld[:, r * 64:(r + 1) * 64], lhsT=ckvnT[:, kb * 128:(kb + 1) * 128],
                                                       rhs=w_ukv_b[:, h * 128 + 64:h * 128 + 128], start=True, stop=True)
                            return ins
                        op(pe, f, reads=[b_wukv] + b_ckvn, writes=[b_bld])
                        op(dve, lambda: nc.vector.tensor_copy(out=Vaug[sl][:, kb0:kb0 + n, vc:vc + 64],
                                                              in_=ps_bld[:, 0:n * 64].rearrange("p (r v) -> p r v", v=64)),
                           reads=[b_bld], writes=[b_V[sl]])
                    out.append(stepv)
                for ci in range(NCH):
                    def stepk(ci=ci):
                        c0, W = chunk_cols[ci]
                        op(pe, lambda: nc.tensor.matmul(ps_bld[0:64, 0:W], lhsT=w_ukv_b[:, h * 128:h * 128 + 64], rhs=ckvnT[:, c0:c0 + W], start=True, stop=True),
                           reads=[b_wukv, b_ckvn[ci]], writes=[b_bld])
                        op(dve, lambda: nc.vector.tensor_copy(out=KT[sl][0:64, c0:c0 + W], in_=ps_bld[0:64, 0:W]), reads=[b_bld], writes=[b_KT[sl]])
                    out.append(stepk)
                return out

            def build_head(h):
                for st_ in build_steps(h):
                    st_()

            stgc = [SB(es, "stgc%d" % i, [128, 1024], F32) for i in range(3)]; b_stgc = [dbuf() for _ in range(3)]
            outc = [SB(es, "outc%d" % i, [128, 1024], BF16) for i in range(3)]; b_outc = [Buf() for _ in range(3)]
            b_outd = [dbuf() for _ in range(3)]
            pieces = []
            for e_ in range(1, NEXP + 1):
                rows_ = slice(e_ * 128, (e_ + 1) * 128)
                for pc_i in range(6):
                    srcd_ = (wgu_lo, wgu_hi, wd_h)[pc_i // 2]
                    col_ = (pc_i % 2) * 1024
                    if pc_i < 4:
                        dst_ = wbf_gu[rows_, pc_i * 1024:(pc_i + 1) * 1024]
                    else:
                        dst_ = wbf_d[rows_, (pc_i - 4) * 1024:(pc_i - 3) * 1024]
                    pieces.append((srcd_[rows_, col_:col_ + 1024], dst_))
            NPC = len(pieces)

            def pc_in(i):
                k3 = i % 3
                op(sp, lambda: nc.sync.dma_start(out=stgc[k3][:, :], in_=pieces[i][0]), writes=[b_stgc[k3]], dma=b_stgc[k3])

            def pc_step(i):
                k3 = i % 3
                if i + 2 < NPC:
                    pc_in(i + 2)
                op(dve, lambda: nc.vector.tensor_copy(out=outc[k3][:, :], in_=stgc[k3][:, :]), reads=[b_stgc[k3]], writes=[b_outc[k3]])
                op(sp, lambda: nc.sync.dma_start(out=pieces[i][1], in_=outc[k3][:, :]), reads=[b_outc[k3]], writes=[b_outd[k3]], dma=b_outd[k3])
            pc_in(0)
            pc_in(1)
            pc_next = [0]

            build_head(0)
            pairs = []
            for gi in range(4):
                seq = []
                base = 1 + 16 * gi
                for r in range(3):
                    for i2 in range(0, 4, 2):
                        seq.append((gi * 3 + r, [(base + 4 * r + i2 + u, None) for u in range(2)]))
                for i2 in range(0, 4, 2):
                    seq.append((None, [(base + 12 + i2 + u, i2 + u) for u in range(2)]))
                fb = list(range(1, base))
                for i2 in range(0, len(fb), 2):
                    seq.append((None, [(b_, None) for b_ in fb[i2:i2 + 2]]))
                seq.append((None, [(0, None)]))
                nst = sum(len(p[1]) for p in seq)
                cnt_ = 0
                for bcol, items in seq:
                    ent = []
                    for (blk, md) in items:
                        ent.append((gi, blk, md, cnt_ == 0, cnt_ == nst - 1))
                        cnt_ += 1
                    pairs.append((bcol, ent))
            npair = len(pairs)
            gp = [0]
            for h in range(8):
                sl = h % 2
                R = slice(0, 64) if sl == 0 else slice(64, 128)
                pending_build = build_steps(h + 1) if h + 1 < 8 else []

                def emit_S(m):
                    pp = (gp[0] + m) % 2
                    ent = pairs[m][1]

                    def f():
                        ins = None
                        for j, (gi, blk, md, fst, lst) in enumerate(ent):
                            ins = nc.tensor.matmul(ps_S[pp][:, j, :], lhsT=KT[sl][0:96, blk * 128:(blk + 1) * 128], rhs=QT[0:96, h, gi * 512:(gi + 1) * 512],
                                                   start=True, stop=True)
                        return ins
                    gis = sorted(set(e_[0] for e_ in ent))
                    op(pe, f, reads=[b_KT[sl]] + [b_QT[h][g_] for g_ in gis], writes=[b_S[pp]])

                emit_S(0)
                emit_S(1)
                for m in range(npair):
                    pp = (gp[0] + m) % 2
                    pt = (gp[0] + m) % 3
                    bcol, ent = pairs[m]
                    nj = len(ent)
                    if bcol is None:
                        op(act, lambda: nc.scalar.activation(out=PT[pt][:, 0:nj, :], in_=ps_S[pp][:, 0:nj, :], func=AF.Exp, scale=SCALE),
                           reads=[b_S[pp]], writes=[b_PT[pt]])
                    else:
                        op(act, lambda: nc.scalar.activation(out=PT[pt][:, 0:nj, :], in_=ps_S[pp][:, 0:nj, :], func=AF.Exp, scale=SCALE, bias=abias[:, bcol:bcol + 1]),
                           reads=[b_S[pp], b_abias], writes=[b_PT[pt]])
                    for j, (gi, blk, md, fst, lst) in enumerate(ent):
                        if md is not None:
                            op(dve, lambda: nc.vector.tensor_tensor(out=PT[pt][:, j, :], in0=PT[pt][:, j, :], in1=dmask_b[:, md, :], op=ALU.mult),
                               reads=[b_dmask, b_PT[pt]], writes=[b_PT[pt]])
                    if m + 2 < npair:
                        emit_S(m + 2)
                    if pending_build and m % 3 == 2:
                        pending_build.pop(0)()
                    if m % 3 == 0 and pc_next[0] < NPC:
                        pc_step(pc_next[0])
                        pc_next[0] += 1
                    for j, (gi, blk, md, fst, lst) in enumerate(ent):
                        ob = (h * 4 + gi) % 2
                        op(pe, lambda: nc.tensor.matmul(ps_O[ob][:, :], lhsT=Vaug[sl][:, blk, :], rhs=PT[pt][:, j, :], start=fst, stop=lst),
                           reads=[b_V[sl], b_PT[pt]], writes=[b_O[ob]])
                        if lst:
                            op(dve, lambda: nc.vector.tensor_copy(out=OTsb[:, :], in_=ps_O[ob][:, :]), reads=[b_O[ob]], writes=[b_OTsb])
                            op(pe, lambda: nc.tensor.matmul(ps_bc[:, :], lhsT=selm[:, sl, :], rhs=OTsb[:, :], start=True, stop=True),
                               reads=[b_sel, b_OTsb], writes=[b_bc])
                            op(dve, lambda: nc.vector.reciprocal(out=rec[R, :], in_=ps_bc[R, :]), reads=[b_bc], writes=[b_rec])
                            op(dve, lambda: nc.vector.tensor_tensor(out=attnT[R, h // 2, gi * 512:(gi + 1) * 512], in0=OTsb[R, :], in1=rec[R, :], op=ALU.mult),
                               reads=[b_OTsb, b_rec], writes=[b_attn[gi]])
                while pending_build:
                    pending_build.pop(0)()
                gp[0] += npair
            while pc_next[0] < NPC:
                pc_step(pc_next[0])
                pc_next[0] += 1
            for k3_ in range(3):
                _wait(sp, b_outd[k3_].w)
            if dbg:
                dbg_out["attnT"] = dout("dbg_attnT", [128, 4, 2048], BF16)
                dbg_out["ckvnT"] = dout("dbg_ckvnT", [128, P_TOT], BF16)
                dbg_out["KT0"] = dout("dbg_KT0", [96, P_TOT], BF16)
                dbg_out["KT1"] = dout("dbg_KT1", [96, P_TOT], BF16)
                bd = dbuf()
                op(sp, lambda: nc.sync.dma_start(out=dbg_out["attnT"][:, :, :], in_=attnT[:]), reads=b_attn, writes=[bd], dma=bd)
                op(sp, lambda: nc.sync.dma_start(out=dbg_out["ckvnT"][:, :], in_=ckvnT[:]), reads=b_ckvn, writes=[bd], dma=bd)
                op(sp, lambda: nc.sync.dma_start(out=dbg_out["KT0"][:, :], in_=KT0[:]), reads=[b_KT[0]], writes=[bd], dma=bd)
                op(sp, lambda: nc.sync.dma_start(out=dbg_out["KT1"][:, :], in_=KT1[:]), reads=[b_KT[1]], writes=[bd], dma=bd)
                _wait(sp, bd.w)
            asq = SB(es, "asq", [128, 4, 512], BF16); b_asq = Buf()
            atmp = SB(es, "atmp", [128, 512], F32); b_atmp = Buf()
            ra = SB(es, "ra", [128, 512], F32); b_ra = Buf()
            for gi in range(4):
                cols = slice(gi * 512, (gi + 1) * 512)
                op(act, lambda: nc.scalar.activation(out=asq[:, :, :], in_=attnT[:, :, cols], func=AF.Square), reads=[b_attn[gi]], writes=[b_asq])
                op(pe, mm_acc(ps_bc[:, :], lambda c: ones_bf[:, :], lambda c: asq[:, c, :], 4), reads=[b_ones, b_asq], writes=[b_bc])
                rstd_from_psum(ps_bc, b_bc, 128, 512, 1.0 / 512, atmp, b_atmp, ra, b_ra)
                for c in range(4):
                    op(dve, lambda: nc.vector.scalar_tensor_tensor(out=attnT[:, c, cols], in0=attnT[:, c, cols], scalar=g_a[:, c:c + 1], in1=ra[:, :],
                                                                   op0=ALU.mult, op1=ALU.mult),
                       reads=[b_attn[gi], b_ga, b_ra], writes=[b_attn[gi]])
            nc.all_engine_barrier()

        hn2T = at(RA[0], [128, 8, 2048], BF16)
        b_hn2 = [Buf() for _ in range(16)]
        comb = SB(cst, "comb", [128, 16, 33], F32)
        b_comb = Buf()
        b_yall = dbuf()
        b_hn2d = dbuf()
        b_mapinit = dbuf()
        b_scat = dbuf()
        BIG = 1.0e30
        NT = 64 + NOV
        mapt = SB(cst, "mapt", [128, NT, 4], F32); b_mapt = dbuf()
        gidx = SB(cst, "gidx", [128, NT], I32); b_gidx = Buf()
        sidx = SB(cst, "sidx", [128, NT], I32); b_sidx = Buf()
        ovidx = SB(cst, "ovidx", [128, NOV], I32); b_ovidx = Buf()
        with ExitStack() as pes:
            es = Bump(RF)
            esl = Bump(RB)
            w_oa_b, b_woa = load(es, "w_oa_b", w_out_a[:, :, :], [128, 4, D], BF16, cast=True)
            w_oc_b, b_woc = load(es, "w_oc_b", w_out_c[:, :, :], [128, 4, D], BF16, cast=True)
            g_pm, b_gpm = load(es, "g_pm_bc", g_pm_d[0:1, :].partition_broadcast(128), [128, D])
            g_pf, b_gpf = load(es, "g_pf", g_pf_d[:, :], [128, 8])
            wr_t, b_wr = load(es, "wr_t", wr_d[:, :, :], [128, 8, 36])
            br_t, b_br = load(es, "br_bc", br_d[0:1, :].partition_broadcast(128), [128, 36])
            ident, b_id = load(es, "ident", ident_d[:, :], [128, 128])
            op(dve, lambda: nc.vector.memset(comb[:, :, 0:1], 1.0), writes=[b_comb])

            def two(name, shape, dt, dma=False):
                return [SB(esl, "%s%d" % (name, i), shape, dt) for i in range(2)], [dbuf() if dma else Buf() for i in range(2)]
            xo = [SB(esl, "xo%d" % i, [128, D], F32) for i in range(3)]; b_xo = [dbuf() for _ in range(3)]

            def load_xo(tb_):
                op(sp, lambda: nc.sync.dma_start(out=xo[tb_ % 3][:, :], in_=xown[tb_]), writes=[b_xo[tb_ % 3]], dma=b_xo[tb_ % 3])
            load_xo(0)
            load_xo(1)
            hm, b_hm = two("hm", [128, D], F32)
            hh, b_hh = two("hh", [128, D], F32)
            hs, b_hs = two("hs", [128, D], F32)
            junk, b_junk = two("junk", [128, D], BF16)
            hsT, b_hsT = two("hsT", [128, 8, 128], F32)
            sm, b_sm = two("sm", [128, 8], F32)
            b_smc = [[Buf() for _ in range(8)] for _ in range(2)]
            lgall = SB(es, "lgall", [128, 16, 36], F32); b_lg = Buf()
            g_pfb, b_gpfb = load(es, "g_pfb", g_pfb_d[0:1, :].partition_broadcast(128), [128, D])
            hrow, b_hrow = two("hrow", [128, D], BF16)
            zrow = SB(es, "zrow", [1, D], BF16); b_zrow = Buf()
            op(dve, lambda: nc.vector.memset(zrow[:], 0.0), writes=[b_zrow])
            op(sp, lambda: nc.sync.dma_start(out=hn2d[2048:2049, :], in_=zrow[:, :]), reads=[b_zrow], dma=b_hn2d)
            op(sp, lambda: nc.sync.dma_start(out=maps_d[:, :], in_=mapinit_d[:, :]), dma=b_mapinit, writes=[b_mapinit])
            ps_mix = [PS(pes, "ps_mix%d" % i, [128, D]) for i in range(2)]; b_pmix = [Buf(), Buf()]
            ps_T = PS(pes, "ps_T", [128, 8, 128]); b_pT = Buf()
            ps_lg = [PS(pes, "ps_lg%d" % i, [128, 36]) for i in range(2)]; b_plg = [Buf(), Buf()]

            hh3 = [hh[0], hh[1], SB(esl, "hh2", [128, D], F32)]; b_hh3 = [b_hh[0], b_hh[1], Buf()]
            sm3 = [SB(esl, "sm3_%d" % i, [128, 8], F32) for i in range(3)]
            b_sm3 = [[Buf() for _ in range(8)] for _ in range(3)]

            def st1(tb):
                k, k3 = tb % 2, tb % 3
                tc0 = tb * 128
                bs = b_sm3[k3]

                def S(i):
                    return sm3[k3][:, i:i + 1]
                if tb + 2 < 16:
                    load_xo(tb + 2)

                def fmix():
                    ins = None
                    for half in range(2):
                        hc = slice(half * 512, (half + 1) * 512)
                        for h in range(4):
                            ins = nc.tensor.matmul(ps_mix[k][:, hc], lhsT=attnT[:, h, tc0:tc0 + 128], rhs=w_oa_b[:, h, hc], start=(h == 0), stop=False)
                        for cc in range(4):
                            ins = nc.tensor.matmul(ps_mix[k][:, hc], lhsT=convnT[:, cc, tc0:tc0 + 128], rhs=w_oc_b[:, cc, hc], start=False, stop=(cc == 3))
                    return ins
                op(pe, fmix, reads=[b_attn[tb // 4], b_convn[tb // 4], b_woa, b_woc], writes=[b_pmix[k]])
                op(act, lambda: nc.scalar.activation(out=junk[k][:, :], in_=ps_mix[k][:, :], func=AF.Square, accum_out=S(0)),
                   reads=[b_pmix[k]], writes=[b_junk[k], bs[0]])
                op(act, lambda: nc.scalar.activation(out=S(2), in_=S(0), func=AF.Sqrt, scale=1.0 / D, bias=eps_t[:, 0:1]), reads=[bs[0], b_eps], writes=[bs[2]])
                op(dve, lambda: nc.vector.reciprocal(out=S(2), in_=S(2)), reads=[bs[2]], writes=[bs[2]])
                op(dve, lambda: nc.vector.scalar_tensor_tensor(out=hm[k][:, :], in0=ps_mix[k][:, :], scalar=S(2), in1=g_pm[:, :], op0=ALU.mult, op1=ALU.mult),
                   reads=[b_pmix[k], bs[2], b_gpm], writes=[b_hm[k]])
                op(dve, lambda: nc.vector.tensor_tensor(out=hh3[k3][:, :], in0=hm[k][:, :], in1=xo[k3][:, :], op=ALU.add), reads=[b_hm[k], b_xo[k3]], writes=[b_hh3[k3]])
                op(sp, lambda: nc.sync.dma_start(out=y[tb], in_=hh3[k3][:, :]), reads=[b_hh3[k3]], writes=[b_yall], dma=b_yall)

            def st2(tb):
                k, k3 = tb % 2, tb % 3
                tc0 = tb * 128
                bs = b_sm3[k3]

                def S(i):
                    return sm3[k3][:, i:i + 1]
                op(act, lambda: nc.scalar.activation(out=junk[k][:, :], in_=hh3[k3][:, :], func=AF.Square, accum_out=S(1)),
                   reads=[b_hh3[k3]], writes=[b_junk[k], bs[1]])
                op(act, lambda: nc.scalar.activation(out=S(3), in_=S(1), func=AF.Sqrt, scale=1.0 / D, bias=eps_t[:, 0:1]), reads=[bs[1], b_eps], writes=[bs[3]])
                op(dve, lambda: nc.vector.reciprocal(out=S(3), in_=S(3)), reads=[bs[3]], writes=[bs[3]])
                op(act, lambda: nc.scalar.activation(out=hs[k][:, :], in_=hh3[k3][:, :], func=AF.Copy, scale=S(3)), reads=[b_hh3[k3], bs[3]], writes=[b_hs[k]])

                def ftr():
                    ins = None
                    for c in range(8):
                        ins = nc.tensor.transpose(ps_T[:, c, :], hs[k][:, c * 128:(c + 1) * 128], ident[:, :])
                    return ins
                op(pe, ftr, reads=[b_hs[k], b_id], writes=[b_pT])
                op(dve, lambda: nc.vector.tensor_tensor(out=hrow[k][:, :], in0=hs[k][:, :], in1=g_pfb[:, :], op=ALU.mult), reads=[b_hs[k], b_gpfb], writes=[b_hrow[k]])
                op(sp, lambda: nc.sync.dma_start(out=hn2d[tc0:tc0 + 128, :], in_=hrow[k][:, :]), reads=[b_hrow[k]], dma=b_hn2d)

            def st3(tb):
                k = tb % 2
                tc0 = tb * 128
                op(dve, lambda: nc.vector.tensor_tensor(out=hsT[k][:, :, :], in0=ps_T[:, :, :], in1=g_pf[:, :].unsqueeze(2).to_broadcast([128, 8, 128]), op=ALU.mult),
                   reads=[b_pT, b_gpf], writes=[b_hsT[k]])
                op(act, lambda: nc.scalar.activation(out=hn2T[:, :, tc0:tc0 + 128], in_=hsT[k][:, :, :], func=AF.Copy), reads=[b_hsT[k]], writes=[b_hn2[tb]])
                op(pe, mm_acc(ps_lg[k][:, :], lambda c: hsT[k][:, c, :], lambda c: wr_t[:, c, :], 8), reads=[b_hsT[k], b_wr], writes=[b_plg[k]])
                op(dve, lambda: nc.vector.tensor_tensor(out=lgall[:, tb, :], in0=ps_lg[k][:, :], in1=br_t[:, :], op=ALU.add), reads=[b_plg[k], b_br], writes=[b_lg])

            for it in range(16 + 2):
                if it < 16:
                    st1(it)
                if 0 <= it - 2 < 16:
                    st3(it - 2)
                if 0 <= it - 1 < 16:
                    st2(it - 1)

            _wait(sp, b_yall.w)
            _wait(sp, (b_hn2d.dsem, b_hn2d.dn))
            nc.all_engine_barrier()
            es = Bump(RB)

            def T(name, shape):
                return SB(es, name, shape, F32), Buf()
            gmax, b_gmax = T("gmax", [128, 16]); gm, b_gm = T("gm", [128, 16, 4]); gsh, b_gsh = T("gsh", [128, 16, 4])
            gsum, b_gsum = T("gsum", [128, 16]); gw, b_gw = T("gw", [128, 16]); pen, b_pen = T("pen", [128, 16, 4])
            elm, b_elm = T("elm", [128, 16, 32]); elm2, b_elm2 = T("elm2", [128, 16, 32])
            m1, b_m1 = T("m1", [128, 16]); m2, b_m2 = T("m2", [128, 16])
            oh1, b_oh1 = T("oh1", [128, 16, 32]); oh2, b_oh2 = T("oh2", [128, 16, 32])
            dd, b_dd = T("dd", [128, 16]); w1, b_w1 = T("w1", [128, 16]); w2, b_w2 = T("w2", [128, 16])
            lg_g = lgall[:, :, 0:4]
            lg_e = lgall[:, :, 4:36]

            def bc(t2d, n):
                return t2d[:, :].unsqueeze(2).to_broadcast([128, 16, n])
            op(dve, lambda: nc.vector.tensor_reduce(out=gmax[:, :], in_=lg_g, axis=AX.X, op=ALU.max), reads=[b_lg], writes=[b_gmax])
            op(dve, lambda: nc.vector.tensor_tensor(out=gm[:, :, :], in0=lg_g, in1=bc(gmax, 4), op=ALU.is_equal), reads=[b_lg, b_gmax], writes=[b_gm])
            op(dve, lambda: nc.vector.tensor_tensor(out=gsh[:, :, :], in0=lg_g, in1=bc(gmax, 4), op=ALU.subtract), reads=[b_lg, b_gmax], writes=[b_gsh])
            op(act, lambda: nc.scalar.activation(out=gsh[:, :, :], in_=gsh[:, :, :], func=AF.Exp), reads=[b_gsh], writes=[b_gsh])
            op(dve, lambda: nc.vector.tensor_reduce(out=gsum[:, :], in_=gsh[:, :, :], axis=AX.X, op=ALU.add), reads=[b_gsh], writes=[b_gsum])
            op(dve, lambda: nc.vector.reciprocal(out=gw[:, :], in_=gsum[:, :]), reads=[b_gsum], writes=[b_gw])
            op(dve, lambda: nc.vector.tensor_scalar(out=pen[:, :, :], in0=gm[:, :, :], scalar1=BIG, scalar2=-BIG, op0=ALU.mult, op1=ALU.add), reads=[b_gm], writes=[b_pen])
            op(dve, lambda: nc.vector.tensor_tensor(out=elm[:, :, :].rearrange("p b (g e) -> p b g e", g=4),
                                                    in0=lg_e.rearrange("p b (g e) -> p b g e", g=4),
                                                    in1=pen[:, :, :].unsqueeze(3).to_broadcast([128, 16, 4, 8]), op=ALU.add),
               reads=[b_lg, b_pen], writes=[b_elm])
            op(dve, lambda: nc.vector.tensor_reduce(out=m1[:, :], in_=elm[:, :, :], axis=AX.X, op=ALU.max), reads=[b_elm], writes=[b_m1])
            op(dve, lambda: nc.vector.tensor_tensor(out=oh1[:, :, :], in0=elm[:, :, :], in1=bc(m1, 32), op=ALU.is_equal), reads=[b_elm, b_m1], writes=[b_oh1])
            op(dve, lambda: nc.vector.scalar_tensor_tensor(out=elm2[:, :, :], in0=oh1[:, :, :], scalar=-BIG, in1=elm[:, :, :], op0=ALU.mult, op1=ALU.add),
               reads=[b_oh1, b_elm], writes=[b_elm2])
            op(dve, lambda: nc.vector.tensor_reduce(out=m2[:, :], in_=elm2[:, :, :], axis=AX.X, op=ALU.max), reads=[b_elm2], writes=[b_m2])
            op(dve, lambda: nc.vector.tensor_tensor(out=oh2[:, :, :], in0=elm2[:, :, :], in1=bc(m2, 32), op=ALU.is_equal), reads=[b_elm2, b_m2], writes=[b_oh2])
            op(dve, lambda: nc.vector.tensor_tensor(out=dd[:, :], in0=m2[:, :], in1=m1[:, :], op=ALU.subtract), reads=[b_m1, b_m2], writes=[b_dd])
            op(act, lambda: nc.scalar.activation(out=dd[:, :], in_=dd[:, :], func=AF.Exp), reads=[b_dd], writes=[b_dd])
            op(dve, lambda: nc.vector.tensor_scalar(out=w1[:, :], in0=dd[:, :], scalar1=1.0, scalar2=None, op0=ALU.add), reads=[b_dd], writes=[b_w1])
            op(dve, lambda: nc.vector.reciprocal(out=w1[:, :], in_=w1[:, :]), reads=[b_w1], writes=[b_w1])
            op(dve, lambda: nc.vector.tensor_tensor(out=w2[:, :], in0=dd[:, :], in1=w1[:, :], op=ALU.mult), reads=[b_dd, b_w1], writes=[b_w2])
            op(dve, lambda: nc.vector.tensor_tensor(out=w1[:, :], in0=w1[:, :], in1=gw[:, :], op=ALU.mult), reads=[b_w1, b_gw], writes=[b_w1])
            op(dve, lambda: nc.vector.tensor_tensor(out=w2[:, :], in0=w2[:, :], in1=gw[:, :], op=ALU.mult), reads=[b_w2, b_gw], writes=[b_w2])
            ps_pp = ps_mix[0][:, :].rearrange("p (a n) -> p a n", a=2); b_pp = b_pmix[0]
            ps_pt = ps_mix[1][:, :].rearrange("p (a n) -> p a n", a=2); b_pt = b_pmix[1]
            triu_b, b_triu = load(es, "triu_b", triu_d[:, :], [128, 128], BF16, cast=True)
            vb, b_vb = load(es, "vb", vb_d[0:1, :].partition_broadcast(128), [128, 64])
            tokid, b_tok = load(es, "tokid", tokid_d[:, :], [128, 16])
            pidx, b_pidx = load(es, "pidx", pidx_d[:, :], [128, 1])
            jidx, b_jidx = load(es, "jidx", jidx_d[0:1, :].partition_broadcast(128), [128, NOV])
            ohv_b = SB(es, "ohv_b", [128, 16, 64], BF16); b_ohvb = Buf()
            ohv, b_ohv = T("ohv", [128, 16, 64])
            tot_s, b_tot = T("tot_s", [128, 16, 64]); boff, b_boff = T("boff", [128, 16, 64])
            rank, b_rank = T("rank", [128, 16, 64]); wk, b_wk = T("wk", [128, 16, 64])
            cnt, b_cnt = T("cnt", [128, 64]); ovt, b_ovt = T("ovt", [128, 64]); ovend, b_ovend = T("ovend", [128, 64])
            ovst, b_ovst = T("ovst", [128, 64]); one64, b_one64 = T("one64", [128, 64])
            rr, b_rr = T("rr", [128, 16, 2]); basev, b_basev = T("basev", [128, 16, 2]); ovs, b_ovs = T("ovs", [128, 16, 2])
            isov, b_isov = T("isov", [128, 16, 2]); posf, b_posf = T("posf", [128, 16, 2])
            pos_i = SB(cst, "pos_i", [128, 16, 2], I32); b_posi = Buf()
            srcm = SB(cst, "srcm", [128, 32, 4], F32); b_srcm = Buf()
            cmpj, b_cmpj = T("cmpj", [128, NOV, 64]); ovv, b_ovv = T("ovv", [128, NOV]); ovf, b_ovf = T("ovf", [128, NOV])
            op(dve, lambda: nc.vector.tensor_copy(out=ohv[:, :, 0:32], in_=oh1[:, :, :]), reads=[b_oh1], writes=[b_ohv])
            op(dve, lambda: nc.vector.tensor_copy(out=ohv[:, :, 32:64], in_=oh2[:, :, :]), reads=[b_oh2], writes=[b_ohv])
            op(dve, lambda: nc.vector.tensor_copy(out=ohv_b[:, :, :], in_=ohv[:, :, :]), reads=[b_ohv], writes=[b_ohvb])
            ohv2d = ohv_b.rearrange("p b v -> p (b v)")
            for hb in range(2):
                op(pe, lambda: nc.tensor.matmul(ps_pp[:, hb, :], lhsT=triu_b[:, :], rhs=ohv2d[:, hb * 512:(hb + 1) * 512], start=True, stop=True),
                   reads=[b_triu, b_ohvb], writes=[b_pp])
                op(pe, lambda: nc.tensor.matmul(ps_pt[:, hb, :], lhsT=ones_bf[:, :], rhs=ohv2d[:, hb * 512:(hb + 1) * 512], start=True, stop=True),
                   reads=[b_ones, b_ohvb], writes=[b_pt])
            op(act, lambda: nc.scalar.activation(out=tot_s.rearrange("p b v -> p (b v)"), in_=ps_pt.rearrange("p a n -> p (a n)"), func=AF.Copy),
               reads=[b_pt], writes=[b_tot])
            op(dve, lambda: nc.vector.memset(boff[:, 0, :], 0.0), writes=[b_boff])
            for b_ in range(1, 16):
                op(dve, lambda: nc.vector.tensor_tensor(out=boff[:, b_, :], in0=boff[:, b_ - 1, :], in1=tot_s[:, b_ - 1, :], op=ALU.add),
                   reads=[b_boff, b_tot], writes=[b_boff])
            op(dve, lambda: nc.vector.tensor_tensor(out=cnt[:, :], in0=boff[:, 15, :], in1=tot_s[:, 15, :], op=ALU.add), reads=[b_boff, b_tot], writes=[b_cnt])
            op(dve, lambda: nc.vector.tensor_tensor(out=rank.rearrange("p b v -> p (b v)"), in0=ps_pp.rearrange("p a n -> p (a n)"),
                                                    in1=boff.rearrange("p b v -> p (b v)"), op=ALU.add), reads=[b_pp, b_boff], writes=[b_rank])
            op(dve, lambda: nc.vector.tensor_tensor(out=wk[:, :, :], in0=ohv[:, :, :], in1=rank[:, :, :], op=ALU.mult), reads=[b_ohv, b_rank], writes=[b_wk])
            op(dve, lambda: nc.vector.tensor_reduce(out=rr[:, :, :], in_=wk.rearrange("p b (k e) -> p b k e", k=2), axis=AX.X, op=ALU.add), reads=[b_wk], writes=[b_rr])
            op(dve, lambda: nc.vector.tensor_scalar(out=rr[:, :, :], in0=rr[:, :, :], scalar1=-1.0, scalar2=None, op0=ALU.add), reads=[b_rr], writes=[b_rr])
            op(dve, lambda: nc.vector.tensor_scalar(out=ovt[:, :], in0=cnt[:, :], scalar1=-float(CAP), scalar2=0.0, op0=ALU.add, op1=ALU.max), reads=[b_cnt], writes=[b_ovt])
            op(dve, lambda: nc.vector.tensor_scalar(out=ovt[:, :], in0=ovt[:, :], scalar1=127.0, scalar2=1.0 / 128, op0=ALU.add, op1=ALU.mult), reads=[b_ovt], writes=[b_ovt])
            op(dve, lambda: nc.vector.tensor_scalar(out=ovt[:, :], in0=ovt[:, :], scalar1=-0.5 + 1.0 / 256, scalar2=MAGIC, op0=ALU.add, op1=ALU.add), reads=[b_ovt], writes=[b_ovt])
            op(dve, lambda: nc.vector.tensor_scalar(out=ovt[:, :], in0=ovt[:, :], scalar1=-MAGIC, scalar2=None, op0=ALU.add), reads=[b_ovt], writes=[b_ovt])
            op(dve, lambda: nc.vector.memset(one64[:, :], 1.0), writes=[b_one64])
            op(dve, lambda: nc.vector.tensor_tensor_scan(out=ovend[:, :], data0=one64[:, :], data1=ovt[:, :], initial=0.0, op0=ALU.mult, op1=ALU.add),
               reads=[b_one64, b_ovt], writes=[b_ovend])
            op(dve, lambda: nc.vector.tensor_tensor(out=ovst[:, :], in0=ovend[:, :], in1=ovt[:, :], op=ALU.subtract), reads=[b_ovend, b_ovt], writes=[b_ovst])
            op(dve, lambda: nc.vector.tensor_scalar(out=ovst[:, :], in0=ovst[:, :], scalar1=128.0, scalar2=None, op0=ALU.mult), reads=[b_ovst], writes=[b_ovst])

            def bcv(t2d):
                return t2d[:, :].unsqueeze(1).to_broadcast([128, 16, 64])
            op(dve, lambda: nc.vector.tensor_tensor(out=wk[:, :, :], in0=ohv[:, :, :], in1=bcv(vb), op=ALU.mult), reads=[b_ohv, b_vb, b_wk], writes=[b_wk])
            op(dve, lambda: nc.vector.tensor_reduce(out=basev[:, :, :], in_=wk.rearrange("p b (k e) -> p b k e", k=2), axis=AX.X, op=ALU.add), reads=[b_wk], writes=[b_basev])
            op(dve, lambda: nc.vector.tensor_tensor(out=wk[:, :, :], in0=ohv[:, :, :], in1=bcv(ovst), op=ALU.mult), reads=[b_ohv, b_ovst, b_wk], writes=[b_wk])
            op(dve, lambda: nc.vector.tensor_reduce(out=ovs[:, :, :], in_=wk.rearrange("p b (k e) -> p b k e", k=2), axis=AX.X, op=ALU.add), reads=[b_wk], writes=[b_ovs])
            op(dve, lambda: nc.vector.tensor_scalar(out=isov[:, :, :], in0=rr[:, :, :], scalar1=float(CAP), scalar2=None, op0=ALU.is_ge), reads=[b_rr], writes=[b_isov])
            op(dve, lambda: nc.vector.tensor_tensor(out=ovs[:, :, :], in0=ovs[:, :, :], in1=basev[:, :, :], op=ALU.subtract), reads=[b_ovs, b_basev], writes=[b_ovs])
            op(dve, lambda: nc.vector.scalar_tensor_tensor(out=ovs[:, :, :], in0=ovs[:, :, :], scalar=float(8192 - CAP), in1=isov[:, :, :], op0=ALU.add, op1=ALU.mult),
               reads=[b_ovs, b_isov], writes=[b_ovs])
            op(dve, lambda: nc.vector.tensor_tensor(out=posf[:, :, :], in0=basev[:, :, :], in1=rr[:, :, :], op=ALU.add), reads=[b_basev, b_rr], writes=[b_posf])
            op(dve, lambda: nc.vector.tensor_tensor(out=posf[:, :, :], in0=posf[:, :, :], in1=ovs[:, :, :], op=ALU.add), reads=[b_posf, b_ovs], writes=[b_posf])
            op(dve, lambda: nc.vector.tensor_copy(out=pos_i[:, :, :], in_=posf[:, :, :]), reads=[b_posf], writes=[b_posi])
            srcv = srcm.rearrange("p (b k) c -> p b k c", k=2)
            op(dve, lambda: nc.vector.memset(srcm[:, :, :], 0.0), writes=[b_srcm])
            for kk in range(2):
                op(dve, lambda: nc.vector.tensor_copy(out=srcv[:, :, kk, 0], in_=tokid[:, :]), reads=[b_tok], writes=[b_srcm])
                op(dve, lambda: nc.vector.tensor_scalar(out=srcv[:, :, kk, 1], in0=tokid[:, :], scalar1=float(2048 * kk), scalar2=None, op0=ALU.add), reads=[b_tok], writes=[b_srcm])
                op(dve, lambda: nc.vector.tensor_copy(out=srcv[:, :, kk, 2], in_=(w1 if kk == 0 else w2)[:, :]), reads=[b_w1, b_w2], writes=[b_srcm])
            for tb in range(16):
                for kk in range(2):
                    op(pool, lambda: nc.gpsimd.indirect_dma_start(out=maps_d[:, :], out_offset=bass.IndirectOffsetOnAxis(ap=pos_i[:, tb, kk:kk + 1], axis=0),
                                                                  in_=srcv[:, tb, kk, :], in_offset=None, bounds_check=BND[NSLOT - 1], oob_is_err=False),
                       reads=[b_posi, b_srcm, b_mapinit], dma=b_scat)
            b_scat.w = (b_scat.dsem, b_scat.dn)
            op(dve, lambda: nc.vector.tensor_tensor(out=cmpj[:, :, :], in0=ovend[:, :].unsqueeze(1).to_broadcast([128, NOV, 64]),
                                                    in1=jidx[:, :].unsqueeze(2).to_broadcast([128, NOV, 64]), op=ALU.is_le), reads=[b_ovend, b_jidx], writes=[b_cmpj])
            op(dve, lambda: nc.vector.tensor_reduce(out=ovv[:, :], in_=cmpj[:, :, :], axis=AX.X, op=ALU.add), reads=[b_cmpj], writes=[b_ovv])
            op(dve, lambda: nc.vector.tensor_scalar(out=ovf[:, :], in0=ovv[:, :], scalar1=32.0, scalar2=-32.0, op0=ALU.is_ge, op1=ALU.mult), reads=[b_ovv], writes=[b_ovf])
            op(dve, lambda: nc.vector.tensor_tensor(out=ovf[:, :], in0=ovf[:, :], in1=ovv[:, :], op=ALU.add), reads=[b_ovf, b_ovv], writes=[b_ovf])
            op(dve, lambda: nc.vector.tensor_scalar(out=ovf[:, :], in0=ovf[:, :], scalar1=1.0, scalar2=128.0, op0=ALU.add, op1=ALU.mult), reads=[b_ovf], writes=[b_ovf])
            op(dve, lambda: nc.vector.tensor_scalar(out=ovf[:, :], in0=ovf[:, :], scalar1=pidx[:, 0:1], scalar2=None, op0=ALU.add), reads=[b_ovf, b_pidx], writes=[b_ovf])
            op(dve, lambda: nc.vector.tensor_scalar(out=ovv[:, :], in0=ovv[:, :], scalar1=64.0, scalar2=1.0e6, op0=ALU.is_ge, op1=ALU.mult), reads=[b_ovv], writes=[b_ovv])
            op(dve, lambda: nc.vector.tensor_tensor(out=ovf[:, :], in0=ovf[:, :], in1=ovv[:, :], op=ALU.add), reads=[b_ovf, b_ovv], writes=[b_ovf])
            op(dve, lambda: nc.vector.tensor_copy(out=ovidx[:, :], in_=ovf[:, :]), reads=[b_ovf], writes=[b_ovidx])
            op(dve, lambda: nc.vector.tensor_tensor(out=oh1[:, :, :], in0=oh1[:, :, :], in1=bc(w1, 32), op=ALU.mult), reads=[b_oh1, b_w1], writes=[b_oh1])
            op(dve, lambda: nc.vector.tensor_tensor(out=oh2[:, :, :], in0=oh2[:, :, :], in1=bc(w2, 32), op=ALU.mult), reads=[b_oh2, b_w2], writes=[b_oh2])
            op(dve, lambda: nc.vector.tensor_tensor(out=comb[:, :, 1:33], in0=oh1[:, :, :], in1=oh2[:, :, :], op=ALU.add), reads=[b_oh1, b_oh2, b_comb], writes=[b_comb])
            if dbg:
                dbg_out["comb"] = dout("dbg_comb", [128, 16, 33])
                bd = dbuf()
                op(sp, lambda: nc.sync.dma_start(out=dbg_out["comb"][:, :, :], in_=comb[:, :, :]), reads=[b_comb], writes=[bd], dma=bd)
                _wait(sp, bd.w)
            _wait(sp, b_yall.w)
            nc.all_engine_barrier()

        acc = at(RB[0], [128, 16, D], F32)
        b_acc = [Buf() for _ in range(16)]
        b_Ysc = dbuf()
        es = Bump(RF, RC, RD)
        NS = 4
        ra_b = Bump(RA)
        wgu = [SB(es if i < 2 else ra_b, "wgu%d" % i, [128, 8, 512], BF16) for i in range(NS)]
        wd = [SB(es if i < 2 else ra_b, "wd%d" % i, [128, 2, D], BF16) for i in range(NS)]
        b_wg = [dbuf() for _ in range(NS)]
        b_wg2 = [dbuf() for _ in range(NS)]
        b_wdn = [dbuf() for _ in range(NS)]

        def load_gu(e):
            s_ = e % NS
            rows = slice(e * 128, (e + 1) * 128)
            op(pool, lambda: nc.gpsimd.dma_start(out=wgu[s_][:, 0:4, :].rearrange("p c f -> p (c f)"), in_=wgu_lo[rows, :]), writes=[b_wg[s_]], dma=b_wg[s_])
            op(pool, lambda: nc.gpsimd.dma_start(out=wgu[s_][:, 4:8, :].rearrange("p c f -> p (c f)"), in_=wgu_hi[rows, :]), writes=[b_wg2[s_]], dma=b_wg2[s_])

        def load_d(e):
            s_ = e % NS
            rows = slice(e * 128, (e + 1) * 128)
            op(pool, lambda: nc.gpsimd.dma_start(out=wd[s_][:, :, :].rearrange("p j d -> p (j d)"), in_=wd_h[rows, :]), writes=[b_wdn[s_]], dma=b_wdn[s_])

        load_gu(0)
        load_d(0)
        with ExitStack() as pes:
            sg = [SB(es, "sg%d" % i, [128, 512], BF16) for i in range(2)]; b_sg = [Buf(), Buf()]
            hid = [SB(es, "hid%d" % i, [128, 2, 512], BF16) for i in range(2)]; b_hid = [Buf(), Buf()]
            ps_g = [PS(pes, "ps_g%d" % i, [128, 512]) for i in range(2)]; b_psg = [Buf(), Buf()]
            ps_u = [PS(pes, "ps_u%d" % i, [128, 512]) for i in range(2)]; b_psu = [Buf(), Buf()]
            ps_d = [PS(pes, "ps_d%d" % i, [128, D]) for i in range(2)]; b_psd = [Buf(), Buf()]

            def GU(t, hsl):
                tcs = slice(t * 512, (t + 1) * 512)
                rd = [b_wg[0], b_wg2[0]] + b_hn2[t * 4:(t + 1) * 4]
                for j in range(2):
                    op(pe, mm_acc(ps_g[j][:, :], lambda c: wgu[0][:, c, j * 128:(j + 1) * 128], lambda c: hn2T[:, c, tcs], 8), reads=rd, writes=[b_psg[j]])
                for j in range(2):
                    op(pe, mm_acc(ps_u[j][:, :], lambda c: wgu[0][:, c, 256 + j * 128:256 + (j + 1) * 128], lambda c: hn2T[:, c, tcs], 8), reads=rd, writes=[b_psu[j]])
                for j in range(2):
                    op(act, lambda: nc.scalar.activation(out=sg[j][:, :], in_=ps_g[j][:, :], func=AF.Silu), reads=[b_psg[j]], writes=[b_sg[j]])
                for j in range(2):
                    op(dve, lambda: nc.vector.tensor_tensor(out=hid[hsl][:, j, :], in0=ps_u[j][:, :], in1=sg[j][:, :], op=ALU.mult),
                       reads=[b_psu[j], b_sg[j]], writes=[b_hid[hsl]])

            def DOWN(t, hsl):
                for r in range(4):
                    tb = t * 4 + r
                    db = tb % 2

                    def f():
                        ins = None
                        for half in range(2):
                            hc = slice(half * 512, (half + 1) * 512)
                            for j in range(2):
                                ins = nc.tensor.matmul(ps_d[db][:, hc], lhsT=hid[hsl][:, j, r * 128:(r + 1) * 128], rhs=wd[0][:, j, hc], start=(j == 0), stop=(j == 1))
                        return ins
                    op(pe, f, reads=[b_hid[hsl], b_wdn[0]], writes=[b_psd[db]])
                    op(act, lambda: nc.scalar.activation(out=acc[:, tb, :], in_=ps_d[db][:, :], func=AF.Copy), reads=[b_psd[db]], writes=[b_acc[tb]])

            for t in range(4):
                GU(t, t % 2)
                if t > 0:
                    DOWN(t - 1, (t - 1) % 2)
            DOWN(3, 1)
            _wait(sp, b_scat.w)
            op(sp, lambda: nc.sync.dma_start(out=mapt[:, :, :], in_=maps_d.rearrange("(s p) c -> p s c", p=128)), reads=[b_scat], writes=[b_mapt], dma=b_mapt)
            op(dve, lambda: nc.vector.tensor_copy(out=gidx[:, :], in_=mapt[:, :, 0]), reads=[b_mapt], writes=[b_gidx])
            op(dve, lambda: nc.vector.tensor_copy(out=sidx[:, :], in_=mapt[:, :, 1]), reads=[b_mapt], writes=[b_sidx])
            nc.all_engine_barrier()

        with ExitStack() as pes:
            ident_b, b_idb = load(es, "ident_b", ident_d[:, :], [128, 128], BF16, cast=True)
            NOVS = 3
            wgu_ov = [SB(es, "wgu_ov%d" % i, [128, 8, 512], BF16) for i in range(NOVS)]
            wd_ov = [SB(es, "wd_ov%d" % i, [128, 2, D], BF16) for i in range(NOVS)]
            b_wgov = [dbuf() for _ in range(NOVS)]
            b_wgov2 = [dbuf() for _ in range(NOVS)]
            b_wdov = [dbuf() for _ in range(NOVS)]
            for i in range(NOVS):
                op(dve, lambda: nc.vector.memset(wgu_ov[i][:], 0.0), writes=[b_wgov[i], b_wgov2[i]])
                op(dve, lambda: nc.vector.memset(wd_ov[i][:], 0.0), writes=[b_wdov[i]])
            NXG = 4
            xg = [SB(es, "xg%d" % i, [128, D], BF16) for i in range(NXG)]; b_xg = [dbuf() for _ in range(NXG)]
            for i in range(NXG):
                op(dve, lambda: nc.vector.memset(xg[i][:], 0.0), writes=[b_xg[i]])
            xT = [SB(es, "xT%d" % i, [128, 8, 128], BF16) for i in range(3)]; b_xT = [Buf() for _ in range(3)]
            sgt = [SB(es, "sgt%d" % i, [128, 256], BF16) for i in range(2)]; b_sgt = [Buf(), Buf()]
            hidt = [SB(es, "hidt%d" % i, [128, 256], BF16) for i in range(3)]; b_hidt = [Buf() for _ in range(3)]
            hT = [SB(es, "hT%d" % i, [128, 2, 128], BF16) for i in range(3)]; b_hT = [Buf() for _ in range(3)]
            yo = [SB(es, "yo%d" % i, [128, D], F32) for i in range(2)]; b_yo = [Buf(), Buf()]
            ps_xT = [PS(pes, "ps_xT%d" % i, [128, 8, 128], BF16) for i in range(2)]; b_pxT = [Buf(), Buf()]
            ps_gu = [PS(pes, "ps_gu%d" % i, [128, 512]) for i in range(2)]; b_pgu = [Buf(), Buf()]
            ps_hT = PS(pes, "ps_hT", [128, 2, 128], BF16); b_phT = Buf()
            ps_dn = PS(pes, "ps_dn", [128, D]); b_pdn = Buf()

            tiles = [(k_ * 32 + e_, e_ + 1, None) for e_ in range(NEXP) for k_ in range(2)] + [(64 + j_, None, j_) for j_ in range(NOV)]
            NTL = len(tiles)
            def load_gu_fast(e):
                s_ = e % NS
                rows = slice(e * 128, (e + 1) * 128)
                op(sp, lambda: nc.sync.dma_start(out=wgu[s_][:, 0:4, :].rearrange("p c f -> p (c f)"), in_=wbf_gu[rows, 0:2048]), reads=b_outd, writes=[b_wg[s_]], dma=b_wg[s_])
                op(sp, lambda: nc.sync.dma_start(out=wgu[s_][:, 4:8, :].rearrange("p c f -> p (c f)"), in_=wbf_gu[rows, 2048:4096]), reads=b_outd, writes=[b_wg2[s_]], dma=b_wg2[s_])

            def load_d_fast(e):
                s_ = e % NS
                rows = slice(e * 128, (e + 1) * 128)
                op(sp, lambda: nc.sync.dma_start(out=wd[s_][:, :, :].rearrange("p j d -> p (j d)"), in_=wbf_d[rows, :]), reads=b_outd, writes=[b_wdn[s_]], dma=b_wdn[s_])

            def wgu_of(ti):
                scol, e_st, j_ov = tiles[ti]
                if e_st is not None:
                    return wgu[e_st % NS], [b_wg[e_st % NS], b_wg2[e_st % NS]]
                return wgu_ov[j_ov % NOVS], [b_wgov[j_ov % NOVS], b_wgov2[j_ov % NOVS]]

            def wd_of(ti):
                scol, e_st, j_ov = tiles[ti]
                if e_st is not None:
                    return wd[e_st % NS], b_wdn[e_st % NS]
                return wd_ov[j_ov % NOVS], b_wdov[j_ov % NOVS]

            def stage_G(ti):
                scol, e_st, j_ov = tiles[ti]
                if e_st is not None:
                    if scol < 32:
                        load_gu_fast(e_st)
                else:
                    W_gu, bW = wgu_of(ti)
                    ioa = bass.IndirectOffsetOnAxis(ap=ovidx[:, j_ov:j_ov + 1], axis=0)
                    op(pool, lambda: nc.gpsimd.indirect_dma_start(out=W_gu[:, :, :].rearrange("p c f -> p (c f)"), out_offset=None, in_=wbf_gu[:, :], in_offset=ioa,
                                                                  bounds_check=BND[NROW - 1], oob_is_err=False),
                       reads=[b_ovidx] + b_outd, writes=[bW[0], bW[1]], dma=bW[0])

            def stage_Gx(ti):
                scol = tiles[ti][0]
                g4 = ti % NXG
                op(pool, lambda: nc.gpsimd.indirect_dma_start(out=xg[g4][:, :], out_offset=None, in_=hn2d[:, :],
                                                              in_offset=bass.IndirectOffsetOnAxis(ap=gidx[:, scol:scol + 1], axis=0), bounds_check=BND[2048], oob_is_err=False),
                   reads=[b_gidx], writes=[b_xg[g4]], dma=b_xg[g4])

            def stage_G2(ti):
                scol, e_st, j_ov = tiles[ti]
                if e_st is not None:
                    if scol < 32:
                        load_d_fast(e_st)
                else:
                    W_d, bW = wd_of(ti)
                    ioa = bass.IndirectOffsetOnAxis(ap=ovidx[:, j_ov:j_ov + 1], axis=0)
                    op(pool, lambda: nc.gpsimd.indirect_dma_start(out=W_d[:, :, :].rearrange("p j d -> p (j d)"), out_offset=None, in_=wbf_d[:, :], in_offset=ioa,
                                                                  bounds_check=BND[NROW - 1], oob_is_err=False),
                       reads=[b_ovidx], writes=[bW], dma=bW)

            def stage_A(ti):
                g4, p2, x3 = ti % NXG, ti % 2, ti % 3

                def ftr():
                    ins = None
                    for c in range(8):
                        ins = nc.tensor.transpose(ps_xT[p2][:, c, :], xg[g4][:, c * 128:(c + 1) * 128], ident_b[:, :])
                    return ins
                op(pe, ftr, reads=[b_xg[g4], b_idb], writes=[b_pxT[p2]])
                op(act, lambda: nc.scalar.activation(out=xT[x3][:, :, :], in_=ps_xT[p2][:, :, :], func=AF.Copy), reads=[b_pxT[p2]], writes=[b_xT[x3]])

            def stage_B(ti):
                p2, x3 = ti % 2, ti % 3
                W_gu, bW = wgu_of(ti)
                op(pe, mm_acc(ps_gu[p2][:, :], lambda c: xT[x3][:, c, :], lambda c: W_gu[:, c, :], 8), reads=[b_xT[x3]] + bW, writes=[b_pgu[p2]])
                op(act, lambda: nc.scalar.activation(out=sgt[p2][:, :], in_=ps_gu[p2][:, 0:256], func=AF.Silu), reads=[b_pgu[p2]], writes=[b_sgt[p2]])
                op(dve, lambda: nc.vector.tensor_tensor(out=hidt[x3][:, :], in0=ps_gu[p2][:, 256:512], in1=sgt[p2][:, :], op=ALU.mult),
                   reads=[b_pgu[p2], b_sgt[p2]], writes=[b_hidt[x3]])

            def stage_C(ti):
                x3 = ti % 3

                def ftr2():
                    ins = None
                    for j in range(2):
                        ins = nc.tensor.transpose(ps_hT[:, j, :], hidt[x3][:, j * 128:(j + 1) * 128], ident_b[:, :])
                    return ins
                op(pe, ftr2, reads=[b_hidt[x3], b_idb], writes=[b_phT])
                op(dve, lambda: nc.vector.tensor_copy(out=hT[x3][:, :, :], in_=ps_hT[:, :, :]), reads=[b_phT], writes=[b_hT[x3]])

            def stage_D(ti):
                scol = tiles[ti][0]
                p2, x3 = ti % 2, ti % 3
                W_d, bW = wd_of(ti)

                def fdn():
                    ins = None
                    for half in range(2):
                        hc = slice(half * 512, (half + 1) * 512)
                        for j in range(2):
                            ins = nc.tensor.matmul(ps_dn[:, hc], lhsT=hT[x3][:, j, :], rhs=W_d[:, j, hc], start=(j == 0), stop=(j == 1))
                    return ins
                op(pe, fdn, reads=[b_hT[x3], bW], writes=[b_pdn])
                op(act, lambda: nc.scalar.activation(out=yo[p2][:, :], in_=ps_dn[:, :], func=AF.Copy, scale=mapt[:, scol, 2:3]),
                   reads=[b_pdn, b_mapt], writes=[b_yo[p2]])
                op(pool, lambda: nc.gpsimd.indirect_dma_start(out=Yd[:, :], out_offset=bass.IndirectOffsetOnAxis(ap=sidx[:, scol:scol + 1], axis=0),
                                                              in_=yo[p2][:, :], in_offset=None, bounds_check=BND[4095], oob_is_err=False),
                   reads=[b_yo[p2], b_sidx], dma=b_Ysc)

            stage_Gx(0)
            stage_Gx(1)
            stage_Gx(2)
            stage_G(0)
            stage_G(1)
            for it in range(NTL + 3):
                if it + 3 < NTL:
                    stage_Gx(it + 3)
                if it < NTL:
                    stage_A(it)
                if 0 <= it - 1 < NTL:
                    stage_B(it - 1)
                if 0 <= it - 2 < NTL:
                    stage_C(it - 2)
                if 0 <= it - 3 < NTL:
                    stage_D(it - 3)
                if it + 2 < NTL:
                    stage_G(it + 2)
                if it < NTL:
                    stage_G2(it)
            b_Ysc.w = (b_Ysc.dsem, b_Ysc.dn)
            _wait(pool, b_Ysc.w)
            nc.all_engine_barrier()

        b_yout = dbuf()
        with ExitStack() as pes:
            es = Bump(RF, RC, RD)
            g_po, b_gpo = load(es, "g_po_bc", g_po_d[0:1, :].partition_broadcast(128), [128, D])
            hf = [SB(es, "hf%d" % i, [128, D], F32) for i in range(3)]; b_hf = [dbuf() for _ in range(3)]
            y0 = [SB(es, "y0%d" % i, [128, D], F32) for i in range(3)]; b_y0 = [dbuf() for _ in range(3)]
            y1 = [SB(es, "y1%d" % i, [128, D], F32) for i in range(3)]; b_y1 = [dbuf() for _ in range(3)]

            def load_fin(tb_):
                k3 = tb_ % 3
                op(sp, lambda: nc.sync.dma_start(out=hf[k3][:, :], in_=y[tb_]), reads=[b_yall], writes=[b_hf[k3]], dma=b_hf[k3])
                op(sp, lambda: nc.sync.dma_start(out=y0[k3][:, :], in_=Yd[tb_ * 128:(tb_ + 1) * 128, :]), reads=[b_Ysc], writes=[b_y0[k3]], dma=b_y0[k3])
                op(sp, lambda: nc.sync.dma_start(out=y1[k3][:, :], in_=Yd[2048 + tb_ * 128:2048 + (tb_ + 1) * 128, :]), reads=[b_Ysc], writes=[b_y1[k3]], dma=b_y1[k3])
            load_fin(0)
            load_fin(1)
            oo = [SB(es, "oo%d" % i, [128, D], F32) for i in range(2)]; b_oo = [Buf(), Buf()]
            junk = SB(es, "f_junk", [128, D], BF16); b_junk = Buf()
            sm = SB(es, "f_sm", [128, 4], F32); b_s0 = Buf(); b_s1 = Buf()
            for tb in range(16):
                k = tb % 2
                k3 = tb % 3
                if tb + 2 < 16:
                    load_fin(tb + 2)
                op(dve, lambda: nc.vector.tensor_tensor(out=y0[k3][:, :], in0=y0[k3][:, :], in1=y1[k3][:, :], op=ALU.add), reads=[b_y0[k3], b_y1[k3]], writes=[b_y0[k3]])
                op(dve, lambda: nc.vector.tensor_tensor(out=acc[:, tb, :], in0=acc[:, tb, :], in1=y0[k3][:, :], op=ALU.add), reads=[b_acc[tb], b_y0[k3]], writes=[b_acc[tb]])
                op(act, lambda: nc.scalar.activation(out=junk[:, :], in_=acc[:, tb, :], func=AF.Square, accum_out=sm[:, 0:1]),
                   reads=[b_acc[tb]], writes=[b_junk, b_s0])
                op(act, lambda: nc.scalar.activation(out=sm[:, 1:2], in_=sm[:, 0:1], func=AF.Sqrt, scale=1.0 / D, bias=eps_t[:, 0:1]),
                   reads=[b_s0, b_eps], writes=[b_s1])
                op(dve, lambda: nc.vector.reciprocal(out=sm[:, 1:2], in_=sm[:, 1:2]), reads=[b_s1], writes=[b_s1])
                op(dve, lambda: nc.vector.scalar_tensor_tensor(out=oo[k][:, :], in0=acc[:, tb, :], scalar=sm[:, 1:2], in1=g_po[:, :], op0=ALU.mult, op1=ALU.mult),
                   reads=[b_acc[tb], b_s1, b_gpo], writes=[b_oo[k]])
                op(dve, lambda: nc.vector.tensor_tensor(out=oo[k][:, :], in0=oo[k][:, :], in1=hf[k3][:, :], op=ALU.add), reads=[b_oo[k], b_hf[k3]], writes=[b_oo[k]])
                op(sp, lambda: nc.sync.dma_start(out=y[tb], in_=oo[k][:, :]), reads=[b_oo[k]], writes=[b_yout], dma=b_yout)
            _wait(sp, b_yout.w)
    return nc, dbg_out


def _prep_shared(inp):
    f = np.float32
    def cp(a):
        return np.ascontiguousarray(a, dtype=f)
    w_in = cp(inp["w_in"][0].reshape(8, 128, 1952).transpose(1, 0, 2))
    w_uq = cp(inp["w_uq"][0].reshape(2, 128, 768).transpose(1, 0, 2))
    w_ukv = cp(inp["w_ukv"][0])
    w_out = inp["w_out"][0]
    w_out_a = cp(w_out[:512].reshape(4, 128, D).transpose(1, 0, 2))
    w_out_c = cp(w_out[512:].reshape(4, 128, D).transpose(1, 0, 2))
    wr = np.concatenate([inp["w_group_router"][0], inp["w_expert_router"][0]], axis=1)
    wr = cp(wr.reshape(8, 128, 36).transpose(1, 0, 2))
    br = cp(np.concatenate([inp["b_group_router"][0], inp["b_expert_router"][0]])[None, :])
    w_gate = np.concatenate([inp["w_sh_gate"], inp["w_gate"][0]], axis=0)
    w_up = np.concatenate([inp["w_sh_up"], inp["w_up"][0]], axis=0)
    w_down = np.concatenate([inp["w_sh_down"], inp["w_down"][0]], axis=0)
    gu = np.empty((NEXP + 1, 128, 8, 512), f)
    gu[:, :, :, 0:256] = w_gate.reshape(NEXP + 1, 8, 128, 256).transpose(0, 2, 1, 3)
    gu[:, :, :, 256:512] = w_up.reshape(NEXP + 1, 8, 128, 256).transpose(0, 2, 1, 3)
    wgu_lo = cp(gu[:, :, 0:4].reshape((NEXP + 1) * 128, 2048))
    wgu_hi = cp(gu[:, :, 4:8].reshape((NEXP + 1) * 128, 2048))
    wd_h = cp(w_down.reshape(NEXP + 1, 2, 128, D).transpose(0, 2, 1, 3).reshape((NEXP + 1) * 128, 2048))
    triu = np.triu(np.ones((128, 128), f))
    vbase = (np.arange(64, dtype=f) * 128)[None, :]
    tokid = (np.arange(16, dtype=f)[None, :] * 128 + np.arange(128, dtype=f)[:, None])
    pidx = np.arange(128, dtype=f)[:, None]
    jidx = np.arange(NOV, dtype=f)[None, :]
    mapinit = np.zeros((NSLOT, 4), f); mapinit[:, 0] = 2048.0; mapinit[:, 1] = 1.0e6
    def pc(v, p):
        return cp(v.reshape(-1, p).T)
    ident = np.eye(128, dtype=f)
    rot = np.zeros((96, 96), f)
    for i in range(16):
        rot[64 + i + 16, 64 + i] = -1.0
        rot[64 + i, 64 + i + 16] = 1.0
    sel = np.zeros((65, 64), f); sel[64, :] = 1.0
    inv_freq = (1.0 / (10000.0 ** (np.arange(0, 32, 2, dtype=np.float32) / np.float32(32)))).astype(f)
    invf = np.zeros((96, 1), f); invf[64:80, 0] = inv_freq; invf[80:96, 0] = inv_freq
    dmask = np.zeros((128, 4, 512), f)
    for d_ in range(4):
        dmask[:, d_, :] = (np.arange(512)[None, :] >= (np.arange(128)[:, None] + 128 * d_)).astype(f)
    prefix = np.concatenate([np.zeros(112, np.int32), np.arange(16, dtype=np.int32)])[None, :]
    return dict(
        w_in=w_in, w_uq=w_uq, w_ukv=w_ukv, w_out_a=w_out_a, w_out_c=w_out_c, wr=wr, br=br,
        wgu_lo=wgu_lo, wgu_hi=wgu_hi, wd_h=wd_h, triu=cp(triu), vbase=cp(vbase), tokid=cp(tokid), pidx=cp(pidx), jidx=cp(jidx), mapinit=mapinit,
        g_pfb=cp(inp["pre_ffn_norm"][0][None, :]),
        g_pre=pc(inp["pre_mix_norm"][0], 128), g_q=pc(inp["q_norm"][0], 128), g_kv=pc(inp["kv_norm"][0], 128),
        g_a=pc(inp["attn_out_norm"][0], 128), g_c=pc(inp["conv_out_norm"][0], 128),
        convw=cp(inp["conv_w"][0].T.reshape(4, 128, 3).transpose(1, 0, 2)),
        g_pm=cp(inp["post_mix_norm"][0][None, :]), g_pf=pc(inp["pre_ffn_norm"][0], 128), g_po=cp(inp["post_ffn_norm"][0][None, :]),
        ident=ident, rot96=rot, invf=invf, dmask=dmask, prefix=prefix,
    )


def groups_of(j):
    return [j, 7 - j, 8 + j, 15 - j]


def _prep_core(inp, core, shared, batch_cache):
    b, j = core // 4, core % 4
    f = np.float32
    x = inp["x"]
    if b not in batch_cache:
        xb = np.asarray(x[b], dtype=f)
        hTm = np.ascontiguousarray(xb.reshape(16, 512, 8, 128).transpose(0, 3, 2, 1))
        h0 = np.zeros((128, D), f)
        h0[112:] = np.asarray(inp["meta_tokens"], dtype=f)
        hT0 = np.ascontiguousarray(h0.reshape(128, 8, 128).transpose(2, 1, 0))
        pos = np.ascontiguousarray(np.asarray(inp["positions"][b], dtype=np.int32)[None, :])
        batch_cache[b] = (hTm, hT0, pos)
    hTm, hT0, pos = batch_cache[b]
    Gs = groups_of(j)
    hq = np.empty((4, 128, 8, 514), f)
    posq = np.empty((4, 512), np.int32)
    xown = np.empty((16, 128, D), f)
    order = []
    abias = np.zeros((128, 12), f)
    for s_, G in enumerate(Gs):
        others = [g for g in range(4 * s_, 4 * s_ + 4) if g != G]
        emp = [g for g in others if g > G]
        ful = [g for g in others if g < G]
        reg = emp + ful + [G]
        for r_, g in enumerate(reg[:3]):
            abias[:, s_ * 3 + r_] = -30000.0 if g > G else 0.0
        order += reg
    hTm_c = np.ascontiguousarray(hTm[order])
    pos_c = np.ascontiguousarray(pos.reshape(16, 512)[order].reshape(1, SEQ))
    for gi, G in enumerate(Gs):
        hq[gi, :, :, 2:] = hTm[G]
        hq[gi, :, :, 0:2] = hTm[G - 1][:, :, 510:512] if G > 0 else hT0[:, :, 126:128]
        posq[gi] = pos[0, 512 * G:512 * G + 512]
        xown[gi * 4:(gi + 1) * 4] = np.asarray(x[b, 512 * G:512 * G + 512], dtype=f).reshape(4, 128, D)
    m = dict(shared)
    m.update(hT0=hT0, hTm=hTm_c, hq=hq, pos=pos_c, posq=posq, xown=xown, abias=abias)
    return m


def kernel(**inputs):
    dbg = bool(os.environ.get("MK_DEBUG"))
    inp = {k: np.asarray(v) for k, v in inputs.items()}
    shared = _prep_shared(inp)
    cache = {}
    in_maps = [_prep_core(inp, c, shared, cache) for c in range(8)]
    nc, dbg_out = build_program(dbg=dbg)
    res = run_bass_kernel_spmd(nc, in_maps, core_ids=list(range(8)))
    out = np.empty((2, SEQ, D), np.float32)
    for c in range(8):
        b, j = c // 4, c % 4
        yv = np.asarray(res.results[c]["y"])
        for gi, G in enumerate(groups_of(j)):
            out[b, 512 * G:512 * G + 512] = yv[gi * 4:(gi + 1) * 4].reshape(512, D)
    if dbg:
        kernel.dbg = [{k: np.asarray(res.results[c]["dbg_" + k]) for k in dbg_out} for c in range(8)]
    return out
```

```python
import os
from contextlib import ExitStack

import numpy as np
import concourse.bass as bass
import concourse.mybir as mybir
from concourse.bass_utils import run_bass_kernel_spmd

F32 = mybir.dt.float32
BF16 = mybir.dt.bfloat16
I32 = mybir.dt.int32
AF = mybir.ActivationFunctionType
ALU = mybir.AluOpType
AX = mybir.AxisListType

D = 1024
SEQ = 8192
NCH = 17
P_TOT = 128 + 16 * 512
NKB = 65
EXT = [17, 33, 49, 65]
UNC0 = [1, 17, 33, 49]
SCALE = float((64 + 32) ** -0.5)
EPS = 1e-6
MAGIC = 12582912.0
TWO_PI = 6.283185307179586
NEXP = 32
CAP = int(os.environ.get('MK_CAP', '128'))
NOV = 30
NSLOT = 64 * 128 + NOV * 128


class Q:
    def __init__(self, eng, sem):
        self.eng, self.sem, self.n, self.seen = eng, sem, 0, {}


class Buf:
    __slots__ = ("w", "rs", "dsem", "dn")

    def __init__(self, dsem=None):
        self.w, self.rs, self.dsem, self.dn = None, {}, dsem, 0


def _wait(q, tok):
    if tok is None:
        return
    sem, v = tok
    k = id(sem)
    if q.seen.get(k, 0) >= v:
        return
    q.eng.wait_ge(sem, v)
    q.seen[k] = v


_LD = {}


def _finish_loads():
    for st in _LD.values():
        ld = st["buf"]
        if st["pending"]:
            for b in st["pending"]:
                b.w = (ld.dsem, ld.dn)
            st["pending"] = []
            st["last_total"] = ld.dn
            st["waited"] = set()


def _ld_pending():
    return any(st["pending"] for st in _LD.values())


def _is_ld_buf(d):
    return any(d is st["buf"] for st in _LD.values())


def op(q, fn, reads=(), writes=(), dma=None):
    if _ld_pending() and not (dma is not None and _is_ld_buf(dma)):
        _finish_loads()
    for b in reads:
        _wait(q, b.w)
    for b in writes:
        _wait(q, b.w)
        for k, t in b.rs.items():
            _wait(q, t)
    ins = fn()
    if dma is not None:
        ins.then_inc(dma.dsem, 16)
        dma.dn += 16
        tok = (dma.dsem, dma.dn)
    else:
        ins.then_inc(q.sem, 1)
        q.n += 1
        tok = (q.sem, q.n)
    for b in reads:
        k = id(tok[0])
        if b.rs.get(k, (None, 0))[1] < tok[1]:
            b.rs[k] = tok
    for b in writes:
        b.w = tok
        b.rs = {}
    return tok


def build_program(dbg=False):
    nc = bass.Bass("TRN2", target_bir_lowering=False)

    def din(name, shape, dt=F32):
        return nc.dram_tensor(name, list(shape), dt, kind="ExternalInput").ap()

    def dout(name, shape, dt=F32):
        return nc.dram_tensor(name, list(shape), dt, kind="ExternalOutput").ap()

    hT0 = din("hT0", [128, 8, 128])
    hTm = din("hTm", [16, 128, 8, 512])
    hq = din("hq", [4, 128, 8, 514])
    posrow = din("pos", [1, SEQ], I32)
    posq = din("posq", [4, 512], I32)
    xown = din("xown", [16, 128, D])
    abias_d = din("abias", [128, 12])
    w_in = din("w_in", [128, 8, 1952])
    w_uq = din("w_uq", [128, 2, 768])
    w_ukv = din("w_ukv", [128, 1024])
    w_out_a = din("w_out_a", [128, 4, D])
    w_out_c = din("w_out_c", [128, 4, D])
    wr_d = din("wr", [128, 8, 36])
    br_d = din("br", [1, 36])
    NROW = (NEXP + 1) * 128
    wgu_lo = din("wgu_lo", [NROW, 2048])
    wgu_hi = din("wgu_hi", [NROW, 2048])
    wd_h = din("wd_h", [NROW, 2048])
    triu_d = din("triu", [128, 128])
    vb_d = din("vbase", [1, 64])
    tokid_d = din("tokid", [128, 16])
    pidx_d = din("pidx", [128, 1])
    jidx_d = din("jidx", [1, NOV])
    mapinit_d = din("mapinit", [NSLOT, 4])
    g_pfb_d = din("g_pfb", [1, D])
    wbf_gu = nc.dram_tensor("wbf_gu", [NROW, 4096], BF16, kind="Internal").ap()
    wbf_d = nc.dram_tensor("wbf_d", [NROW, 2048], BF16, kind="Internal").ap()
    hn2d = nc.dram_tensor("hn2d", [2049, D], BF16, kind="Internal").ap()
    maps_d = nc.dram_tensor("maps", [NSLOT, 4], F32, kind="Internal").ap()
    Yd = nc.dram_tensor("Yd", [4096, D], F32, kind="Internal").ap()
    g_pre_d = din("g_pre", [128, 8])
    g_q_d = din("g_q", [128, 2])
    g_kv_d = din("g_kv", [128, 1])
    g_a_d = din("g_a", [128, 4])
    g_c_d = din("g_c", [128, 4])
    convw_d = din("convw", [128, 4, 3])
    g_pm_d = din("g_pm", [1, D])
    g_pf_d = din("g_pf", [128, 8])
    g_po_d = din("g_po", [1, D])
    ident_d = din("ident", [128, 128])
    rot_d = din("rot96", [96, 96])
    invf_d = din("invf", [96, 1])
    dmask_d = din("dmask", [128, 4, 512])
    prefix_d = din("prefix", [1, 128], I32)
    y = dout("y", [16, 128, D])
    dbg_out = {}

    with ExitStack() as top:
        def sem(name):
            return top.enter_context(nc.semaphore(name))

        pe = Q(nc.tensor, sem("s_pe"))
        act = Q(nc.scalar, sem("s_act"))
        dve = Q(nc.vector, sem("s_dve"))
        pool = Q(nc.gpsimd, sem("s_pool"))
        sp = Q(nc.sync, sem("s_sp"))
        nsem = [0]

        def dbuf():
            nsem[0] += 1
            return Buf(sem("d%d" % nsem[0]))

        ARENA_BYTES = 210000
        arena = top.enter_context(nc.sbuf_tensor("arena", [128, ARENA_BYTES], mybir.dt.uint8))
        RA = (0, 32768)
        RB = (32768, 98304)
        RC = (98304, 131072)
        RD = (131072, 147456)
        RE = (147456, 155648)
        RF = (155648, ARENA_BYTES)

        class Bump:
            def __init__(self, *segs):
                self.segs = [list(sg) for sg in segs]

            def alloc(self, nbytes):
                nbytes = (nbytes + 63) // 64 * 64
                for sg in self.segs:
                    if sg[1] - sg[0] >= nbytes:
                        off = sg[0]
                        sg[0] += nbytes
                        return off
                raise RuntimeError("arena exhausted for %d bytes: %s" % (nbytes, self.segs))

        DTB = {F32: 4, BF16: 2, I32: 4}

        def SB(es, name, shape, dt):
            shape = list(shape)
            n = 1
            for d_ in shape[1:]:
                n *= d_
            off = es.alloc(n * DTB[dt])
            v = arena[0:shape[0], off:off + n * DTB[dt]].bitcast(dt)
            if len(shape) == 3:
                v = v.rearrange("p (a b) -> p a b", a=shape[1])
            elif len(shape) != 2:
                raise ValueError(shape)
            return v

        def at(region_off, shape, dt):
            return SB(Bump((region_off, ARENA_BYTES)), "", shape, dt)

        def PS(pes, name, shape, dt=F32):
            return pes.enter_context(nc.psum_tensor("ps_" + name, list(shape), dt))

        _LD.clear()
        for _k in ("sp", "pool"):
            _LD[_k] = dict(pending=[], buf=dbuf(), last_total=0, waited=set())
        BND = {}
        for _v in (NSLOT - 1, NROW - 1, 2048, 4095):
            BND[_v] = nc.gpsimd.alloc_register("bnd%d" % _v)
            nc.gpsimd.reg_mov(BND[_v], _v)

        def load(es, name, src, shape, dt=F32, q=None, cast=False):
            t = SB(es, name, shape, dt)
            b = Buf()
            qq = pool if cast else (q or sp)
            st = _LD["pool" if qq is pool else "sp"]
            ld = st["buf"]
            if id(qq) not in st["waited"]:
                _wait(qq, (ld.dsem, st["last_total"]) if st["last_total"] else None)
                st["waited"].add(id(qq))
            op(qq, lambda: qq.eng.dma_start(out=t[:], in_=src), writes=[b], dma=ld)
            st["pending"].append(b)
            return t, b

        cst = Bump(RE)
        ones_bf = SB(cst, "ones_bf", [128, 128], BF16)
        b_ones = Buf()
        op(pool, lambda: nc.gpsimd.memset(ones_bf[:], 1.0), writes=[b_ones])
        rot_t, b_rot = load(cst, "rot_t", rot_d[:, :], [96, 96])
        invf_t, b_invf = load(cst, "invf_t", invf_d[:, :], [96, 1])

        QT = at(RA[0], [96, 8, 2048], BF16)
        b_QT = [[Buf() for _ in range(4)] for _ in range(8)]
        convnT = at(RD[0], [128, 4, 2048], BF16)
        b_convn = [Buf() for _ in range(4)]

        def emit_rope(es_tiles, src_ap, width, add16, tagbufs):
            pos_i, ang, kf, cs, sn, b_pos, b_ang, b_kf, b_cos, b_sin = tagbufs
            R = slice(64, 96)
            op(sp, lambda: nc.sync.dma_start(out=pos_i[R, 0:width], in_=src_ap.partition_broadcast(32)),
               writes=[b_pos], dma=b_pos)
            op(dve, lambda: nc.vector.tensor_scalar(out=ang[R, 0:width], in0=pos_i[R, 0:width], scalar1=float(add16),
                                                    scalar2=invf_t[R, 0:1], op0=ALU.add, op1=ALU.mult),
               reads=[b_pos, b_invf], writes=[b_ang])
            op(dve, lambda: nc.vector.tensor_scalar(out=kf[R, 0:width], in0=ang[R, 0:width], scalar1=1.0 / TWO_PI,
                                                    scalar2=MAGIC, op0=ALU.mult, op1=ALU.add),
               reads=[b_ang], writes=[b_kf])
            op(dve, lambda: nc.vector.tensor_scalar(out=kf[R, 0:width], in0=kf[R, 0:width], scalar1=-MAGIC,
                                                    scalar2=-TWO_PI, op0=ALU.add, op1=ALU.mult),
               reads=[b_kf], writes=[b_kf])
            op(dve, lambda: nc.vector.tensor_tensor(out=ang[R, 0:width], in0=ang[R, 0:width], in1=kf[R, 0:width], op=ALU.add),
               reads=[b_ang, b_kf], writes=[b_ang])
            op(dve, lambda: nc.vector.tensor_scalar(out=ang[R, 0:width], in0=ang[R, 0:width], scalar1=-3.1415925,
                                                    scalar2=3.1415925, op0=ALU.max, op1=ALU.min),
               reads=[b_ang], writes=[b_ang])
            op(act, lambda: nc.scalar.activation(out=sn[R, 0:width], in_=ang[R, 0:width], func=AF.Sin),
               reads=[b_ang], writes=[b_sin])
            op(act, lambda: nc.scalar.activation(out=kf[R, 0:width], in_=ang[R, 0:width], func=AF.Sin, scale=0.5),
               reads=[b_ang], writes=[b_kf])
            op(dve, lambda: nc.vector.tensor_tensor(out=kf[R, 0:width], in0=kf[R, 0:width], in1=kf[R, 0:width], op=ALU.mult),
               reads=[b_kf], writes=[b_kf])
            op(dve, lambda: nc.vector.tensor_scalar(out=cs[R, 0:width], in0=kf[R, 0:width], scalar1=-2.0,
                                                    scalar2=1.0, op0=ALU.mult, op1=ALU.add),
               reads=[b_kf], writes=[b_cos])

        def rstd_from_psum(ps_t, b_ps, rows, width, inv_n, tmp, b_tmp, out_t, b_out):
            op(act, lambda: nc.scalar.activation(out=tmp[0:rows, 0:width], in_=ps_t[0:rows, 0:width], func=AF.Sqrt,
                                                 scale=float(inv_n), bias=eps_t[0:rows, 0:1]),
               reads=[b_ps, b_eps], writes=[b_tmp])
            op(dve, lambda: nc.vector.reciprocal(out=out_t[0:rows, 0:width], in_=tmp[0:rows, 0:width]),
               reads=[b_tmp], writes=[b_out])

        eps_t = SB(cst, "eps_t", [128, 1], F32)
        b_eps = Buf()
        op(pool, lambda: nc.gpsimd.memset(eps_t[:], EPS), writes=[b_eps])

        def mm_acc(ps_ap, lhs_fn, rhs_fn, nk):
            def f():
                ins = None
                for c in range(nk):
                    ins = nc.tensor.matmul(ps_ap, lhsT=lhs_fn(c), rhs=rhs_fn(c), start=(c == 0), stop=(c == nk - 1))
                return ins
            return f

        def load_scaled_w(es, name, src_fn, ncols, g_t, b_g, stage, b_stage):
            t = SB(es, name, [128, 8, ncols], BF16)
            b = Buf()
            for c in range(8):
                k = c % 2
                op(sp, lambda: nc.sync.dma_start(out=stage[k][:, 0:ncols], in_=src_fn(c)), writes=[b_stage[k]], dma=b_stage[k])
                op(dve, lambda: nc.vector.tensor_scalar(out=t[:, c, :], in0=stage[k][:, 0:ncols], scalar1=g_t[:, c:c + 1], scalar2=None, op0=ALU.mult),
                   reads=[b_stage[k], b_g], writes=[b])
            return t, b

        with ExitStack() as pes:
            es = Bump(RF, RB, RC)
            g_pre, b_gpre = load(es, "g_pre", g_pre_d[:, :], [128, 8])
            convf = SB(es, "q_convf", [128, 4, 512], F32); b_convf = Buf()
            _stg = convf.rearrange("p a b -> p (a b)")
            _bst = dbuf()
            w_in_b, b_win = load_scaled_w(es, "w_in_b", lambda c: w_in[:, c, :], 1952, g_pre, b_gpre, [_stg, _stg], [_bst, _bst])
            w_uq_b, b_wuq = load(es, "w_uq_b", w_uq[:, :, :], [128, 2, 768], BF16, cast=True)
            g_q, b_gq = load(es, "g_q", g_q_d[:, :], [128, 2])
            g_c, b_gc = load(es, "g_c", g_c_d[:, :], [128, 4])
            convw, b_cw = load(es, "convw", convw_d[:, :, :], [128, 4, 3])

            def two(name, shape, dt, dma=False):
                return [SB(es, "%s%d" % (name, i), shape, dt) for i in range(2)], [dbuf() if dma else Buf() for i in range(2)]

            def one2(name, shape, dt):
                t_ = SB(es, name, shape, dt); b_ = Buf()
                return [t_, t_], [b_, b_]
            xf_, b_xf_ = two("q_xf", [128, 8, 514], F32, dma=True)
            xb_, b_xb_ = one2("q_xb", [128, 8, 514], BF16)
            xsq = SB(es, "q_xsq", [128, 8, 514], BF16); b_xsq = Buf()
            rstd_, b_rstd_ = one2("q_rstd", [128, 514], F32)
            rstd2_, b_rstd2_ = one2("q_rstd2", [128, 514], F32)
            tmp = SB(es, "q_tmp", [128, 514], F32); b_tmp = Buf()
            cqf = SB(es, "q_cqf", [128, 2, 512], F32); b_cqf = Buf()
            cqsq = SB(es, "q_cqsq", [128, 2, 512], BF16); b_cqsq = Buf()
            rq = SB(es, "q_rq", [128, 512], F32); b_rq = Buf()
            cqn_, b_cqn_ = one2("q_cqn", [128, 2, 512], BF16)
            QF_, b_QF_ = two("q_QF", [96, 512], F32)
            t1_, b_t1_ = one2("q_t1", [96, 512], F32)
            t2_, b_t2_ = one2("q_t2", [96, 512], F32)
            pos_i = SB(es, "q_pos", [96, 512], I32); ang = SB(es, "q_ang", [96, 512], F32)
            kf = SB(es, "q_kf", [96, 512], F32)
            cs_, b_cos_ = one2("q_cos", [96, 512], F32)
            sn_, b_sin_ = one2("q_sin", [96, 512], F32)
            b_pos = dbuf(); b_ang = Buf(); b_kf = Buf()
            gcf_, b_gcf_ = two("q_gcf", [128, 514], F32)
            u_, b_u_ = two("q_u", [128, 514], F32)
            yv_, b_y_ = two("q_y", [128, 512], F32)
            csq = SB(es, "q_csq", [128, 4, 512], BF16); b_csq = Buf()
            rc = SB(es, "q_rc", [128, 512], F32); b_rc = Buf()

            ps_st = PS(pes, "q_ps_st", [128, 512]); b_ps_st = Buf()
            ps_sm = PS(pes, "q_ps_sm", [128, 8, 2]); b_ps_sm = [Buf(), Buf(), Buf()]
            G = [PS(pes, "q_G%d" % i, [128, 512]) for i in range(6)]; b_G = [Buf() for _ in range(6)]

            for gi in range(4):
                kq = gi % 2
                xf, b_xf, xb, b_xb = xf_[kq], b_xf_[kq], xb_[kq], b_xb_[kq]
                rstd, b_rstd, rstd2, b_rstd2 = rstd_[kq], b_rstd_[kq], rstd2_[kq], b_rstd2_[kq]
                cs, b_cos, sn, b_sin = cs_[kq], b_cos_[kq], sn_[kq], b_sin_[kq]
                cqn, b_cqn = cqn_[kq], b_cqn_[kq]
                gc0 = gi * 512
                op(sp, lambda: nc.sync.dma_start(out=xf[:], in_=hq[gi]), writes=[b_xf], dma=b_xf)
                op(act, lambda: nc.scalar.activation(out=xb[:], in_=xf[:], func=AF.Copy), reads=[b_xf], writes=[b_xb])
                op(act, lambda: nc.scalar.activation(out=xsq[:], in_=xf[:], func=AF.Square), reads=[b_xf], writes=[b_xsq])
                op(pe, mm_acc(ps_st[:, :], lambda c: ones_bf[:, :], lambda c: xsq[:, c, 2:514], 8), reads=[b_ones, b_xsq], writes=[b_ps_st])
                op(pe, mm_acc(ps_sm[:, 0, :], lambda c: ones_bf[:, :], lambda c: xsq[:, c, 0:2], 8), reads=[b_ones, b_xsq], writes=[b_ps_sm[0]])
                op(act, lambda: nc.scalar.activation(out=tmp[:, 2:514], in_=ps_st[:, :], func=AF.Sqrt, scale=1.0 / D, bias=eps_t[:, 0:1]),
                   reads=[b_ps_st, b_eps], writes=[b_tmp])
                op(act, lambda: nc.scalar.activation(out=tmp[:, 0:2], in_=ps_sm[:, 0, :], func=AF.Sqrt, scale=1.0 / D, bias=eps_t[:, 0:1]),
                   reads=[b_ps_sm[0], b_eps], writes=[b_tmp])
                op(dve, lambda: nc.vector.reciprocal(out=rstd[:, :], in_=tmp[:, :]), reads=[b_tmp], writes=[b_rstd])
                op(dve, lambda: nc.vector.tensor_tensor(out=rstd2[:, :], in0=rstd[:, :], in1=rstd[:, :], op=ALU.mult), reads=[b_rstd], writes=[b_rstd2])
                emit_rope(None, posq[gi:gi + 1, :], 512, 16, (pos_i, ang, kf, cs, sn, b_pos, b_ang, b_kf, b_cos, b_sin))

                for m in range(2):
                    op(pe, mm_acc(G[m][:, :], lambda c: w_in_b[:, c, m * 128:(m + 1) * 128], lambda c: xb[:, c, 2:514], 8),
                       reads=[b_win, b_xb], writes=[b_G[m]])
                    op(dve, lambda: nc.vector.tensor_tensor(out=cqf[:, m, :], in0=G[m][:, :], in1=rstd[:, 2:514], op=ALU.mult),
                       reads=[b_G[m], b_rstd], writes=[b_cqf])
                op(act, lambda: nc.scalar.activation(out=cqsq[:], in_=cqf[:], func=AF.Square), reads=[b_cqf], writes=[b_cqsq])
                op(pe, mm_acc(ps_st[:, :], lambda c: ones_bf[:, :], lambda c: cqsq[:, c, :], 2), reads=[b_ones, b_cqsq], writes=[b_ps_st])
                rstd_from_psum(ps_st, b_ps_st, 128, 512, 1.0 / 256, tmp, b_tmp, rq, b_rq)
                for m in range(2):
                    op(dve, lambda: nc.vector.scalar_tensor_tensor(out=cqn[:, m, :], in0=cqf[:, m, :], scalar=g_q[:, m:m + 1], in1=rq[:, :],
                                                                   op0=ALU.mult, op1=ALU.mult),
                       reads=[b_cqf, b_gq, b_rq], writes=[b_cqn])
                def head_step(h):
                    kh = h % 2
                    pq, bq, pr, br_ = G[4], b_G[4], G[5], b_G[5]
                    QF, b_QF, t1, b_t1, t2, b_t2 = QF_[kh], b_QF_[kh], t1_[kh], b_t1_[kh], t2_[kh], b_t2_[kh]
                    op(pe, mm_acc(pq[0:96, :], lambda c: w_uq_b[:, c, h * 96:(h + 1) * 96], lambda c: cqn[:, c, :], 2),
                       reads=[b_wuq, b_cqn], writes=[bq])
                    op(act, lambda: nc.scalar.activation(out=QF[:, :], in_=pq[0:96, :], func=AF.Copy), reads=[bq], writes=[b_QF])
                    op(pe, lambda: nc.tensor.matmul(pr[0:96, :], lhsT=rot_t[:, :], rhs=QF[:, :], start=True, stop=True),
                       reads=[b_rot, b_QF], writes=[br_])
                    op(act, lambda: nc.scalar.activation(out=QT[0:64, h, gc0:gc0 + 512], in_=QF[0:64, :], func=AF.Copy), reads=[b_QF], writes=[b_QT[h][gi]])
                    op(dve, lambda: nc.vector.tensor_tensor(out=t1[64:96, :], in0=QF[64:96, :], in1=cs[64:96, :], op=ALU.mult),
                       reads=[b_QF, b_cos], writes=[b_t1])
                    op(dve, lambda: nc.vector.tensor_tensor(out=t2[64:96, :], in0=pr[64:96, :], in1=sn[64:96, :], op=ALU.mult),
                       reads=[br_, b_sin], writes=[b_t2])
                    op(dve, lambda: nc.vector.tensor_tensor(out=QT[64:96, h, gc0:gc0 + 512], in0=t1[64:96, :], in1=t2[64:96, :], op=ALU.add),
                       reads=[b_t1, b_t2], writes=[b_QT[h][gi]])
                def conv_step(cc):
                    kc = cc % 2
                    pa, ba, pb, bb, pc_, bc_ = G[0], b_G[0], G[1], b_G[1], G[2], b_G[2]
                    bh = b_ps_sm[1 + kc]
                    ph0, ph1 = ps_sm[:, 1 + kc * 2, :], ps_sm[:, 2 + kc * 2, :]
                    gcf, b_gcf, u, b_u, yv, b_y = gcf_[kc], b_gcf_[kc], u_[kc], b_u_[kc], yv_[kc], b_y_[kc]
                    cb = 416 + cc * 128
                    cg = 416 + 512 + cc * 128
                    cx = 416 + 1024 + cc * 128
                    op(pe, mm_acc(pb[:, :], lambda c: w_in_b[:, c, cg:cg + 128], lambda c: xb[:, c, 2:514], 8), reads=[b_win, b_xb], writes=[bb])
                    op(pe, mm_acc(pc_[:, :], lambda c: w_in_b[:, c, cx:cx + 128], lambda c: xb[:, c, 2:514], 8), reads=[b_win, b_xb], writes=[bc_])
                    op(pe, mm_acc(ph0, lambda c: w_in_b[:, c, cg:cg + 128], lambda c: xb[:, c, 0:2], 8), reads=[b_win, b_xb], writes=[bh])
                    op(pe, mm_acc(ph1, lambda c: w_in_b[:, c, cx:cx + 128], lambda c: xb[:, c, 0:2], 8), reads=[b_win, b_xb], writes=[bh])
                    op(pe, mm_acc(pa[:, :], lambda c: w_in_b[:, c, cb:cb + 128], lambda c: xb[:, c, 2:514], 8), reads=[b_win, b_xb], writes=[ba])
                    op(act, lambda: nc.scalar.activation(out=gcf[:, 2:514], in_=pb[:, :], func=AF.Copy), reads=[bb], writes=[b_gcf])
                    op(act, lambda: nc.scalar.activation(out=gcf[:, 0:2], in_=ph0, func=AF.Copy), reads=[bh], writes=[b_gcf])
                    op(dve, lambda: nc.vector.tensor_tensor(out=u[:, 2:514], in0=pc_[:, :], in1=gcf[:, 2:514], op=ALU.mult),
                       reads=[bc_, b_gcf], writes=[b_u])
                    op(dve, lambda: nc.vector.tensor_tensor(out=u[:, 0:2], in0=ph1, in1=gcf[:, 0:2], op=ALU.mult),
                       reads=[bh, b_gcf], writes=[b_u])
                    op(dve, lambda: nc.vector.tensor_tensor(out=u[:, :], in0=u[:, :], in1=rstd2[:, :], op=ALU.mult), reads=[b_u, b_rstd2], writes=[b_u])
                    op(dve, lambda: nc.vector.tensor_scalar(out=yv[:, :], in0=u[:, 0:512], scalar1=convw[:, cc, 0:1], scalar2=None, op0=ALU.mult),
                       reads=[b_u, b_cw], writes=[b_y])
                    op(dve, lambda: nc.vector.scalar_tensor_tensor(out=yv[:, :], in0=u[:, 1:513], scalar=convw[:, cc, 1:2], in1=yv[:, :], op0=ALU.mult, op1=ALU.add),
                       reads=[b_u, b_cw, b_y], writes=[b_y])
                    op(dve, lambda: nc.vector.scalar_tensor_tensor(out=yv[:, :], in0=u[:, 2:514], scalar=convw[:, cc, 2:3], in1=yv[:, :], op0=ALU.mult, op1=ALU.add),
                       reads=[b_u, b_cw, b_y], writes=[b_y])
                    op(dve, lambda: nc.vector.tensor_tensor(out=yv[:, :], in0=yv[:, :], in1=rstd[:, 2:514], op=ALU.mult), reads=[b_y, b_rstd], writes=[b_y])
                    op(dve, lambda: nc.vector.tensor_tensor(out=convf[:, cc, :], in0=pa[:, :], in1=yv[:, :], op=ALU.mult), reads=[ba, b_y], writes=[b_convf])
                for step_ in (lambda: conv_step(0), lambda: head_step(0), lambda: head_step(1), lambda: conv_step(1), lambda: head_step(2), lambda: head_step(3),
                              lambda: conv_step(2), lambda: head_step(4), lambda: head_step(5), lambda: conv_step(3), lambda: head_step(6), lambda: head_step(7)):
                    step_()
                op(act, lambda: nc.scalar.activation(out=csq[:], in_=convf[:], func=AF.Square), reads=[b_convf], writes=[b_csq])
                op(pe, mm_acc(ps_st[:, :], lambda c: ones_bf[:, :], lambda c: csq[:, c, :], 4), reads=[b_ones, b_csq], writes=[b_ps_st])
                rstd_from_psum(ps_st, b_ps_st, 128, 512, 1.0 / 512, tmp, b_tmp, rc, b_rc)
                for cc in range(4):
                    op(dve, lambda: nc.vector.scalar_tensor_tensor(out=convnT[:, cc, gc0:gc0 + 512], in0=convf[:, cc, :], scalar=g_c[:, cc:cc + 1], in1=rc[:, :],
                                                                   op0=ALU.mult, op1=ALU.mult),
                       reads=[b_convf, b_gc, b_rc], writes=[b_convn[gi]])
            nc.all_engine_barrier()

        ckvnT = at(RB[0], [128, P_TOT], BF16)
        b_ckvn = [Buf() for _ in range(NCH)]
        KT0 = at(RB[0] + 16640, [96, P_TOT], BF16)
        b_KT = [Buf(), Buf()]
        chunk_cols = [(0, 128)] + [(128 + 512 * g, 512) for g in range(16)]

        with ExitStack() as pes:
            es = Bump(RF, RC, (RB[0] + 49920, RB[1]))
            g_pre, b_gpre = load(es, "a_g_pre", g_pre_d[:, :], [128, 8])
            g_kv, b_gkv = load(es, "a_g_kv", g_kv_d[:, :], [128, 1])
            stage = [SB(es, "a_stage%d" % i, [128, 160], F32) for i in range(2)]; b_stage = [dbuf(), dbuf()]
            w_kv_b, b_wkv = load_scaled_w(es, "w_kv_b", lambda c: w_in[:, c, 256:384], 128, g_pre, b_gpre, stage, b_stage)
            w_kpe_b = SB(es, "w_kpe_b", [128, 8, 96], BF16)
            b_wkpe = Buf()
            op(dve, lambda: nc.vector.memset(w_kpe_b[:], 0.0), writes=[b_wkpe])
            for c in range(8):
                k = c % 2
                op(sp, lambda: nc.sync.dma_start(out=stage[k][:, 0:32], in_=w_in[:, c, 384:416]), writes=[b_stage[k]], dma=b_stage[k])
                op(dve, lambda: nc.vector.tensor_scalar(out=w_kpe_b[:, c, 64:96], in0=stage[k][:, 0:32], scalar1=g_pre[:, c:c + 1], scalar2=None, op0=ALU.mult),
                   reads=[b_stage[k], b_gpre], writes=[b_wkpe])
            KPE = SB(es, "a_KPE", [96, 512], F32); b_KPE = Buf()
            op(dve, lambda: nc.vector.memset(KPE[:], 0.0), writes=[b_KPE])

            def two(name, shape, dt, dma=False):
                return [SB(es, "%s%d" % (name, i), shape, dt) for i in range(2)], [dbuf() if dma else Buf() for i in range(2)]
            xf, b_xf = two("a_xf", [128, 8, 512], F32, dma=True)
            xb, b_xb = two("a_xb", [128, 8, 512], BF16)
            _xs = SB(es, "a_xsq", [128, 8, 512], BF16); _bxs = Buf()
            xsq, b_xsq = [_xs, _xs], [_bxs, _bxs]
            rstd, b_rstd = two("a_rstd", [128, 512], F32)
            _t = SB(es, "a_tmp", [128, 512], F32); _bt = Buf(); tmp, b_tmp = [_t, _t], [_bt, _bt]
            ckvf, b_ckvf = two("a_ckvf", [128, 512], F32)
            sqb, b_sqb = two("a_sqb", [128, 512], BF16)
            _r = SB(es, "a_rkv", [128, 512], F32); _br = Buf(); rkv, b_rkv = [_r, _r], [_br, _br]
            _a = SB(es, "a_t1", [96, 512], F32); _ba = Buf(); t1, b_t1 = [_a, _a], [_ba, _ba]
            _c = SB(es, "a_t2", [96, 512], F32); _bc = Buf(); t2, b_t2 = [_c, _c], [_bc, _bc]
            pos_i = SB(es, "a_pos", [96, 512], I32); ang = SB(es, "a_ang", [96, 512], F32)
            kf = SB(es, "a_kf", [96, 512], F32)
            cs_, b_cos_ = two("a_cos", [96, 512], F32)
            sn_, b_sin_ = two("a_sin", [96, 512], F32)
            b_pos = dbuf(); b_ang = Buf(); b_kf = Buf()
            ps_st = [PS(pes, "a_ps_st%d" % i, [128, 512]) for i in range(2)]; b_ps_st = [Buf(), Buf()]
            ps_kv = [PS(pes, "a_ps_kv%d" % i, [128, 512]) for i in range(2)]; b_ps_kv = [Buf(), Buf()]
            ps_kp = [PS(pes, "a_ps_kp%d" % i, [96, 512]) for i in range(2)]; b_ps_kp = [Buf(), Buf()]
            ps_r = [PS(pes, "a_ps_r%d" % i, [96, 512]) for i in range(2)]; b_ps_r = [Buf(), Buf()]

            for ci in range(NCH):
                k = ci % 2
                c0, W = chunk_cols[ci]
                cs, b_cos, sn, b_sin = cs_[k], b_cos_[k], sn_[k], b_sin_[k]
                src = hT0[:, :, :] if ci == 0 else hTm[ci - 1]
                op(sp, lambda: nc.sync.dma_start(out=xf[k][:, :, 0:W], in_=src), writes=[b_xf[k]], dma=b_xf[k])
                op(act, lambda: nc.scalar.activation(out=xb[k][:, :, 0:W], in_=xf[k][:, :, 0:W], func=AF.Copy), reads=[b_xf[k]], writes=[b_xb[k]])
                op(act, lambda: nc.scalar.activation(out=xsq[k][:, :, 0:W], in_=xf[k][:, :, 0:W], func=AF.Square), reads=[b_xf[k]], writes=[b_xsq[k]])
                if ci == 0:
                    emit_rope(None, prefix_d[0:1, :], W, 0, (pos_i, ang, kf, cs, sn, b_pos, b_ang, b_kf, b_cos, b_sin))
                else:
                    emit_rope(None, posrow[0:1, 512 * (ci - 1):512 * ci], W, 16, (pos_i, ang, kf, cs, sn, b_pos, b_ang, b_kf, b_cos, b_sin))
                op(pe, mm_acc(ps_st[k][:, 0:W], lambda c: ones_bf[:, :], lambda c: xsq[k][:, c, 0:W], 8), reads=[b_ones, b_xsq[k]], writes=[b_ps_st[k]])
                rstd_from_psum(ps_st[k], b_ps_st[k], 128, W, 1.0 / D, tmp[k], b_tmp[k], rstd[k], b_rstd[k])
                op(pe, mm_acc(ps_kv[k][:, 0:W], lambda c: w_kv_b[:, c, :], lambda c: xb[k][:, c, 0:W], 8), reads=[b_wkv, b_xb[k]], writes=[b_ps_kv[k]])
                op(pe, mm_acc(ps_kp[k][:, 0:W], lambda c: w_kpe_b[:, c, :], lambda c: xb[k][:, c, 0:W], 8), reads=[b_wkpe, b_xb[k]], writes=[b_ps_kp[k]])
                op(dve, lambda: nc.vector.tensor_tensor(out=ckvf[k][:, 0:W], in0=ps_kv[k][:, 0:W], in1=rstd[k][:, 0:W], op=ALU.mult),
                   reads=[b_ps_kv[k], b_rstd[k]], writes=[b_ckvf[k]])
                op(act, lambda: nc.scalar.activation(out=sqb[k][:, 0:W], in_=ckvf[k][:, 0:W], func=AF.Square), reads=[b_ckvf[k]], writes=[b_sqb[k]])
                op(pe, lambda: nc.tensor.matmul(ps_st[k][:, 0:W], lhsT=ones_bf[:, :], rhs=sqb[k][:, 0:W], start=True, stop=True),
                   reads=[b_ones, b_sqb[k]], writes=[b_ps_st[k]])
                op(dve, lambda: nc.vector.tensor_tensor(out=KPE[64:96, 0:W], in0=ps_kp[k][64:96, 0:W], in1=rstd[k][64:96, 0:W], op=ALU.mult),
                   reads=[b_ps_kp[k], b_rstd[k]], writes=[b_KPE])
                op(pe, lambda: nc.tensor.matmul(ps_r[k][:, 0:W], lhsT=rot_t[:, :], rhs=KPE[:, 0:W], start=True, stop=True),
                   reads=[b_rot, b_KPE], writes=[b_ps_r[k]])
                rstd_from_psum(ps_st[k], b_ps_st[k], 128, W, 1.0 / 128, tmp[k], b_tmp[k], rkv[k], b_rkv[k])
                op(dve, lambda: nc.vector.scalar_tensor_tensor(out=ckvnT[:, c0:c0 + W], in0=ckvf[k][:, 0:W], scalar=g_kv[:, 0:1], in1=rkv[k][:, 0:W],
                                                               op0=ALU.mult, op1=ALU.mult),
                   reads=[b_ckvf[k], b_gkv, b_rkv[k]], writes=[b_ckvn[ci]])
                op(dve, lambda: nc.vector.tensor_tensor(out=t1[k][64:96, 0:W], in0=KPE[64:96, 0:W], in1=cs[64:96, 0:W], op=ALU.mult),
                   reads=[b_KPE, b_cos], writes=[b_t1[k]])
                op(dve, lambda: nc.vector.tensor_tensor(out=t2[k][64:96, 0:W], in0=ps_r[k][64:96, 0:W], in1=sn[64:96, 0:W], op=ALU.mult),
                   reads=[b_ps_r[k], b_sin], writes=[b_t2[k]])
                op(dve, lambda: nc.vector.tensor_tensor(out=KT0[64:96, c0:c0 + W], in0=t1[k][64:96, 0:W], in1=t2[k][64:96, 0:W], op=ALU.add),
                   reads=[b_t1[k], b_t2[k]], writes=[b_KT[0]])
            nc.all_engine_barrier()

        attnT = at(RC[0], [128, 4, 2048], BF16)
        b_attn = [Buf() for _ in range(4)]
        with ExitStack() as pes:
            es = Bump(RF, (RC[0] + 16384, RC[1]), (RB[0] + 49920, RB[1]))
            KT1 = at(RB[0] + 33280, [96, P_TOT], BF16)
            KT = [KT0, KT1]
            op(dve, lambda: nc.vector.tensor_copy(out=KT1[64:96, :], in_=KT0[64:96, :]), reads=[b_KT[0]], writes=[b_KT[1]])
            w_ukv_b, b_wukv = load(es, "w_ukv_b", w_ukv[:, :], [128, 1024], BF16, cast=True)
            selm = SB(es, "selm", [128, 2, 128], F32); b_sel = Buf()
            op(dve, lambda: nc.vector.memset(selm[:], 0.0), writes=[b_sel])
            op(dve, lambda: nc.vector.memset(selm[64:65, 0, 0:64], 1.0), writes=[b_sel])
            op(dve, lambda: nc.vector.memset(selm[0:1, 1, 64:128], 1.0), writes=[b_sel])
            dmask_b, b_dmask = load(es, "dmask_b", dmask_d[:, :, :], [128, 4, 512], BF16, cast=True)
            abias, b_abias = load(es, "abias", abias_d[:, :], [128, 12])
            g_a, b_ga = load(es, "g_a", g_a_d[:, :], [128, 4])
            Vaug = [SB(es, "Vaug%d" % i, [128, NKB, 128], BF16) for i in range(2)]
            b_V = [Buf(), Buf()]
            for i in range(2):
                onec = 64 if i == 0 else 0
                op(pool, lambda: nc.gpsimd.memset(Vaug[i][:], 0.0), writes=[b_V[i]])
                op(pool, lambda: nc.gpsimd.memset(Vaug[i][:, :, onec:onec + 1], 1.0), writes=[b_V[i]])
                op(pool, lambda: nc.gpsimd.memset(Vaug[i][0:112, 0, onec:onec + 1], 0.0), writes=[b_V[i]])
            PT = [SB(es, "PT%d" % i, [128, 2, 512], BF16) for i in range(3)]; b_PT = [Buf(), Buf(), Buf()]
            OTsb = SB(es, "OTsb", [128, 512], F32); b_OTsb = Buf()
            rec = SB(es, "rec", [128, 512], F32); b_rec = Buf()
            ps_S = [PS(pes, "ps_S%d" % i, [128, 2, 512]) for i in range(2)]; b_S = [Buf(), Buf()]
            ps_O = [PS(pes, "ps_O%d" % i, [128, 512]) for i in range(2)]; b_O = [Buf(), Buf()]
            ps_bc = PS(pes, "ps_bc", [128, 512]); b_bc = Buf()
            ps_bld = PS(pes, "ps_bld", [128, 512]); b_bld = Buf()

            def build_steps(h):
                sl = h % 2
                vc = 0 if sl == 0 else 64
                out = []
                for kb0 in range(0, NKB, 8):
                    def stepv(kb0=kb0):
                        n = min(8, NKB - kb0)

                        def f():
                            ins = None
                            for r in range(n):
                                kb = kb0 + r
                                ins = nc.tensor.matmul(ps_bld[:, r * 64:(r + 1) * 64], lhsT=ckvnT[:, kb * 128:(kb + 1) * 128],
                                                       rhs=w_ukv_b[:, h * 128 + 64:h * 128 + 128], start=True, stop=True)
                            return ins
                        op(pe, f, reads=[b_wukv] + b_ckvn, writes=[b_bld])
                        op(dve, lambda: nc.vector.tensor_copy(out=Vaug[sl][:, kb0:kb0 + n, vc:vc + 64],
                                                              in_=ps_bld[:, 0:n * 64].rearrange("p (r v) -> p r v", v=64)),
                           reads=[b_bld], writes=[b_V[sl]])
                    out.append(stepv)
                for ci in range(NCH):
                    def stepk(ci=ci):
                        c0, W = chunk_cols[ci]
                        op(pe, lambda: nc.tensor.matmul(ps_bld[0:64, 0:W], lhsT=w_ukv_b[:, h * 128:h * 128 + 64], rhs=ckvnT[:, c0:c0 + W], start=True, stop=True),
                           reads=[b_wukv, b_ckvn[ci]], writes=[b_bld])
                        op(dve, lambda: nc.vector.tensor_copy(out=KT[sl][0:64, c0:c0 + W], in_=ps_bld[0:64, 0:W]), reads=[b_bld], writes=[b_KT[sl]])
                    out.append(stepk)
                return out

            def build_head(h):
                for st_ in build_steps(h):
                    st_()

            stgc = [SB(es, "stgc%d" % i, [128, 1024], F32) for i in range(3)]; b_stgc = [dbuf() for _ in range(3)]
            outc = [SB(es, "outc%d" % i, [128, 1024], BF16) for i in range(3)]; b_outc = [Buf() for _ in range(3)]
            b_outd = [dbuf() for _ in range(3)]
            pieces = []
            for e_ in range(1, NEXP + 1):
                rows_ = slice(e_ * 128, (e_ + 1) * 128)
                for pc_i in range(6):
                    srcd_ = (wgu_lo, wgu_hi, wd_h)[pc_i // 2]
                    col_ = (pc_i % 2) * 1024
                    if pc_i < 4:
                        dst_ = wbf_gu[rows_, pc_i * 1024:(pc_i + 1) * 1024]
                    else:
                        dst_ = wbf_d[rows_, (pc_i - 4) * 1024:(pc_i - 3) * 1024]
                    pieces.append((srcd_[rows_, col_:col_ + 1024], dst_))
            NPC = len(pieces)

            def pc_in(i):
                k3 = i % 3
                op(sp, lambda: nc.sync.dma_start(out=stgc[k3][:, :], in_=pieces[i][0]), writes=[b_stgc[k3]], dma=b_stgc[k3])

            def pc_step(i):
                k3 = i % 3
                if i + 2 < NPC:
                    pc_in(i + 2)
                op(dve, lambda: nc.vector.tensor_copy(out=outc[k3][:, :], in_=stgc[k3][:, :]), reads=[b_stgc[k3]], writes=[b_outc[k3]])
                op(sp, lambda: nc.sync.dma_start(out=pieces[i][1], in_=outc[k3][:, :]), reads=[b_outc[k3]], writes=[b_outd[k3]], dma=b_outd[k3])
            pc_in(0)
            pc_in(1)
            pc_next = [0]

            build_head(0)
            pairs = []
            for gi in range(4):
                seq = []
                base = 1 + 16 * gi
                for r in range(3):
                    for i2 in range(0, 4, 2):
                        seq.append((gi * 3 + r, [(base + 4 * r + i2 + u, None) for u in range(2)]))
                for i2 in range(0, 4, 2):
                    seq.append((None, [(base + 12 + i2 + u, i2 + u) for u in range(2)]))
                fb = list(range(1, base))
                for i2 in range(0, len(fb), 2):
                    seq.append((None, [(b_, None) for b_ in fb[i2:i2 + 2]]))
                seq.append((None, [(0, None)]))
                nst = sum(len(p[1]) for p in seq)
                cnt_ = 0
                for bcol, items in seq:
                    ent = []
                    for (blk, md) in items:
                        ent.append((gi, blk, md, cnt_ == 0, cnt_ == nst - 1))
                        cnt_ += 1
                    pairs.append((bcol, ent))
            npair = len(pairs)
            gp = [0]
            for h in range(8):
                sl = h % 2
                R = slice(0, 64) if sl == 0 else slice(64, 128)
                pending_build = build_steps(h + 1) if h + 1 < 8 else []

                def emit_S(m):
                    pp = (gp[0] + m) % 2
                    ent = pairs[m][1]

                    def f():
                        ins = None
                        for j, (gi, blk, md, fst, lst) in enumerate(ent):
                            ins = nc.tensor.matmul(ps_S[pp][:, j, :], lhsT=KT[sl][0:96, blk * 128:(blk + 1) * 128], rhs=QT[0:96, h, gi * 512:(gi + 1) * 512],
                                                   start=True, stop=True)
                        return ins
                    gis = sorted(set(e_[0] for e_ in ent))
                    op(pe, f, reads=[b_KT[sl]] + [b_QT[h][g_] for g_ in gis], writes=[b_S[pp]])

                emit_S(0)
                emit_S(1)
                for m in range(npair):
                    pp = (gp[0] + m) % 2
                    pt = (gp[0] + m) % 3
                    bcol, ent = pairs[m]
                    nj = len(ent)
                    if bcol is None:
                        op(act, lambda: nc.scalar.activation(out=PT[pt][:, 0:nj, :], in_=ps_S[pp][:, 0:nj, :], func=AF.Exp, scale=SCALE),
                           reads=[b_S[pp]], writes=[b_PT[pt]])
                    else:
                        op(act, lambda: nc.scalar.activation(out=PT[pt][:, 0:nj, :], in_=ps_S[pp][:, 0:nj, :], func=AF.Exp, scale=SCALE, bias=abias[:, bcol:bcol + 1]),
                           reads=[b_S[pp], b_abias], writes=[b_PT[pt]])
                    for j, (gi, blk, md, fst, lst) in enumerate(ent):
                        if md is not None:
                            op(dve, lambda: nc.vector.tensor_tensor(out=PT[pt][:, j, :], in0=PT[pt][:, j, :], in1=dmask_b[:, md, :], op=ALU.mult),
                               reads=[b_dmask, b_PT[pt]], writes=[b_PT[pt]])
                    if m + 2 < npair:
                        emit_S(m + 2)
                    if pending_build and m % 3 == 2:
                        pending_build.pop(0)()
                    if m % 3 == 0 and pc_next[0] < NPC:
                        pc_step(pc_next[0])
                        pc_next[0] += 1
                    for j, (gi, blk, md, fst, lst) in enumerate(ent):
                        ob = (h * 4 + gi) % 2
                        op(pe, lambda: nc.tensor.matmul(ps_O[ob][:, :], lhsT=Vaug[sl][:, blk, :], rhs=PT[pt][:, j, :], start=fst, stop=lst),
                           reads=[b_V[sl], b_PT[pt]], writes=[b_O[ob]])
                        if lst:
                            op(dve, lambda: nc.vector.tensor_copy(out=OTsb[:, :], in_=ps_O[ob][:, :]), reads=[b_O[ob]], writes=[b_OTsb])
                            op(pe, lambda: nc.tensor.matmul(ps_bc[:, :], lhsT=selm[:, sl, :], rhs=OTsb[:, :], start=True, stop=True),
                               reads=[b_sel, b_OTsb], writes=[b_bc])
                            op(dve, lambda: nc.vector.reciprocal(out=rec[R, :], in_=ps_bc[R, :]), reads=[b_bc], writes=[b_rec])
                            op(dve, lambda: nc.vector.tensor_tensor(out=attnT[R, h // 2, gi * 512:(gi + 1) * 512], in0=OTsb[R, :], in1=rec[R, :], op=ALU.mult),
                               reads=[b_OTsb, b_rec], writes=[b_attn[gi]])
                while pending_build:
                    pending_build.pop(0)()
                gp[0] += npair
            while pc_next[0] < NPC:
                pc_step(pc_next[0])
                pc_next[0] += 1
            for k3_ in range(3):
                _wait(sp, b_outd[k3_].w)
            if dbg:
                dbg_out["attnT"] = dout("dbg_attnT", [128, 4, 2048], BF16)
                dbg_out["ckvnT"] = dout("dbg_ckvnT", [128, P_TOT], BF16)
                dbg_out["KT0"] = dout("dbg_KT0", [96, P_TOT], BF16)
                dbg_out["KT1"] = dout("dbg_KT1", [96, P_TOT], BF16)
                bd = dbuf()
                op(sp, lambda: nc.sync.dma_start(out=dbg_out["attnT"][:, :, :], in_=attnT[:]), reads=b_attn, writes=[bd], dma=bd)
                op(sp, lambda: nc.sync.dma_start(out=dbg_out["ckvnT"][:, :], in_=ckvnT[:]), reads=b_ckvn, writes=[bd], dma=bd)
                op(sp, lambda: nc.sync.dma_start(out=dbg_out["KT0"][:, :], in_=KT0[:]), reads=[b_KT[0]], writes=[bd], dma=bd)
                op(sp, lambda: nc.sync.dma_start(out=dbg_out["KT1"][:, :], in_=KT1[:]), reads=[b_KT[1]], writes=[bd], dma=bd)
                _wait(sp, bd.w)
            asq = SB(es, "asq", [128, 4, 512], BF16); b_asq = Buf()
            atmp = SB(es, "atmp", [128, 512], F32); b_atmp = Buf()
            ra = SB(es, "ra", [128, 512], F32); b_ra = Buf()
            for gi in range(4):
                cols = slice(gi * 512, (gi + 1) * 512)
                op(act, lambda: nc.scalar.activation(out=asq[:, :, :], in_=attnT[:, :, cols], func=AF.Square), reads=[b_attn[gi]], writes=[b_asq])
                op(pe, mm_acc(ps_bc[:, :], lambda c: ones_bf[:, :], lambda c: asq[:, c, :], 4), reads=[b_ones, b_asq], writes=[b_bc])
                rstd_from_psum(ps_bc, b_bc, 128, 512, 1.0 / 512, atmp, b_atmp, ra, b_ra)
                for c in range(4):
                    op(dve, lambda: nc.vector.scalar_tensor_tensor(out=attnT[:, c, cols], in0=attnT[:, c, cols], scalar=g_a[:, c:c + 1], in1=ra[:, :],
                                                                   op0=ALU.mult, op1=ALU.mult),
                       reads=[b_attn[gi], b_ga, b_ra], writes=[b_attn[gi]])
            nc.all_engine_barrier()

        hn2T = at(RA[0], [128, 8, 2048], BF16)
        b_hn2 = [Buf() for _ in range(16)]
        comb = SB(cst, "comb", [128, 16, 33], F32)
        b_comb = Buf()
        b_yall = dbuf()
        b_hn2d = dbuf()
        b_mapinit = dbuf()
        b_scat = dbuf()
        BIG = 1.0e30
        NT = 64 + NOV
        mapt = SB(cst, "mapt", [128, NT, 4], F32); b_mapt = dbuf()
        gidx = SB(cst, "gidx", [128, NT], I32); b_gidx = Buf()
        sidx = SB(cst, "sidx", [128, NT], I32); b_sidx = Buf()
        ovidx = SB(cst, "ovidx", [128, NOV], I32); b_ovidx = Buf()
        with ExitStack() as pes:
            es = Bump(RF)
            esl = Bump(RB)
            w_oa_b, b_woa = load(es, "w_oa_b", w_out_a[:, :, :], [128, 4, D], BF16, cast=True)
            w_oc_b, b_woc = load(es, "w_oc_b", w_out_c[:, :, :], [128, 4, D], BF16, cast=True)
            g_pm, b_gpm = load(es, "g_pm_bc", g_pm_d[0:1, :].partition_broadcast(128), [128, D])
            g_pf, b_gpf = load(es, "g_pf", g_pf_d[:, :], [128, 8])
            wr_t, b_wr = load(es, "wr_t", wr_d[:, :, :], [128, 8, 36])
            br_t, b_br = load(es, "br_bc", br_d[0:1, :].partition_broadcast(128), [128, 36])
            ident, b_id = load(es, "ident", ident_d[:, :], [128, 128])
            op(dve, lambda: nc.vector.memset(comb[:, :, 0:1], 1.0), writes=[b_comb])

            def two(name, shape, dt, dma=False):
                return [SB(esl, "%s%d" % (name, i), shape, dt) for i in range(2)], [dbuf() if dma else Buf() for i in range(2)]
            xo = [SB(esl, "xo%d" % i, [128, D], F32) for i in range(3)]; b_xo = [dbuf() for _ in range(3)]

            def load_xo(tb_):
                op(sp, lambda: nc.sync.dma_start(out=xo[tb_ % 3][:, :], in_=xown[tb_]), writes=[b_xo[tb_ % 3]], dma=b_xo[tb_ % 3])
            load_xo(0)
            load_xo(1)
            hm, b_hm = two("hm", [128, D], F32)
            hh, b_hh = two("hh", [128, D], F32)
            hs, b_hs = two("hs", [128, D], F32)
            junk, b_junk = two("junk", [128, D], BF16)
            hsT, b_hsT = two("hsT", [128, 8, 128], F32)
            sm, b_sm = two("sm", [128, 8], F32)
            b_smc = [[Buf() for _ in range(8)] for _ in range(2)]
            lgall = SB(es, "lgall", [128, 16, 36], F32); b_lg = Buf()
            g_pfb, b_gpfb = load(es, "g_pfb", g_pfb_d[0:1, :].partition_broadcast(128), [128, D])
            hrow, b_hrow = two("hrow", [128, D], BF16)
            zrow = SB(es, "zrow", [1, D], BF16); b_zrow = Buf()
            op(dve, lambda: nc.vector.memset(zrow[:], 0.0), writes=[b_zrow])
            op(sp, lambda: nc.sync.dma_start(out=hn2d[2048:2049, :], in_=zrow[:, :]), reads=[b_zrow], dma=b_hn2d)
            op(sp, lambda: nc.sync.dma_start(out=maps_d[:, :], in_=mapinit_d[:, :]), dma=b_mapinit, writes=[b_mapinit])
            ps_mix = [PS(pes, "ps_mix%d" % i, [128, D]) for i in range(2)]; b_pmix = [Buf(), Buf()]
            ps_T = PS(pes, "ps_T", [128, 8, 128]); b_pT = Buf()
            ps_lg = [PS(pes, "ps_lg%d" % i, [128, 36]) for i in range(2)]; b_plg = [Buf(), Buf()]

            hh3 = [hh[0], hh[1], SB(esl, "hh2", [128, D], F32)]; b_hh3 = [b_hh[0], b_hh[1], Buf()]
            sm3 = [SB(esl, "sm3_%d" % i, [128, 8], F32) for i in range(3)]
            b_sm3 = [[Buf() for _ in range(8)] for _ in range(3)]

            def st1(tb):
                k, k3 = tb % 2, tb % 3
                tc0 = tb * 128
                bs = b_sm3[k3]

                def S(i):
                    return sm3[k3][:, i:i + 1]
                if tb + 2 < 16:
                    load_xo(tb + 2)

                def fmix():
                    ins = None
                    for half in range(2):
                        hc = slice(half * 512, (half + 1) * 512)
                        for h in range(4):
                            ins = nc.tensor.matmul(ps_mix[k][:, hc], lhsT=attnT[:, h, tc0:tc0 + 128], rhs=w_oa_b[:, h, hc], start=(h == 0), stop=False)
                        for cc in range(4):
                            ins = nc.tensor.matmul(ps_mix[k][:, hc], lhsT=convnT[:, cc, tc0:tc0 + 128], rhs=w_oc_b[:, cc, hc], start=False, stop=(cc == 3))
                    return ins
                op(pe, fmix, reads=[b_attn[tb // 4], b_convn[tb // 4], b_woa, b_woc], writes=[b_pmix[k]])
                op(act, lambda: nc.scalar.activation(out=junk[k][:, :], in_=ps_mix[k][:, :], func=AF.Square, accum_out=S(0)),
                   reads=[b_pmix[k]], writes=[b_junk[k], bs[0]])
                op(act, lambda: nc.scalar.activation(out=S(2), in_=S(0), func=AF.Sqrt, scale=1.0 / D, bias=eps_t[:, 0:1]), reads=[bs[0], b_eps], writes=[bs[2]])
                op(dve, lambda: nc.vector.reciprocal(out=S(2), in_=S(2)), reads=[bs[2]], writes=[bs[2]])
                op(dve, lambda: nc.vector.scalar_tensor_tensor(out=hm[k][:, :], in0=ps_mix[k][:, :], scalar=S(2), in1=g_pm[:, :], op0=ALU.mult, op1=ALU.mult),
                   reads=[b_pmix[k], bs[2], b_gpm], writes=[b_hm[k]])
                op(dve, lambda: nc.vector.tensor_tensor(out=hh3[k3][:, :], in0=hm[k][:, :], in1=xo[k3][:, :], op=ALU.add), reads=[b_hm[k], b_xo[k3]], writes=[b_hh3[k3]])
                op(sp, lambda: nc.sync.dma_start(out=y[tb], in_=hh3[k3][:, :]), reads=[b_hh3[k3]], writes=[b_yall], dma=b_yall)

            def st2(tb):
                k, k3 = tb % 2, tb % 3
                tc0 = tb * 128
                bs = b_sm3[k3]

                def S(i):
                    return sm3[k3][:, i:i + 1]
                op(act, lambda: nc.scalar.activation(out=junk[k][:, :], in_=hh3[k3][:, :], func=AF.Square, accum_out=S(1)),
                   reads=[b_hh3[k3]], writes=[b_junk[k], bs[1]])
                op(act, lambda: nc.scalar.activation(out=S(3), in_=S(1), func=AF.Sqrt, scale=1.0 / D, bias=eps_t[:, 0:1]), reads=[bs[1], b_eps], writes=[bs[3]])
                op(dve, lambda: nc.vector.reciprocal(out=S(3), in_=S(3)), reads=[bs[3]], writes=[bs[3]])
                op(act, lambda: nc.scalar.activation(out=hs[k][:, :], in_=hh3[k3][:, :], func=AF.Copy, scale=S(3)), reads=[b_hh3[k3], bs[3]], writes=[b_hs[k]])

                def ftr():
                    ins = None
                    for c in range(8):
                        ins = nc.tensor.transpose(ps_T[:, c, :], hs[k][:, c * 128:(c + 1) * 128], ident[:, :])
                    return ins
                op(pe, ftr, reads=[b_hs[k], b_id], writes=[b_pT])
                op(dve, lambda: nc.vector.tensor_tensor(out=hrow[k][:, :], in0=hs[k][:, :], in1=g_pfb[:, :], op=ALU.mult), reads=[b_hs[k], b_gpfb], writes=[b_hrow[k]])
                op(sp, lambda: nc.sync.dma_start(out=hn2d[tc0:tc0 + 128, :], in_=hrow[k][:, :]), reads=[b_hrow[k]], dma=b_hn2d)

            def st3(tb):
                k = tb % 2
                tc0 = tb * 128
                op(dve, lambda: nc.vector.tensor_tensor(out=hsT[k][:, :, :], in0=ps_T[:, :, :], in1=g_pf[:, :].unsqueeze(2).to_broadcast([128, 8, 128]), op=ALU.mult),
                   reads=[b_pT, b_gpf], writes=[b_hsT[k]])
                op(act, lambda: nc.scalar.activation(out=hn2T[:, :, tc0:tc0 + 128], in_=hsT[k][:, :, :], func=AF.Copy), reads=[b_hsT[k]], writes=[b_hn2[tb]])
                op(pe, mm_acc(ps_lg[k][:, :], lambda c: hsT[k][:, c, :], lambda c: wr_t[:, c, :], 8), reads=[b_hsT[k], b_wr], writes=[b_plg[k]])
                op(dve, lambda: nc.vector.tensor_tensor(out=lgall[:, tb, :], in0=ps_lg[k][:, :], in1=br_t[:, :], op=ALU.add), reads=[b_plg[k], b_br], writes=[b_lg])

            for it in range(16 + 2):
                if it < 16:
                    st1(it)
                if 0 <= it - 2 < 16:
                    st3(it - 2)
                if 0 <= it - 1 < 16:
                    st2(it - 1)

            _wait(sp, b_yall.w)
            _wait(sp, (b_hn2d.dsem, b_hn2d.dn))
            nc.all_engine_barrier()
            es = Bump(RB)

            def T(name, shape):
                return SB(es, name, shape, F32), Buf()
            gmax, b_gmax = T("gmax", [128, 16]); gm, b_gm = T("gm", [128, 16, 4]); gsh, b_gsh = T("gsh", [128, 16, 4])
            gsum, b_gsum = T("gsum", [128, 16]); gw, b_gw = T("gw", [128, 16]); pen, b_pen = T("pen", [128, 16, 4])
            elm, b_elm = T("elm", [128, 16, 32]); elm2, b_elm2 = T("elm2", [128, 16, 32])
            m1, b_m1 = T("m1", [128, 16]); m2, b_m2 = T("m2", [128, 16])
            oh1, b_oh1 = T("oh1", [128, 16, 32]); oh2, b_oh2 = T("oh2", [128, 16, 32])
            dd, b_dd = T("dd", [128, 16]); w1, b_w1 = T("w1", [128, 16]); w2, b_w2 = T("w2", [128, 16])
            lg_g = lgall[:, :, 0:4]
            lg_e = lgall[:, :, 4:36]

            def bc(t2d, n):
                return t2d[:, :].unsqueeze(2).to_broadcast([128, 16, n])
            op(dve, lambda: nc.vector.tensor_reduce(out=gmax[:, :], in_=lg_g, axis=AX.X, op=ALU.max), reads=[b_lg], writes=[b_gmax])
            op(dve, lambda: nc.vector.tensor_tensor(out=gm[:, :, :], in0=lg_g, in1=bc(gmax, 4), op=ALU.is_equal), reads=[b_lg, b_gmax], writes=[b_gm])
            op(dve, lambda: nc.vector.tensor_tensor(out=gsh[:, :, :], in0=lg_g, in1=bc(gmax, 4), op=ALU.subtract), reads=[b_lg, b_gmax], writes=[b_gsh])
            op(act, lambda: nc.scalar.activation(out=gsh[:, :, :], in_=gsh[:, :, :], func=AF.Exp), reads=[b_gsh], writes=[b_gsh])
            op(dve, lambda: nc.vector.tensor_reduce(out=gsum[:, :], in_=gsh[:, :, :], axis=AX.X, op=ALU.add), reads=[b_gsh], writes=[b_gsum])
            op(dve, lambda: nc.vector.reciprocal(out=gw[:, :], in_=gsum[:, :]), reads=[b_gsum], writes=[b_gw])
            op(dve, lambda: nc.vector.tensor_scalar(out=pen[:, :, :], in0=gm[:, :, :], scalar1=BIG, scalar2=-BIG, op0=ALU.mult, op1=ALU.add), reads=[b_gm], writes=[b_pen])
            op(dve, lambda: nc.vector.tensor_tensor(out=elm[:, :, :].rearrange("p b (g e) -> p b g e", g=4),
                                                    in0=lg_e.rearrange("p b (g e) -> p b g e", g=4),
                                                    in1=pen[:, :, :].unsqueeze(3).to_broadcast([128, 16, 4, 8]), op=ALU.add),
               reads=[b_lg, b_pen], writes=[b_elm])
            op(dve, lambda: nc.vector.tensor_reduce(out=m1[:, :], in_=elm[:, :, :], axis=AX.X, op=ALU.max), reads=[b_elm], writes=[b_m1])
            op(dve, lambda: nc.vector.tensor_tensor(out=oh1[:, :, :], in0=elm[:, :, :], in1=bc(m1, 32), op=ALU.is_equal), reads=[b_elm, b_m1], writes=[b_oh1])
            op(dve, lambda: nc.vector.scalar_tensor_tensor(out=elm2[:, :, :], in0=oh1[:, :, :], scalar=-BIG, in1=elm[:, :, :], op0=ALU.mult, op1=ALU.add),
               reads=[b_oh1, b_elm], writes=[b_elm2])
            op(dve, lambda: nc.vector.tensor_reduce(out=m2[:, :], in_=elm2[:, :, :], axis=AX.X, op=ALU.max), reads=[b_elm2], writes=[b_m2])
            op(dve, lambda: nc.vector.tensor_tensor(out=oh2[:, :, :], in0=elm2[:, :, :], in1=bc(m2, 32), op=ALU.is_equal), reads=[b_elm2, b_m2], writes=[b_oh2])
            op(dve, lambda: nc.vector.tensor_tensor(out=dd[:, :], in0=m2[:, :], in1=m1[:, :], op=ALU.subtract), reads=[b_m1, b_m2], writes=[b_dd])
            op(act, lambda: nc.scalar.activation(out=dd[:, :], in_=dd[:, :], func=AF.Exp), reads=[b_dd], writes=[b_dd])
            op(dve, lambda: nc.vector.tensor_scalar(out=w1[:, :], in0=dd[:, :], scalar1=1.0, scalar2=None, op0=ALU.add), reads=[b_dd], writes=[b_w1])
            op(dve, lambda: nc.vector.reciprocal(out=w1[:, :], in_=w1[:, :]), reads=[b_w1], writes=[b_w1])
            op(dve, lambda: nc.vector.tensor_tensor(out=w2[:, :], in0=dd[:, :], in1=w1[:, :], op=ALU.mult), reads=[b_dd, b_w1], writes=[b_w2])
            op(dve, lambda: nc.vector.tensor_tensor(out=w1[:, :], in0=w1[:, :], in1=gw[:, :], op=ALU.mult), reads=[b_w1, b_gw], writes=[b_w1])
            op(dve, lambda: nc.vector.tensor_tensor(out=w2[:, :], in0=w2[:, :], in1=gw[:, :], op=ALU.mult), reads=[b_w2, b_gw], writes=[b_w2])
            ps_pp = ps_mix[0][:, :].rearrange("p (a n) -> p a n", a=2); b_pp = b_pmix[0]
            ps_pt = ps_mix[1][:, :].rearrange("p (a n) -> p a n", a=2); b_pt = b_pmix[1]
            triu_b, b_triu = load(es, "triu_b", triu_d[:, :], [128, 128], BF16, cast=True)
            vb, b_vb = load(es, "vb", vb_d[0:1, :].partition_broadcast(128), [128, 64])
            tokid, b_tok = load(es, "tokid", tokid_d[:, :], [128, 16])
            pidx, b_pidx = load(es, "pidx", pidx_d[:, :], [128, 1])
            jidx, b_jidx = load(es, "jidx", jidx_d[0:1, :].partition_broadcast(128), [128, NOV])
            ohv_b = SB(es, "ohv_b", [128, 16, 64], BF16); b_ohvb = Buf()
            ohv, b_ohv = T("ohv", [128, 16, 64])
            tot_s, b_tot = T("tot_s", [128, 16, 64]); boff, b_boff = T("boff", [128, 16, 64])
            rank, b_rank = T("rank", [128, 16, 64]); wk, b_wk = T("wk", [128, 16, 64])
            cnt, b_cnt = T("cnt", [128, 64]); ovt, b_ovt = T("ovt", [128, 64]); ovend, b_ovend = T("ovend", [128, 64])
            ovst, b_ovst = T("ovst", [128, 64]); one64, b_one64 = T("one64", [128, 64])
            rr, b_rr = T("rr", [128, 16, 2]); basev, b_basev = T("basev", [128, 16, 2]); ovs, b_ovs = T("ovs", [128, 16, 2])
            isov, b_isov = T("isov", [128, 16, 2]); posf, b_posf = T("posf", [128, 16, 2])
            pos_i = SB(cst, "pos_i", [128, 16, 2], I32); b_posi = Buf()
            srcm = SB(cst, "srcm", [128, 32, 4], F32); b_srcm = Buf()
            cmpj, b_cmpj = T("cmpj", [128, NOV, 64]); ovv, b_ovv = T("ovv", [128, NOV]); ovf, b_ovf = T("ovf", [128, NOV])
            op(dve, lambda: nc.vector.tensor_copy(out=ohv[:, :, 0:32], in_=oh1[:, :, :]), reads=[b_oh1], writes=[b_ohv])
            op(dve, lambda: nc.vector.tensor_copy(out=ohv[:, :, 32:64], in_=oh2[:, :, :]), reads=[b_oh2], writes=[b_ohv])
            op(dve, lambda: nc.vector.tensor_copy(out=ohv_b[:, :, :], in_=ohv[:, :, :]), reads=[b_ohv], writes=[b_ohvb])
            ohv2d = ohv_b.rearrange("p b v -> p (b v)")
            for hb in range(2):
                op(pe, lambda: nc.tensor.matmul(ps_pp[:, hb, :], lhsT=triu_b[:, :], rhs=ohv2d[:, hb * 512:(hb + 1) * 512], start=True, stop=True),
                   reads=[b_triu, b_ohvb], writes=[b_pp])
                op(pe, lambda: nc.tensor.matmul(ps_pt[:, hb, :], lhsT=ones_bf[:, :], rhs=ohv2d[:, hb * 512:(hb + 1) * 512], start=True, stop=True),
                   reads=[b_ones, b_ohvb], writes=[b_pt])
            op(act, lambda: nc.scalar.activation(out=tot_s.rearrange("p b v -> p (b v)"), in_=ps_pt.rearrange("p a n -> p (a n)"), func=AF.Copy),
               reads=[b_pt], writes=[b_tot])
            op(dve, lambda: nc.vector.memset(boff[:, 0, :], 0.0), writes=[b_boff])
            for b_ in range(1, 16):
                op(dve, lambda: nc.vector.tensor_tensor(out=boff[:, b_, :], in0=boff[:, b_ - 1, :], in1=tot_s[:, b_ - 1, :], op=ALU.add),
                   reads=[b_boff, b_tot], writes=[b_boff])
            op(dve, lambda: nc.vector.tensor_tensor(out=cnt[:, :], in0=boff[:, 15, :], in1=tot_s[:, 15, :], op=ALU.add), reads=[b_boff, b_tot], writes=[b_cnt])
            op(dve, lambda: nc.vector.tensor_tensor(out=rank.rearrange("p b v -> p (b v)"), in0=ps_pp.rearrange("p a n -> p (a n)"),
                                                    in1=boff.rearrange("p b v -> p (b v)"), op=ALU.add), reads=[b_pp, b_boff], writes=[b_rank])
            op(dve, lambda: nc.vector.tensor_tensor(out=wk[:, :, :], in0=ohv[:, :, :], in1=rank[:, :, :], op=ALU.mult), reads=[b_ohv, b_rank], writes=[b_wk])
            op(dve, lambda: nc.vector.tensor_reduce(out=rr[:, :, :], in_=wk.rearrange("p b (k e) -> p b k e", k=2), axis=AX.X, op=ALU.add), reads=[b_wk], writes=[b_rr])
            op(dve, lambda: nc.vector.tensor_scalar(out=rr[:, :, :], in0=rr[:, :, :], scalar1=-1.0, scalar2=None, op0=ALU.add), reads=[b_rr], writes=[b_rr])
            op(dve, lambda: nc.vector.tensor_scalar(out=ovt[:, :], in0=cnt[:, :], scalar1=-float(CAP), scalar2=0.0, op0=ALU.add, op1=ALU.max), reads=[b_cnt], writes=[b_ovt])
            op(dve, lambda: nc.vector.tensor_scalar(out=ovt[:, :], in0=ovt[:, :], scalar1=127.0, scalar2=1.0 / 128, op0=ALU.add, op1=ALU.mult), reads=[b_ovt], writes=[b_ovt])
            op(dve, lambda: nc.vector.tensor_scalar(out=ovt[:, :], in0=ovt[:, :], scalar1=-0.5 + 1.0 / 256, scalar2=MAGIC, op0=ALU.add, op1=ALU.add), reads=[b_ovt], writes=[b_ovt])
            op(dve, lambda: nc.vector.tensor_scalar(out=ovt[:, :], in0=ovt[:, :], scalar1=-MAGIC, scalar2=None, op0=ALU.add), reads=[b_ovt], writes=[b_ovt])
            op(dve, lambda: nc.vector.memset(one64[:, :], 1.0), writes=[b_one64])
            op(dve, lambda: nc.vector.tensor_tensor_scan(out=ovend[:, :], data0=one64[:, :], data1=ovt[:, :], initial=0.0, op0=ALU.mult, op1=ALU.add),
               reads=[b_one64, b_ovt], writes=[b_ovend])
            op(dve, lambda: nc.vector.tensor_tensor(out=ovst[:, :], in0=ovend[:, :], in1=ovt[:, :], op=ALU.subtract), reads=[b_ovend, b_ovt], writes=[b_ovst])
            op(dve, lambda: nc.vector.tensor_scalar(out=ovst[:, :], in0=ovst[:, :], scalar1=128.0, scalar2=None, op0=ALU.mult), reads=[b_ovst], writes=[b_ovst])

            def bcv(t2d):
                return t2d[:, :].unsqueeze(1).to_broadcast([128, 16, 64])
            op(dve, lambda: nc.vector.tensor_tensor(out=wk[:, :, :], in0=ohv[:, :, :], in1=bcv(vb), op=ALU.mult), reads=[b_ohv, b_vb, b_wk], writes=[b_wk])
            op(dve, lambda: nc.vector.tensor_reduce(out=basev[:, :, :], in_=wk.rearrange("p b (k e) -> p b k e", k=2), axis=AX.X, op=ALU.add), reads=[b_wk], writes=[b_basev])
            op(dve, lambda: nc.vector.tensor_tensor(out=wk[:, :, :], in0=ohv[:, :, :], in1=bcv(ovst), op=ALU.mult), reads=[b_ohv, b_ovst, b_wk], writes=[b_wk])
            op(dve, lambda: nc.vector.tensor_reduce(out=ovs[:, :, :], in_=wk.rearrange("p b (k e) -> p b k e", k=2), axis=AX.X, op=ALU.add), reads=[b_wk], writes=[b_ovs])
            op(dve, lambda: nc.vector.tensor_scalar(out=isov[:, :, :], in0=rr[:, :, :], scalar1=float(CAP), scalar2=None, op0=ALU.is_ge), reads=[b_rr], writes=[b_isov])
            op(dve, lambda: nc.vector.tensor_tensor(out=ovs[:, :, :], in0=ovs[:, :, :], in1=basev[:, :, :], op=ALU.subtract), reads=[b_ovs, b_basev], writes=[b_ovs])
            op(dve, lambda: nc.vector.scalar_tensor_tensor(out=ovs[:, :, :], in0=ovs[:, :, :], scalar=float(8192 - CAP), in1=isov[:, :, :], op0=ALU.add, op1=ALU.mult),
               reads=[b_ovs, b_isov], writes=[b_ovs])
            op(dve, lambda: nc.vector.tensor_tensor(out=posf[:, :, :], in0=basev[:, :, :], in1=rr[:, :, :], op=ALU.add), reads=[b_basev, b_rr], writes=[b_posf])
            op(dve, lambda: nc.vector.tensor_tensor(out=posf[:, :, :], in0=posf[:, :, :], in1=ovs[:, :, :], op=ALU.add), reads=[b_posf, b_ovs], writes=[b_posf])
            op(dve, lambda: nc.vector.tensor_copy(out=pos_i[:, :, :], in_=posf[:, :, :]), reads=[b_posf], writes=[b_posi])
            srcv = srcm.rearrange("p (b k) c -> p b k c", k=2)
            op(dve, lambda: nc.vector.memset(srcm[:, :, :], 0.0), writes=[b_srcm])
            for kk in range(2):
                op(dve, lambda: nc.vector.tensor_copy(out=srcv[:, :, kk, 0], in_=tokid[:, :]), reads=[b_tok], writes=[b_srcm])
                op(dve, lambda: nc.vector.tensor_scalar(out=srcv[:, :, kk, 1], in0=tokid[:, :], scalar1=float(2048 * kk), scalar2=None, op0=ALU.add), reads=[b_tok], writes=[b_srcm])
                op(dve, lambda: nc.vector.tensor_copy(out=srcv[:, :, kk, 2], in_=(w1 if kk == 0 else w2)[:, :]), reads=[b_w1, b_w2], writes=[b_srcm])
            for tb in range(16):
                for kk in range(2):
                    op(pool, lambda: nc.gpsimd.indirect_dma_start(out=maps_d[:, :], out_offset=bass.IndirectOffsetOnAxis(ap=pos_i[:, tb, kk:kk + 1], axis=0),
                                                                  in_=srcv[:, tb, kk, :], in_offset=None, bounds_check=BND[NSLOT - 1], oob_is_err=False),
                       reads=[b_posi, b_srcm, b_mapinit], dma=b_scat)
            b_scat.w = (b_scat.dsem, b_scat.dn)
            op(dve, lambda: nc.vector.tensor_tensor(out=cmpj[:, :, :], in0=ovend[:, :].unsqueeze(1).to_broadcast([128, NOV, 64]),
                                                    in1=jidx[:, :].unsqueeze(2).to_broadcast([128, NOV, 64]), op=ALU.is_le), reads=[b_ovend, b_jidx], writes=[b_cmpj])
            op(dve, lambda: nc.vector.tensor_reduce(out=ovv[:, :], in_=cmpj[:, :, :], axis=AX.X, op=ALU.add), reads=[b_cmpj], writes=[b_ovv])
            op(dve, lambda: nc.vector.tensor_scalar(out=ovf[:, :], in0=ovv[:, :], scalar1=32.0, scalar2=-32.0, op0=ALU.is_ge, op1=ALU.mult), reads=[b_ovv], writes=[b_ovf])
            op(dve, lambda: nc.vector.tensor_tensor(out=ovf[:, :], in0=ovf[:, :], in1=ovv[:, :], op=ALU.add), reads=[b_ovf, b_ovv], writes=[b_ovf])
            op(dve, lambda: nc.vector.tensor_scalar(out=ovf[:, :], in0=ovf[:, :], scalar1=1.0, scalar2=128.0, op0=ALU.add, op1=ALU.mult), reads=[b_ovf], writes=[b_ovf])
            op(dve, lambda: nc.vector.tensor_scalar(out=ovf[:, :], in0=ovf[:, :], scalar1=pidx[:, 0:1], scalar2=None, op0=ALU.add), reads=[b_ovf, b_pidx], writes=[b_ovf])
            op(dve, lambda: nc.vector.tensor_scalar(out=ovv[:, :], in0=ovv[:, :], scalar1=64.0, scalar2=1.0e6, op0=ALU.is_ge, op1=ALU.mult), reads=[b_ovv], writes=[b_ovv])
            op(dve, lambda: nc.vector.tensor_tensor(out=ovf[:, :], in0=ovf[:, :], in1=ovv[:, :], op=ALU.add), reads=[b_ovf, b_ovv], writes=[b_ovf])
            op(dve, lambda: nc.vector.tensor_copy(out=ovidx[:, :], in_=ovf[:, :]), reads=[b_ovf], writes=[b_ovidx])
            op(dve, lambda: nc.vector.tensor_tensor(out=oh1[:, :, :], in0=oh1[:, :, :], in1=bc(w1, 32), op=ALU.mult), reads=[b_oh1, b_w1], writes=[b_oh1])
            op(dve, lambda: nc.vector.tensor_tensor(out=oh2[:, :, :], in0=oh2[:, :, :], in1=bc(w2, 32), op=ALU.mult), reads=[b_oh2, b_w2], writes=[b_oh2])
            op(dve, lambda: nc.vector.tensor_tensor(out=comb[:, :, 1:33], in0=oh1[:, :, :], in1=oh2[:, :, :], op=ALU.add), reads=[b_oh1, b_oh2, b_comb], writes=[b_comb])
            if dbg:
                dbg_out["comb"] = dout("dbg_comb", [128, 16, 33])
                bd = dbuf()
                op(sp, lambda: nc.sync.dma_start(out=dbg_out["comb"][:, :, :], in_=comb[:, :, :]), reads=[b_comb], writes=[bd], dma=bd)
                _wait(sp, bd.w)
            _wait(sp, b_yall.w)
            nc.all_engine_barrier()

        acc = at(RB[0], [128, 16, D], F32)
        b_acc = [Buf() for _ in range(16)]
        b_Ysc = dbuf()
        es = Bump(RF, RC, RD)
        NS = 4
        ra_b = Bump(RA)
        wgu = [SB(es if i < 2 else ra_b, "wgu%d" % i, [128, 8, 512], BF16) for i in range(NS)]
        wd = [SB(es if i < 2 else ra_b, "wd%d" % i, [128, 2, D], BF16) for i in range(NS)]
        b_wg = [dbuf() for _ in range(NS)]
        b_wg2 = [dbuf() for _ in range(NS)]
        b_wdn = [dbuf() for _ in range(NS)]

        def load_gu(e):
            s_ = e % NS
            rows = slice(e * 128, (e + 1) * 128)
            op(pool, lambda: nc.gpsimd.dma_start(out=wgu[s_][:, 0:4, :].rearrange("p c f -> p (c f)"), in_=wgu_lo[rows, :]), writes=[b_wg[s_]], dma=b_wg[s_])
            op(pool, lambda: nc.gpsimd.dma_start(out=wgu[s_][:, 4:8, :].rearrange("p c f -> p (c f)"), in_=wgu_hi[rows, :]), writes=[b_wg2[s_]], dma=b_wg2[s_])

        def load_d(e):
            s_ = e % NS
            rows = slice(e * 128, (e + 1) * 128)
            op(pool, lambda: nc.gpsimd.dma_start(out=wd[s_][:, :, :].rearrange("p j d -> p (j d)"), in_=wd_h[rows, :]), writes=[b_wdn[s_]], dma=b_wdn[s_])

        load_gu(0)
        load_d(0)
        with ExitStack() as pes:
            sg = [SB(es, "sg%d" % i, [128, 512], BF16) for i in range(2)]; b_sg = [Buf(), Buf()]
            hid = [SB(es, "hid%d" % i, [128, 2, 512], BF16) for i in range(2)]; b_hid = [Buf(), Buf()]
            ps_g = [PS(pes, "ps_g%d" % i, [128, 512]) for i in range(2)]; b_psg = [Buf(), Buf()]
            ps_u = [PS(pes, "ps_u%d" % i, [128, 512]) for i in range(2)]; b_psu = [Buf(), Buf()]
            ps_d = [PS(pes, "ps_d%d" % i, [128, D]) for i in range(2)]; b_psd = [Buf(), Buf()]

            def GU(t, hsl):
                tcs = slice(t * 512, (t + 1) * 512)
                rd = [b_wg[0], b_wg2[0]] + b_hn2[t * 4:(t + 1) * 4]
                for j in range(2):
                    op(pe, mm_acc(ps_g[j][:, :], lambda c: wgu[0][:, c, j * 128:(j + 1) * 128], lambda c: hn2T[:, c, tcs], 8), reads=rd, writes=[b_psg[j]])
                for j in range(2):
                    op(pe, mm_acc(ps_u[j][:, :], lambda c: wgu[0][:, c, 256 + j * 128:256 + (j + 1) * 128], lambda c: hn2T[:, c, tcs], 8), reads=rd, writes=[b_psu[j]])
                for j in range(2):
                    op(act, lambda: nc.scalar.activation(out=sg[j][:, :], in_=ps_g[j][:, :], func=AF.Silu), reads=[b_psg[j]], writes=[b_sg[j]])
                for j in range(2):
                    op(dve, lambda: nc.vector.tensor_tensor(out=hid[hsl][:, j, :], in0=ps_u[j][:, :], in1=sg[j][:, :], op=ALU.mult),
                       reads=[b_psu[j], b_sg[j]], writes=[b_hid[hsl]])

            def DOWN(t, hsl):
                for r in range(4):
                    tb = t * 4 + r
                    db = tb % 2

                    def f():
                        ins = None
                        for half in range(2):
                            hc = slice(half * 512, (half + 1) * 512)
                            for j in range(2):
                                ins = nc.tensor.matmul(ps_d[db][:, hc], lhsT=hid[hsl][:, j, r * 128:(r + 1) * 128], rhs=wd[0][:, j, hc], start=(j == 0), stop=(j == 1))
                        return ins
                    op(pe, f, reads=[b_hid[hsl], b_wdn[0]], writes=[b_psd[db]])
                    op(act, lambda: nc.scalar.activation(out=acc[:, tb, :], in_=ps_d[db][:, :], func=AF.Copy), reads=[b_psd[db]], writes=[b_acc[tb]])

            for t in range(4):
                GU(t, t % 2)
                if t > 0:
                    DOWN(t - 1, (t - 1) % 2)
            DOWN(3, 1)
            _wait(sp, b_scat.w)
            op(sp, lambda: nc.sync.dma_start(out=mapt[:, :, :], in_=maps_d.rearrange("(s p) c -> p s c", p=128)), reads=[b_scat], writes=[b_mapt], dma=b_mapt)
            op(dve, lambda: nc.vector.tensor_copy(out=gidx[:, :], in_=mapt[:, :, 0]), reads=[b_mapt], writes=[b_gidx])
            op(dve, lambda: nc.vector.tensor_copy(out=sidx[:, :], in_=mapt[:, :, 1]), reads=[b_mapt], writes=[b_sidx])
            nc.all_engine_barrier()

        with ExitStack() as pes:
            ident_b, b_idb = load(es, "ident_b", ident_d[:, :], [128, 128], BF16, cast=True)
            NOVS = 3
            wgu_ov = [SB(es, "wgu_ov%d" % i, [128, 8, 512], BF16) for i in range(NOVS)]
            wd_ov = [SB(es, "wd_ov%d" % i, [128, 2, D], BF16) for i in range(NOVS)]
            b_wgov = [dbuf() for _ in range(NOVS)]
            b_wgov2 = [dbuf() for _ in range(NOVS)]
            b_wdov = [dbuf() for _ in range(NOVS)]
            for i in range(NOVS):
                op(dve, lambda: nc.vector.memset(wgu_ov[i][:], 0.0), writes=[b_wgov[i], b_wgov2[i]])
                op(dve, lambda: nc.vector.memset(wd_ov[i][:], 0.0), writes=[b_wdov[i]])
            NXG = 4
            xg = [SB(es, "xg%d" % i, [128, D], BF16) for i in range(NXG)]; b_xg = [dbuf() for _ in range(NXG)]
            for i in range(NXG):
                op(dve, lambda: nc.vector.memset(xg[i][:], 0.0), writes=[b_xg[i]])
            xT = [SB(es, "xT%d" % i, [128, 8, 128], BF16) for i in range(3)]; b_xT = [Buf() for _ in range(3)]
            sgt = [SB(es, "sgt%d" % i, [128, 256], BF16) for i in range(2)]; b_sgt = [Buf(), Buf()]
            hidt = [SB(es, "hidt%d" % i, [128, 256], BF16) for i in range(3)]; b_hidt = [Buf() for _ in range(3)]
            hT = [SB(es, "hT%d" % i, [128, 2, 128], BF16) for i in range(3)]; b_hT = [Buf() for _ in range(3)]
            yo = [SB(es, "yo%d" % i, [128, D], F32) for i in range(2)]; b_yo = [Buf(), Buf()]
            ps_xT = [PS(pes, "ps_xT%d" % i, [128, 8, 128], BF16) for i in range(2)]; b_pxT = [Buf(), Buf()]
            ps_gu = [PS(pes, "ps_gu%d" % i, [128, 512]) for i in range(2)]; b_pgu = [Buf(), Buf()]
            ps_hT = PS(pes, "ps_hT", [128, 2, 128], BF16); b_phT = Buf()
            ps_dn = PS(pes, "ps_dn", [128, D]); b_pdn = Buf()

            tiles = [(k_ * 32 + e_, e_ + 1, None) for e_ in range(NEXP) for k_ in range(2)] + [(64 + j_, None, j_) for j_ in range(NOV)]
            NTL = len(tiles)
            def load_gu_fast(e):
                s_ = e % NS
                rows = slice(e * 128, (e + 1) * 128)
                op(sp, lambda: nc.sync.dma_start(out=wgu[s_][:, 0:4, :].rearrange("p c f -> p (c f)"), in_=wbf_gu[rows, 0:2048]), reads=b_outd, writes=[b_wg[s_]], dma=b_wg[s_])
                op(sp, lambda: nc.sync.dma_start(out=wgu[s_][:, 4:8, :].rearrange("p c f -> p (c f)"), in_=wbf_gu[rows, 2048:4096]), reads=b_outd, writes=[b_wg2[s_]], dma=b_wg2[s_])

            def load_d_fast(e):
                s_ = e % NS
                rows = slice(e * 128, (e + 1) * 128)
                op(sp, lambda: nc.sync.dma_start(out=wd[s_][:, :, :].rearrange("p j d -> p (j d)"), in_=wbf_d[rows, :]), reads=b_outd, writes=[b_wdn[s_]], dma=b_wdn[s_])

            def wgu_of(ti):
                scol, e_st, j_ov = tiles[ti]
                if e_st is not None:
                    return wgu[e_st % NS], [b_wg[e_st % NS], b_wg2[e_st % NS]]
                return wgu_ov[j_ov % NOVS], [b_wgov[j_ov % NOVS], b_wgov2[j_ov % NOVS]]

            def wd_of(ti):
                scol, e_st, j_ov = tiles[ti]
                if e_st is not None:
                    return wd[e_st % NS], b_wdn[e_st % NS]
                return wd_ov[j_ov % NOVS], b_wdov[j_ov % NOVS]

            def stage_G(ti):
                scol, e_st, j_ov = tiles[ti]
                if e_st is not None:
                    if scol < 32:
                        load_gu_fast(e_st)
                else:
                    W_gu, bW = wgu_of(ti)
                    ioa = bass.IndirectOffsetOnAxis(ap=ovidx[:, j_ov:j_ov + 1], axis=0)
                    op(pool, lambda: nc.gpsimd.indirect_dma_start(out=W_gu[:, :, :].rearrange("p c f -> p (c f)"), out_offset=None, in_=wbf_gu[:, :], in_offset=ioa,
                                                                  bounds_check=BND[NROW - 1], oob_is_err=False),
                       reads=[b_ovidx] + b_outd, writes=[bW[0], bW[1]], dma=bW[0])

            def stage_Gx(ti):
                scol = tiles[ti][0]
                g4 = ti % NXG
                op(pool, lambda: nc.gpsimd.indirect_dma_start(out=xg[g4][:, :], out_offset=None, in_=hn2d[:, :],
                                                              in_offset=bass.IndirectOffsetOnAxis(ap=gidx[:, scol:scol + 1], axis=0), bounds_check=BND[2048], oob_is_err=False),
                   reads=[b_gidx], writes=[b_xg[g4]], dma=b_xg[g4])

            def stage_G2(ti):
                scol, e_st, j_ov = tiles[ti]
                if e_st is not None:
                    if scol < 32:
                        load_d_fast(e_st)
                else:
                    W_d, bW = wd_of(ti)
                    ioa = bass.IndirectOffsetOnAxis(ap=ovidx[:, j_ov:j_ov + 1], axis=0)
                    op(pool, lambda: nc.gpsimd.indirect_dma_start(out=W_d[:, :, :].rearrange("p j d -> p (j d)"), out_offset=None, in_=wbf_d[:, :], in_offset=ioa,
                                                                  bounds_check=BND[NROW - 1], oob_is_err=False),
                       reads=[b_ovidx], writes=[bW], dma=bW)

            def stage_A(ti):
                g4, p2, x3 = ti % NXG, ti % 2, ti % 3

                def ftr():
                    ins = None
                    for c in range(8):
                        ins = nc.tensor.transpose(ps_xT[p2][:, c, :], xg[g4][:, c * 128:(c + 1) * 128], ident_b[:, :])
                    return ins
                op(pe, ftr, reads=[b_xg[g4], b_idb], writes=[b_pxT[p2]])
                op(act, lambda: nc.scalar.activation(out=xT[x3][:, :, :], in_=ps_xT[p2][:, :, :], func=AF.Copy), reads=[b_pxT[p2]], writes=[b_xT[x3]])

            def stage_B(ti):
                p2, x3 = ti % 2, ti % 3
                W_gu, bW = wgu_of(ti)
                op(pe, mm_acc(ps_gu[p2][:, :], lambda c: xT[x3][:, c, :], lambda c: W_gu[:, c, :], 8), reads=[b_xT[x3]] + bW, writes=[b_pgu[p2]])
                op(act, lambda: nc.scalar.activation(out=sgt[p2][:, :], in_=ps_gu[p2][:, 0:256], func=AF.Silu), reads=[b_pgu[p2]], writes=[b_sgt[p2]])
                op(dve, lambda: nc.vector.tensor_tensor(out=hidt[x3][:, :], in0=ps_gu[p2][:, 256:512], in1=sgt[p2][:, :], op=ALU.mult),
                   reads=[b_pgu[p2], b_sgt[p2]], writes=[b_hidt[x3]])

            def stage_C(ti):
                x3 = ti % 3

                def ftr2():
                    ins = None
                    for j in range(2):
                        ins = nc.tensor.transpose(ps_hT[:, j, :], hidt[x3][:, j * 128:(j + 1) * 128], ident_b[:, :])
                    return ins
                op(pe, ftr2, reads=[b_hidt[x3], b_idb], writes=[b_phT])
                op(dve, lambda: nc.vector.tensor_copy(out=hT[x3][:, :, :], in_=ps_hT[:, :, :]), reads=[b_phT], writes=[b_hT[x3]])

            def stage_D(ti):
                scol = tiles[ti][0]
                p2, x3 = ti % 2, ti % 3
                W_d, bW = wd_of(ti)

                def fdn():
                    ins = None
                    for half in range(2):
                        hc = slice(half * 512, (half + 1) * 512)
                        for j in range(2):
                            ins = nc.tensor.matmul(ps_dn[:, hc], lhsT=hT[x3][:, j, :], rhs=W_d[:, j, hc], start=(j == 0), stop=(j == 1))
                    return ins
                op(pe, fdn, reads=[b_hT[x3], bW], writes=[b_pdn])
                op(act, lambda: nc.scalar.activation(out=yo[p2][:, :], in_=ps_dn[:, :], func=AF.Copy, scale=mapt[:, scol, 2:3]),
                   reads=[b_pdn, b_mapt], writes=[b_yo[p2]])
                op(pool, lambda: nc.gpsimd.indirect_dma_start(out=Yd[:, :], out_offset=bass.IndirectOffsetOnAxis(ap=sidx[:, scol:scol + 1], axis=0),
                                                              in_=yo[p2][:, :], in_offset=None, bounds_check=BND[4095], oob_is_err=False),
                   reads=[b_yo[p2], b_sidx], dma=b_Ysc)

            stage_Gx(0)
            stage_Gx(1)
            stage_Gx(2)
            stage_G(0)
            stage_G(1)
            for it in range(NTL + 3):
                if it + 3 < NTL:
                    stage_Gx(it + 3)
                if it < NTL:
                    stage_A(it)
                if 0 <= it - 1 < NTL:
                    stage_B(it - 1)
                if 0 <= it - 2 < NTL:
                    stage_C(it - 2)
                if 0 <= it - 3 < NTL:
                    stage_D(it - 3)
                if it + 2 < NTL:
                    stage_G(it + 2)
                if it < NTL:
                    stage_G2(it)
            b_Ysc.w = (b_Ysc.dsem, b_Ysc.dn)
            _wait(pool, b_Ysc.w)
            nc.all_engine_barrier()

        b_yout = dbuf()
        with ExitStack() as pes:
            es = Bump(RF, RC, RD)
            g_po, b_gpo = load(es, "g_po_bc", g_po_d[0:1, :].partition_broadcast(128), [128, D])
            hf = [SB(es, "hf%d" % i, [128, D], F32) for i in range(3)]; b_hf = [dbuf() for _ in range(3)]
            y0 = [SB(es, "y0%d" % i, [128, D], F32) for i in range(3)]; b_y0 = [dbuf() for _ in range(3)]
            y1 = [SB(es, "y1%d" % i, [128, D], F32) for i in range(3)]; b_y1 = [dbuf() for _ in range(3)]

            def load_fin(tb_):
                k3 = tb_ % 3
                op(sp, lambda: nc.sync.dma_start(out=hf[k3][:, :], in_=y[tb_]), reads=[b_yall], writes=[b_hf[k3]], dma=b_hf[k3])
                op(sp, lambda: nc.sync.dma_start(out=y0[k3][:, :], in_=Yd[tb_ * 128:(tb_ + 1) * 128, :]), reads=[b_Ysc], writes=[b_y0[k3]], dma=b_y0[k3])
                op(sp, lambda: nc.sync.dma_start(out=y1[k3][:, :], in_=Yd[2048 + tb_ * 128:2048 + (tb_ + 1) * 128, :]), reads=[b_Ysc], writes=[b_y1[k3]], dma=b_y1[k3])
            load_fin(0)
            load_fin(1)
            oo = [SB(es, "oo%d" % i, [128, D], F32) for i in range(2)]; b_oo = [Buf(), Buf()]
            junk = SB(es, "f_junk", [128, D], BF16); b_junk = Buf()
            sm = SB(es, "f_sm", [128, 4], F32); b_s0 = Buf(); b_s1 = Buf()
            for tb in range(16):
                k = tb % 2
                k3 = tb % 3
                if tb + 2 < 16:
                    load_fin(tb + 2)
                op(dve, lambda: nc.vector.tensor_tensor(out=y0[k3][:, :], in0=y0[k3][:, :], in1=y1[k3][:, :], op=ALU.add), reads=[b_y0[k3], b_y1[k3]], writes=[b_y0[k3]])
                op(dve, lambda: nc.vector.tensor_tensor(out=acc[:, tb, :], in0=acc[:, tb, :], in1=y0[k3][:, :], op=ALU.add), reads=[b_acc[tb], b_y0[k3]], writes=[b_acc[tb]])
                op(act, lambda: nc.scalar.activation(out=junk[:, :], in_=acc[:, tb, :], func=AF.Square, accum_out=sm[:, 0:1]),
                   reads=[b_acc[tb]], writes=[b_junk, b_s0])
                op(act, lambda: nc.scalar.activation(out=sm[:, 1:2], in_=sm[:, 0:1], func=AF.Sqrt, scale=1.0 / D, bias=eps_t[:, 0:1]),
                   reads=[b_s0, b_eps], writes=[b_s1])
                op(dve, lambda: nc.vector.reciprocal(out=sm[:, 1:2], in_=sm[:, 1:2]), reads=[b_s1], writes=[b_s1])
                op(dve, lambda: nc.vector.scalar_tensor_tensor(out=oo[k][:, :], in0=acc[:, tb, :], scalar=sm[:, 1:2], in1=g_po[:, :], op0=ALU.mult, op1=ALU.mult),
                   reads=[b_acc[tb], b_s1, b_gpo], writes=[b_oo[k]])
                op(dve, lambda: nc.vector.tensor_tensor(out=oo[k][:, :], in0=oo[k][:, :], in1=hf[k3][:, :], op=ALU.add), reads=[b_oo[k], b_hf[k3]], writes=[b_oo[k]])
                op(sp, lambda: nc.sync.dma_start(out=y[tb], in_=oo[k][:, :]), reads=[b_oo[k]], writes=[b_yout], dma=b_yout)
            _wait(sp, b_yout.w)
    return nc, dbg_out


def _prep_shared(inp):
    f = np.float32
    def cp(a):
        return np.ascontiguousarray(a, dtype=f)
    w_in = cp(inp["w_in"][0].reshape(8, 128, 1952).transpose(1, 0, 2))
    w_uq = cp(inp["w_uq"][0].reshape(2, 128, 768).transpose(1, 0, 2))
    w_ukv = cp(inp["w_ukv"][0])
    w_out = inp["w_out"][0]
    w_out_a = cp(w_out[:512].reshape(4, 128, D).transpose(1, 0, 2))
    w_out_c = cp(w_out[512:].reshape(4, 128, D).transpose(1, 0, 2))
    wr = np.concatenate([inp["w_group_router"][0], inp["w_expert_router"][0]], axis=1)
    wr = cp(wr.reshape(8, 128, 36).transpose(1, 0, 2))
    br = cp(np.concatenate([inp["b_group_router"][0], inp["b_expert_router"][0]])[None, :])
    w_gate = np.concatenate([inp["w_sh_gate"], inp["w_gate"][0]], axis=0)
    w_up = np.concatenate([inp["w_sh_up"], inp["w_up"][0]], axis=0)
    w_down = np.concatenate([inp["w_sh_down"], inp["w_down"][0]], axis=0)
    gu = np.empty((NEXP + 1, 128, 8, 512), f)
    gu[:, :, :, 0:256] = w_gate.reshape(NEXP + 1, 8, 128, 256).transpose(0, 2, 1, 3)
    gu[:, :, :, 256:512] = w_up.reshape(NEXP + 1, 8, 128, 256).transpose(0, 2, 1, 3)
    wgu_lo = cp(gu[:, :, 0:4].reshape((NEXP + 1) * 128, 2048))
    wgu_hi = cp(gu[:, :, 4:8].reshape((NEXP + 1) * 128, 2048))
    wd_h = cp(w_down.reshape(NEXP + 1, 2, 128, D).transpose(0, 2, 1, 3).reshape((NEXP + 1) * 128, 2048))
    triu = np.triu(np.ones((128, 128), f))
    vbase = (np.arange(64, dtype=f) * 128)[None, :]
    tokid = (np.arange(16, dtype=f)[None, :] * 128 + np.arange(128, dtype=f)[:, None])
    pidx = np.arange(128, dtype=f)[:, None]
    jidx = np.arange(NOV, dtype=f)[None, :]
    mapinit = np.zeros((NSLOT, 4), f); mapinit[:, 0] = 2048.0; mapinit[:, 1] = 1.0e6
    def pc(v, p):
        return cp(v.reshape(-1, p).T)
    ident = np.eye(128, dtype=f)
    rot = np.zeros((96, 96), f)
    for i in range(16):
        rot[64 + i + 16, 64 + i] = -1.0
        rot[64 + i, 64 + i + 16] = 1.0
    sel = np.zeros((65, 64), f); sel[64, :] = 1.0
    inv_freq = (1.0 / (10000.0 ** (np.arange(0, 32, 2, dtype=np.float32) / np.float32(32)))).astype(f)
    invf = np.zeros((96, 1), f); invf[64:80, 0] = inv_freq; invf[80:96, 0] = inv_freq
    dmask = np.zeros((128, 4, 512), f)
    for d_ in range(4):
        dmask[:, d_, :] = (np.arange(512)[None, :] >= (np.arange(128)[:, None] + 128 * d_)).astype(f)
    prefix = np.concatenate([np.zeros(112, np.int32), np.arange(16, dtype=np.int32)])[None, :]
    return dict(
        w_in=w_in, w_uq=w_uq, w_ukv=w_ukv, w_out_a=w_out_a, w_out_c=w_out_c, wr=wr, br=br,
        wgu_lo=wgu_lo, wgu_hi=wgu_hi, wd_h=wd_h, triu=cp(triu), vbase=cp(vbase), tokid=cp(tokid), pidx=cp(pidx), jidx=cp(jidx), mapinit=mapinit,
        g_pfb=cp(inp["pre_ffn_norm"][0][None, :]),
        g_pre=pc(inp["pre_mix_norm"][0], 128), g_q=pc(inp["q_norm"][0], 128), g_kv=pc(inp["kv_norm"][0], 128),
        g_a=pc(inp["attn_out_norm"][0], 128), g_c=pc(inp["conv_out_norm"][0], 128),
        convw=cp(inp["conv_w"][0].T.reshape(4, 128, 3).transpose(1, 0, 2)),
        g_pm=cp(inp["post_mix_norm"][0][None, :]), g_pf=pc(inp["pre_ffn_norm"][0], 128), g_po=cp(inp["post_ffn_norm"][0][None, :]),
        ident=ident, rot96=rot, invf=invf, dmask=dmask, prefix=prefix,
    )


def groups_of(j):
    return [j, 7 - j, 8 + j, 15 - j]


def _prep_core(inp, core, shared, batch_cache):
    b, j = core // 4, core % 4
    f = np.float32
    x = inp["x"]
    if b not in batch_cache:
        xb = np.asarray(x[b], dtype=f)
        hTm = np.ascontiguousarray(xb.reshape(16, 512, 8, 128).transpose(0, 3, 2, 1))
        h0 = np.zeros((128, D), f)
        h0[112:] = np.asarray(inp["meta_tokens"], dtype=f)
        hT0 = np.ascontiguousarray(h0.reshape(128, 8, 128).transpose(2, 1, 0))
        pos = np.ascontiguousarray(np.asarray(inp["positions"][b], dtype=np.int32)[None, :])
        batch_cache[b] = (hTm, hT0, pos)
    hTm, hT0, pos = batch_cache[b]
    Gs = groups_of(j)
    hq = np.empty((4, 128, 8, 514), f)
    posq = np.empty((4, 512), np.int32)
    xown = np.empty((16, 128, D), f)
    order = []
    abias = np.zeros((128, 12), f)
    for s_, G in enumerate(Gs):
        others = [g for g in range(4 * s_, 4 * s_ + 4) if g != G]
        emp = [g for g in others if g > G]
        ful = [g for g in others if g < G]
        reg = emp + ful + [G]
        for r_, g in enumerate(reg[:3]):
            abias[:, s_ * 3 + r_] = -30000.0 if g > G else 0.0
        order += reg
    hTm_c = np.ascontiguousarray(hTm[order])
    pos_c = np.ascontiguousarray(pos.reshape(16, 512)[order].reshape(1, SEQ))
    for gi, G in enumerate(Gs):
        hq[gi, :, :, 2:] = hTm[G]
        hq[gi, :, :, 0:2] = hTm[G - 1][:, :, 510:512] if G > 0 else hT0[:, :, 126:128]
        posq[gi] = pos[0, 512 * G:512 * G + 512]
        xown[gi * 4:(gi + 1) * 4] = np.asarray(x[b, 512 * G:512 * G + 512], dtype=f).reshape(4, 128, D)
    m = dict(shared)
    m.update(hT0=hT0, hTm=hTm_c, hq=hq, pos=pos_c, posq=posq, xown=xown, abias=abias)
    return m


def kernel(**inputs):
    dbg = bool(os.environ.get("MK_DEBUG"))
    inp = {k: np.asarray(v) for k, v in inputs.items()}
    shared = _prep_shared(inp)
    cache = {}
    in_maps = [_prep_core(inp, c, shared, cache) for c in range(8)]
    nc, dbg_out = build_program(dbg=dbg)
    res = run_bass_kernel_spmd(nc, in_maps, core_ids=list(range(8)))
    out = np.empty((2, SEQ, D), np.float32)
    for c in range(8):
        b, j = c // 4, c % 4
        yv = np.asarray(res.results[c]["y"])
        for gi, G in enumerate(groups_of(j)):
            out[b, 512 * G:512 * G + 512] = yv[gi * 4:(gi + 1) * 4].reshape(512, D)
    if dbg:
        kernel.dbg = [{k: np.asarray(res.results[c]["dbg_" + k]) for k in dbg_out} for c in range(8)]
    return out
```
